# Optimizing a Trainium2 kernel written in Bass

```python
import math
import jax
import jax.numpy as jnp
from jax import lax
import numpy as np

D_MODEL = 2048
BATCH = 16
SEQ = 2048
DEPTH = 4

N_MIXERS = 3
CHUNK = 128
NORM_EPS = 1e-5
D_FF = 4 * D_MODEL

SSD_EXPAND = 2
SSD_INNER = SSD_EXPAND * D_MODEL
SSD_HEAD_DIM = 64
SSD_HEADS = SSD_INNER // SSD_HEAD_DIM
SSD_GROUPS = 8
SSD_HPG = SSD_HEADS // SSD_GROUPS
SSD_STATE = 128
SSD_CONV = 4
SSD_CONV_DIM = SSD_INNER + 2 * SSD_GROUPS * SSD_STATE
SSD_PROJ = SSD_INNER + SSD_CONV_DIM + SSD_HEADS
N_SSD_LAYERS = (DEPTH + 2) // 3

MLSTM_HEADS = 4
MLSTM_QK = D_MODEL // 2
MLSTM_V = D_MODEL
MLSTM_DQK = MLSTM_QK // MLSTM_HEADS
MLSTM_DV = MLSTM_V // MLSTM_HEADS
MLSTM_PROJ = 2 * MLSTM_QK + 2 * MLSTM_V + 2 * MLSTM_HEADS
N_MLSTM_LAYERS = (DEPTH + 1) // 3

S5_WIDTH = D_MODEL
S5_GROUP = 16
S5_GROUPS = S5_WIDTH // S5_GROUP
S5_STATE = 64
N_S5_LAYERS = DEPTH // 3

DT_MIN = 1e-3
DT_MAX = 1e-1

kernel_name = 'hybrid_ssd_mlstm_s5_trunk'


def rmsnorm(x, g):
    xf = x.astype(jnp.float32)
    y = xf * lax.rsqrt(jnp.mean(xf * xf, axis=-1, keepdims=True) + NORM_EPS)
    return y * g.astype(jnp.float32)


def to_chunks(t):
    b, s = t.shape[0], t.shape[1]
    return jnp.moveaxis(t.reshape((b, s // CHUNK, CHUNK) + t.shape[2:]), 1, 0)


def from_chunks(t):
    nc, b, l = t.shape[0], t.shape[1], t.shape[2]
    return jnp.moveaxis(t, 0, 1).reshape((b, nc * l) + t.shape[3:])


def causal_depthwise_conv(x, w, bias):
    k, c = w.shape
    y = lax.conv_general_dilated(x, w[:, None, :].astype(x.dtype), window_strides=(1,),
                                 padding=[(k - 1, 0)], dimension_numbers=('NWC', 'WIO', 'NWC'),
                                 feature_group_count=c)
    return y + bias.astype(x.dtype)


def ssd_chunked_scan(xdt, loga, bmat, cmat):
    b = xdt.shape[0]
    causal = jnp.tril(jnp.ones((CHUNK, CHUNK), dtype=bool))

    def step(state, inp):
        x_c, la_c, b_c, c_c = inp
        acs = jnp.cumsum(la_c, axis=1)
        seg = acs[:, :, None] - acs[:, None, :]
        decay = jnp.exp(jnp.where(causal[None, :, :, None, None], seg, -jnp.inf))
        cb = jnp.einsum('blgn,bsgn->blsg', c_c, b_c)
        y_diag = jnp.einsum('blsgj,bsgjp->blgjp', cb[..., None] * decay, x_c)
        y_off = jnp.einsum('blgn,bgjpn->blgjp', c_c, state) * jnp.exp(acs)[..., None]
        tail = jnp.exp(acs[:, -1:] - acs)
        new_state = (state * jnp.exp(acs[:, -1])[..., None, None]
                     + jnp.einsum('bsgn,bsgj,bsgjp->bgjpn', b_c, tail, x_c))
        return new_state, y_diag + y_off

    state0 = jnp.zeros((b, SSD_GROUPS, SSD_HPG, SSD_HEAD_DIM, SSD_STATE), jnp.float32)
    _, ys = lax.scan(step, state0, (to_chunks(xdt), to_chunks(loga), to_chunks(bmat), to_chunks(cmat)))
    return from_chunks(ys)


def ssd_mixer(h, w_in, conv_w, conv_b, dt_bias, a_log, d_skip, norm_g, w_out):
    b, s, _ = h.shape
    f32 = jnp.float32
    proj = h @ w_in
    z = proj[..., :SSD_INNER]
    xbc = proj[..., SSD_INNER:SSD_INNER + SSD_CONV_DIM]
    dt_raw = proj[..., SSD_INNER + SSD_CONV_DIM:]
    xbc = jax.nn.silu(causal_depthwise_conv(xbc, conv_w, conv_b)).astype(f32)
    nb = SSD_GROUPS * SSD_STATE
    xs = xbc[..., :SSD_INNER].reshape(b, s, SSD_GROUPS, SSD_HPG, SSD_HEAD_DIM)
    bmat = xbc[..., SSD_INNER:SSD_INNER + nb].reshape(b, s, SSD_GROUPS, SSD_STATE)
    cmat = xbc[..., SSD_INNER + nb:].reshape(b, s, SSD_GROUPS, SSD_STATE)
    dt = jax.nn.softplus(dt_raw.astype(f32) + dt_bias.astype(f32)).reshape(b, s, SSD_GROUPS, SSD_HPG)
    a = -jnp.exp(a_log.astype(f32)).reshape(SSD_GROUPS, SSD_HPG)
    y = ssd_chunked_scan(xs * dt[..., None], dt * a, bmat, cmat)
    y = y + d_skip.astype(f32).reshape(SSD_GROUPS, SSD_HPG, 1) * xs
    y = y.reshape(b, s, SSD_INNER) * jax.nn.silu(z.astype(f32))
    return rmsnorm(y, norm_g).astype(h.dtype) @ w_out


def mlstm_chunked(q, k, v, i_pre, log_f):
    b = q.shape[0]
    causal = jnp.tril(jnp.ones((CHUNK, CHUNK), dtype=bool))

    def step(carry, inp):
        c_st, n_st, m_st = carry
        q_c, k_c, v_c, i_c, lf_c = inp
        bcum = jnp.cumsum(lf_c, axis=1)
        intra = bcum[:, :, None] - bcum[:, None, :] + i_c[:, None, :]
        intra = jnp.where(causal[None, :, :, None], intra, -jnp.inf)
        inter = bcum + m_st[:, None]
        m_t = jnp.maximum(inter, jnp.max(intra, axis=2))
        w = jnp.exp(intra - m_t[:, :, None])
        scale_inter = jnp.exp(inter - m_t)
        qk = jnp.einsum('blhd,bshd->blsh', q_c, k_c) * w
        num = (jnp.einsum('blsh,bshe->blhe', qk, v_c)
               + scale_inter[..., None] * jnp.einsum('blhd,bhde->blhe', q_c, c_st))
        den = jnp.sum(qk, axis=2) + scale_inter * jnp.einsum('blhd,bhd->blh', q_c, n_st)
        h_c = num / jnp.maximum(jnp.abs(den), jnp.exp(-m_t))[..., None]
        b_last = bcum[:, -1]
        tail = b_last[:, None] - bcum + i_c
        m_new = jnp.maximum(b_last + m_st, jnp.max(tail, axis=1))
        carry_scale = jnp.exp(b_last + m_st - m_new)
        wk = jnp.exp(tail - m_new[:, None])
        c_new = carry_scale[..., None, None] * c_st + jnp.einsum('bsh,bshd,bshe->bhde', wk, k_c, v_c)
        n_new = carry_scale[..., None] * n_st + jnp.einsum('bsh,bshd->bhd', wk, k_c)
        return (c_new, n_new, m_new), h_c

    carry0 = (jnp.zeros((b, MLSTM_HEADS, MLSTM_DQK, MLSTM_DV), jnp.float32),
              jnp.zeros((b, MLSTM_HEADS, MLSTM_DQK), jnp.float32),
              jnp.zeros((b, MLSTM_HEADS), jnp.float32))
    _, hs = lax.scan(step, carry0, (to_chunks(q), to_chunks(k), to_chunks(v),
                                    to_chunks(i_pre), to_chunks(log_f)))
    return from_chunks(hs)


def mlstm_mixer(h, w_in, gate_b, head_g, w_out):
    b, s, _ = h.shape
    f32 = jnp.float32
    proj = h @ w_in
    q = proj[..., :MLSTM_QK].astype(f32).reshape(b, s, MLSTM_HEADS, MLSTM_DQK) * (MLSTM_DQK ** -0.5)
    k = proj[..., MLSTM_QK:2 * MLSTM_QK].astype(f32).reshape(b, s, MLSTM_HEADS, MLSTM_DQK)
    v = proj[..., 2 * MLSTM_QK:2 * MLSTM_QK + MLSTM_V].astype(f32).reshape(b, s, MLSTM_HEADS, MLSTM_DV)
    o = proj[..., 2 * MLSTM_QK + MLSTM_V:2 * MLSTM_QK + 2 * MLSTM_V].astype(f32)
    gates = proj[..., 2 * MLSTM_QK + 2 * MLSTM_V:].astype(f32) + gate_b.astype(f32)
    i_pre = gates[..., :MLSTM_HEADS]
    log_f = jax.nn.log_sigmoid(gates[..., MLSTM_HEADS:])
    hs = mlstm_chunked(q, k, v, i_pre, log_f)
    hs = rmsnorm(hs, head_g.reshape(MLSTM_HEADS, MLSTM_DV))
    out = jax.nn.sigmoid(o) * hs.reshape(b, s, MLSTM_V)
    return out.astype(h.dtype) @ w_out


def complex_scan_combine(e1, e2):
    a1r, a1i, b1r, b1i = e1
    a2r, a2i, b2r, b2i = e2
    return (a2r * a1r - a2i * a1i, a2r * a1i + a2i * a1r,
            a2r * b1r - a2i * b1i + b2r, a2r * b1i + a2i * b1r + b2i)


def s5_mixer(h, w_in, b_re, b_im, c_re, c_im, d_skip, log_dt, a_re, a_im, w_out):
    b, s, _ = h.shape
    f32 = jnp.float32
    u = (h @ w_in).astype(f32).reshape(b, s, S5_GROUPS, S5_GROUP)
    ar, ai = a_re.astype(f32), a_im.astype(f32)
    dt = jnp.exp(log_dt.astype(f32))[:, None]
    mag = jnp.exp(ar * dt)
    abar_re, abar_im = mag * jnp.cos(ai * dt), mag * jnp.sin(ai * dt)
    den = ar * ar + ai * ai
    zoh_re = ((abar_re - 1.0) * ar + abar_im * ai) / den
    zoh_im = (abar_im * ar - (abar_re - 1.0) * ai) / den
    br, bi = b_re.astype(f32), b_im.astype(f32)
    bbar_re = zoh_re[..., None] * br - zoh_im[..., None] * bi
    bbar_im = zoh_re[..., None] * bi + zoh_im[..., None] * br
    cr, ci = c_re.astype(f32), c_im.astype(f32)

    def step(carry, u_c):
        s_re, s_im = carry
        bu_re = jnp.einsum('blgc,gpc->blgp', u_c, bbar_re)
        bu_im = jnp.einsum('blgc,gpc->blgp', u_c, bbar_im)
        bu_re = bu_re.at[:, 0].add(abar_re * s_re - abar_im * s_im)
        bu_im = bu_im.at[:, 0].add(abar_re * s_im + abar_im * s_re)
        a_b_re = jnp.broadcast_to(abar_re, bu_re.shape)
        a_b_im = jnp.broadcast_to(abar_im, bu_im.shape)
        _, _, st_re, st_im = lax.associative_scan(complex_scan_combine, (a_b_re, a_b_im, bu_re, bu_im), axis=1)
        y = jnp.einsum('blgp,gcp->blgc', st_re, cr) - jnp.einsum('blgp,gcp->blgc', st_im, ci)
        return (st_re[:, -1], st_im[:, -1]), y

    carry0 = (jnp.zeros((b, S5_GROUPS, S5_STATE), f32), jnp.zeros((b, S5_GROUPS, S5_STATE), f32))
    _, ys = lax.scan(step, carry0, to_chunks(u))
    y = from_chunks(ys) + d_skip.astype(f32).reshape(S5_GROUPS, S5_GROUP) * u
    y = jax.nn.gelu(y.reshape(b, s, S5_WIDTH)).astype(h.dtype)
    glu = y @ w_out
    return glu[..., :D_MODEL] * jax.nn.sigmoid(glu[..., D_MODEL:])


def sq_relu_mlp(h, w1, w2):
    return jnp.square(jax.nn.relu(h @ w1)) @ w2


def setup_inputs(seed: int = 0) -> dict:
    key = jax.random.key(seed)
    ks = iter(jax.random.split(key, 40))
    f32 = jnp.float32

    def nrm(shape, scale):
        return jax.random.normal(next(ks), shape, f32) * scale

    def log_uniform_dt(shape):
        return jax.random.uniform(next(ks), shape, f32, minval=math.log(DT_MIN), maxval=math.log(DT_MAX))

    x = nrm((BATCH, SEQ, D_MODEL), 1.0)
    norm_mix_g = 1.0 + nrm((DEPTH, D_MODEL), 0.02)
    norm_mlp_g = 1.0 + nrm((DEPTH, D_MODEL), 0.02)
    ssd_w_in = nrm((N_SSD_LAYERS, D_MODEL, SSD_PROJ), D_MODEL ** -0.5)
    ssd_conv_w = nrm((N_SSD_LAYERS, SSD_CONV, SSD_CONV_DIM), SSD_CONV ** -0.5)
    ssd_conv_b = nrm((N_SSD_LAYERS, SSD_CONV_DIM), 0.02)
    dt0 = jnp.exp(log_uniform_dt((N_SSD_LAYERS, SSD_HEADS)))
    ssd_dt_bias = dt0 + jnp.log(-jnp.expm1(-dt0))
    ssd_a_log = jnp.log(jax.random.uniform(next(ks), (N_SSD_LAYERS, SSD_HEADS), f32, minval=1.0, maxval=16.0))
    ssd_d = 1.0 + nrm((N_SSD_LAYERS, SSD_HEADS), 0.02)
    ssd_norm_g = 1.0 + nrm((N_SSD_LAYERS, SSD_INNER), 0.02)
    ssd_w_out = nrm((N_SSD_LAYERS, SSD_INNER, D_MODEL), SSD_INNER ** -0.5)
    mlstm_w_in = nrm((N_MLSTM_LAYERS, D_MODEL, MLSTM_PROJ), D_MODEL ** -0.5)
    ig_b = nrm((N_MLSTM_LAYERS, MLSTM_HEADS), 0.1)
    fg_b = jnp.linspace(3.0, 6.0, MLSTM_HEADS, dtype=f32)[None] + nrm((N_MLSTM_LAYERS, MLSTM_HEADS), 0.1)
    mlstm_gate_b = jnp.concatenate([ig_b, fg_b], axis=-1)
    mlstm_head_g = 1.0 + nrm((N_MLSTM_LAYERS, MLSTM_V), 0.02)
    mlstm_w_out = nrm((N_MLSTM_LAYERS, MLSTM_V, D_MODEL), MLSTM_V ** -0.5)
    s5_w_in = nrm((N_S5_LAYERS, D_MODEL, S5_WIDTH), D_MODEL ** -0.5)
    s5_b_re = nrm((N_S5_LAYERS, S5_GROUPS, S5_STATE, S5_GROUP), (2 * S5_GROUP) ** -0.5)
    s5_b_im = nrm((N_S5_LAYERS, S5_GROUPS, S5_STATE, S5_GROUP), (2 * S5_GROUP) ** -0.5)
    s5_c_re = nrm((N_S5_LAYERS, S5_GROUPS, S5_GROUP, S5_STATE), 0.5)
    s5_c_im = nrm((N_S5_LAYERS, S5_GROUPS, S5_GROUP, S5_STATE), 0.5)
    s5_d = nrm((N_S5_LAYERS, S5_WIDTH), 1.0)
    s5_log_dt = log_uniform_dt((N_S5_LAYERS, S5_GROUPS))
    s5_a_re = -0.5 + nrm((N_S5_LAYERS, S5_GROUPS, S5_STATE), 0.01)
    s5_a_im = (math.pi * jnp.arange(S5_STATE, dtype=f32))[None, None] + nrm((N_S5_LAYERS, S5_GROUPS, S5_STATE), 0.01)
    s5_w_out = nrm((N_S5_LAYERS, S5_WIDTH, 2 * D_MODEL), S5_WIDTH ** -0.5)
    mlp_w1 = nrm((DEPTH, D_MODEL, D_FF), D_MODEL ** -0.5)
    mlp_w2 = nrm((DEPTH, D_FF, D_MODEL), D_FF ** -0.5)
    final_norm_g = 1.0 + nrm((D_MODEL,), 0.02)
    return {'x': x, 'norm_mix_g': norm_mix_g, 'norm_mlp_g': norm_mlp_g,
            'ssd_w_in': ssd_w_in, 'ssd_conv_w': ssd_conv_w, 'ssd_conv_b': ssd_conv_b,
            'ssd_dt_bias': ssd_dt_bias, 'ssd_a_log': ssd_a_log, 'ssd_d': ssd_d,
            'ssd_norm_g': ssd_norm_g, 'ssd_w_out': ssd_w_out,
            'mlstm_w_in': mlstm_w_in, 'mlstm_gate_b': mlstm_gate_b, 'mlstm_head_g': mlstm_head_g,
            'mlstm_w_out': mlstm_w_out,
            's5_w_in': s5_w_in, 's5_b_re': s5_b_re, 's5_b_im': s5_b_im, 's5_c_re': s5_c_re,
            's5_c_im': s5_c_im, 's5_d': s5_d, 's5_log_dt': s5_log_dt, 's5_a_re': s5_a_re,
            's5_a_im': s5_a_im, 's5_w_out': s5_w_out,
            'mlp_w1': mlp_w1, 'mlp_w2': mlp_w2, 'final_norm_g': final_norm_g}


def reference(x, norm_mix_g, norm_mlp_g,
              ssd_w_in, ssd_conv_w, ssd_conv_b, ssd_dt_bias, ssd_a_log, ssd_d, ssd_norm_g, ssd_w_out,
              mlstm_w_in, mlstm_gate_b, mlstm_head_g, mlstm_w_out,
              s5_w_in, s5_b_re, s5_b_im, s5_c_re, s5_c_im, s5_d, s5_log_dt, s5_a_re, s5_a_im, s5_w_out,
              mlp_w1, mlp_w2, final_norm_g):
    h = x
    for layer in range(DEPTH):
        kind, idx = layer % N_MIXERS, layer // N_MIXERS
        hn = rmsnorm(h, norm_mix_g[layer]).astype(h.dtype)
        if kind == 0:
            mix = ssd_mixer(hn, ssd_w_in[idx], ssd_conv_w[idx], ssd_conv_b[idx], ssd_dt_bias[idx],
                            ssd_a_log[idx], ssd_d[idx], ssd_norm_g[idx], ssd_w_out[idx])
        elif kind == 1:
            mix = mlstm_mixer(hn, mlstm_w_in[idx], mlstm_gate_b[idx], mlstm_head_g[idx], mlstm_w_out[idx])
        else:
            mix = s5_mixer(hn, s5_w_in[idx], s5_b_re[idx], s5_b_im[idx], s5_c_re[idx], s5_c_im[idx],
                           s5_d[idx], s5_log_dt[idx], s5_a_re[idx], s5_a_im[idx], s5_w_out[idx])
        h = h + mix.astype(h.dtype)
        hn = rmsnorm(h, norm_mlp_g[layer]).astype(h.dtype)
        h = h + sq_relu_mlp(hn, mlp_w1[layer], mlp_w2[layer]).astype(h.dtype)
    return rmsnorm(h, final_norm_g).astype(x.dtype)
```

```python
import numpy as np
import ml_dtypes
from contextlib import ExitStack
import concourse.bass as bass
import concourse.mybir as mybir
from concourse.bass_utils import run_bass_kernel_spmd

F32 = mybir.dt.float32
BF16 = mybir.dt.bfloat16
I32 = mybir.dt.int32
AF = mybir.ActivationFunctionType
ALU = mybir.AluOpType
AX = mybir.AxisListType

D = 2048
DFF = 8192
EPS = 1e-5
NDS = 40
ENGS = ("pe", "act", "dve", "pool", "sp")


class KB:
    def __init__(self, nc, es, block):
        self.nc = nc
        self.es = es
        self.block = block
        self.cnt = {e: 0 for e in ENGS}
        self.esem = {e: es.enter_context(nc.semaphore("pg_" + e)) for e in ENGS}
        self.dsem = [es.enter_context(nc.semaphore(f"dq{i}")) for i in range(NDS)]
        self.dval = [0] * NDS
        self.dnext = 0
        self.known = {e: {} for e in ENGS}
        self.res = {}
        eobj = {"pe": nc.tensor, "act": nc.scalar, "dve": nc.vector, "pool": nc.gpsimd, "sp": nc.sync}
        self.sect = {k: (lambda body, e=e: body(e)) for k, e in eobj.items()}

    def _sem(self, key):
        return self.esem[key[1]] if key[0] == "e" else self.dsem[key[1]]

    def _deps(self, eng, reads, writes, is_dma):
        deps = {}

        def need(ev, raw):
            if ev is None:
                return
            k, v = ev
            if (not is_dma) and k == ("e", eng) and not raw:
                return
            if v > deps.get(k, 0):
                deps[k] = v

        for r in reads:
            st = self.res.get(r)
            if st is not None:
                need(st[0], True)
        for w in writes:
            st = self.res.get(w)
            if st is not None:
                need(st[0], False)
                for k, v in st[1].items():
                    need((k, v), False)
        return deps

    def _commit(self, ev, reads, writes):
        for r in reads:
            st = self.res.get(r)
            if st is None:
                st = [None, {}]
                self.res[r] = st
            if ev[1] > st[1].get(ev[0], 0):
                st[1][ev[0]] = ev[1]
        for w in writes:
            self.res[w] = [ev, {}]

    def _emit(self, eng, deps, fn, incsem, incval):
        kn = self.known[eng]
        waits = []
        for k, v in deps.items():
            if kn.get(k, 0) >= v:
                continue
            kn[k] = v
            waits.append((self._sem(k), v))

        def body(e):
            for s, v in waits:
                e.wait_ge(s, v)
            fn(e).then_inc(incsem, incval)

        self.sect[eng](body)

    def op(self, eng, fn, reads=(), writes=()):
        deps = self._deps(eng, reads, writes, False)
        self.cnt[eng] += 1
        ev = (("e", eng), self.cnt[eng])
        self._emit(eng, deps, fn, self.esem[eng], 1)
        self._commit(ev, reads, writes)

    def dma(self, eng, out, in_, reads=(), writes=(), **kw):
        deps = self._deps(eng, reads, writes, True)
        i = self.dnext
        self.dnext = (i + 1) % NDS
        if self.dval[i] > 0:
            k = ("d", i)
            deps[k] = max(deps.get(k, 0), self.dval[i])
        self.dval[i] += 16
        ev = (("d", i), self.dval[i])
        self._emit(eng, deps, lambda e: e.dma_start(out=out, in_=in_, **kw), self.dsem[i], 16)
        self._commit(ev, reads, writes)

    def barrier(self):
        allev = {}
        for e in ENGS:
            if self.cnt[e] > 0:
                allev[("e", e)] = self.cnt[e]
        for i in range(NDS):
            if self.dval[i] > 0:
                allev[("d", i)] = self.dval[i]
        for eng in ENGS:
            kn = self.known[eng]
            waits = []
            for k, v in allev.items():
                if k == ("e", eng) or kn.get(k, 0) >= v:
                    continue
                kn[k] = v
                waits.append((self._sem(k), v))
            if waits:
                def body(e, waits=waits):
                    for s, v in waits:
                        e.wait_ge(s, v)
                self.sect[eng](body)
        self.res = {}


def _consts_np():
    c = {}
    c["ident_bf"] = np.eye(128, dtype=np.float32).astype(ml_dtypes.bfloat16)
    c["ident_f"] = np.eye(128, dtype=np.float32)
    i = np.arange(128)
    c["tri_le"] = (i[:, None] <= i[None, :]).astype(np.float32)
    c["tri_gt"] = (i[:, None] > i[None, :]).astype(np.float32)
    c["ones_f"] = np.ones((128, 128), np.float32)
    c["sel_last"] = np.zeros((128, 128), np.float32)
    c["sel_last"][127, :] = 1.0
    c["gmask"] = (np.arange(128)[:, None] // 16 == np.arange(8)[None, :]).astype(np.float32)
    sw = np.zeros((128, 128), np.float32)
    for p in range(64):
        sw[64 + p, p] = -1.0
        sw[p, 64 + p] = 1.0
    c["swap"] = sw
    c["negmask"] = np.where(i[None, :] > i[:, None], np.float32(-1.0e30), np.float32(0.0)).astype(np.float32)
    return c


class Ctx:
    pass


_UNIQ = [0]


def _uniq(name):
    _UNIQ[0] += 1
    return f"{name}_{_UNIQ[0]}"


def rmsnorm_to_xT(kb, cx, tag, hsrc_ap, gb, xT, tb, hbuf, hk=None):
    nc = kb.nc
    hkey = ("hbuf", tag, tb if hk is None else hk)
    kb.dma("sp", hbuf[:, :], hsrc_ap, writes=[hkey])
    ss = cx.ss
    hn = cx.hn2[tb % 2]
    hnk = ("hn", tb % 2)
    kb.op("act", lambda e: e.activation(out=cx.junk[:, :], in_=hbuf[:, :], func=AF.Square,
                                        accum_out=ss[:, tb:tb + 1]),
          reads=[hkey], writes=["junk", ("ss", tb)])
    kb.op("act", lambda e: e.activation(out=ss[:, tb:tb + 1], in_=ss[:, tb:tb + 1], func=AF.Sqrt,
                                        scale=1.0 / D, bias=cx.epsc[:, 0:1]),
          reads=[("ss", tb)], writes=[("ss", tb)])
    kb.op("dve", lambda e: e.reciprocal(ss[:, tb:tb + 1], ss[:, tb:tb + 1]),
          reads=[("ss", tb)], writes=[("ss", tb)])
    kb.op("dve", lambda e: e.scalar_tensor_tensor(out=hn[:, :], in0=hbuf[:, :], scalar=ss[:, tb:tb + 1],
                                                  in1=gb[:, :], op0=ALU.mult, op1=ALU.mult),
          reads=[hkey, ("ss", tb), "gb"], writes=[hnk])

    def stage2():
        for half in range(2):
            pt = cx.ptr[half]
            for j in range(8):
                kt = half * 8 + j
                kb.op("pe", lambda e, kt=kt, j=j, pt=pt: e.transpose(out=pt[:, j * 128:(j + 1) * 128],
                                                                     in_=hn[:, kt * 128:(kt + 1) * 128],
                                                                     identity=cx.ident_bf[:, :]),
                      reads=[hnk, "ident"], writes=[("ptr", half)])
            if half == 0:
                kb.op("act", lambda e, pt=pt: e.copy(
                    out=xT[:, 0:8, tb * 128:(tb + 1) * 128],
                    in_=pt[:, :].rearrange("p (k t) -> p k t", k=8)),
                    reads=[("ptr", half)], writes=[("xT", tb)])
            else:
                kb.op("dve", lambda e, pt=pt: e.tensor_copy(
                    xT[:, 8:16, tb * 128:(tb + 1) * 128],
                    pt[:, :].rearrange("p (k t) -> p k t", k=8)),
                    reads=[("ptr", half)], writes=[("xT", tb)])

    prev = cx.norm_pending
    cx.norm_pending = stage2
    if prev is not None:
        prev()


def rmsnorm_flush(cx):
    if cx.norm_pending is not None:
        cx.norm_pending()
        cx.norm_pending = None


class WStream:
    def __init__(self, kb, bufs):
        self.kb = kb
        self.bufs = bufs
        self.i = 0

    def load(self, src_ap, kt, ncols):
        i = self.i % len(self.bufs)
        self.i += 1
        wv = self.bufs[i][:, 0:kt * ncols].rearrange("p (k n) -> p k n", k=kt)
        key = ("wb", i)
        self.kb.dma("pool", wv, src_ap, writes=[key])
        return wv, key


def tile_size(T, SEQ):
    return 1024 if (T % 1024 == 0 and SEQ % 1024 == 0) else 512


def gemm_fm(kb, cx, ws, wv_all, col0, nblk, xT, TT, pbanks, epi):
    NTH = TT // 512
    cnt = 0
    for cb in range(nblk):
        wv, wkey = ws.load(wv_all[:, :, col0 + cb * 512:col0 + (cb + 1) * 512], 16, 512)
        for mi in range(4):
            for th in range(NTH):
                pi = pbanks[cnt % len(pbanks)]
                cnt += 1
                ps = cx.psum[pi]
                pk = ("ps", pi)
                for kt in range(16):
                    kb.op("pe", lambda e, kt=kt, mi=mi, ps=ps, wv=wv, th=th: e.matmul(
                        ps[:, :], lhsT=wv[:, kt, mi * 128:(mi + 1) * 128], rhs=xT[:, kt, th * 512:(th + 1) * 512],
                        start=(kt == 0), stop=(kt == 15)),
                        reads=[wkey] + [("xT", th * 4 + i) for i in range(4)], writes=[pk])
                epi(cb * 4 + mi, th, ps, pk)


def gemm_tm(kb, cx, ws, wv_all, col0, nblk, ncols, xT, TT, pbanks, epi):
    NTB = TT // 128
    cnt = 0
    for cb in range(nblk):
        wv, wkey = ws.load(wv_all[:, :, col0 + cb * ncols:col0 + (cb + 1) * ncols], 16, ncols)
        for tb in range(NTB):
            pi = pbanks[cnt % len(pbanks)]
            cnt += 1
            ps = cx.psum[pi]
            pk = ("ps", pi)
            for kt in range(16):
                kb.op("pe", lambda e, kt=kt, tb=tb, ps=ps, wv=wv: e.matmul(
                    ps[:, 0:ncols], lhsT=xT[:, kt, tb * 128:(tb + 1) * 128], rhs=wv[:, kt, :],
                    start=(kt == 0), stop=(kt == 15)), reads=[wkey, ("xT", tb)], writes=[pk])
            epi(cb, tb, ps, pk)


def mlp_phase(kb, cx, layer, h_in, h_out, T):
    nc = kb.nc
    TT = 1024 if T % 1024 == 0 else 512
    NTB = TT // 128
    NTH = TT // 512
    with ExitStack() as es:
        sb = lambda name, shape, dt: es.enter_context(nc.sbuf_tensor(_uniq(name), shape, dt))
        hb = [sb(f"m_hb{i}", [128, D], F32) for i in range(NTB)]
        xT = sb("m_xT", [128, 16, TT], BF16)
        a1T = sb("m_a1T", [128, 16, TT], BF16)
        wb = [sb(f"m_wb{i}", [128, 16 * 512], BF16) for i in range(2)]
        gb = sb("m_gb", [128, D], F32)
        rl = [sb(f"m_rl{i}", [128, 512], F32) for i in range(2)]
        kb.dma("sp", gb[:, :], cx.norm_mlp_g[layer:layer + 1, :].broadcast_to([128, D]), writes=["gb"])
        w1v = cx.mlp_w1[layer].rearrange("(kt p) n -> p kt n", p=128)
        w2v = cx.mlp_w2[layer].rearrange("(kt p) n -> p kt n", p=128)
        wi = 0
        ri = 0
        p1 = 0
        p2 = 0
        for tt in range(T // TT):
            for tb in range(NTB):
                t0 = tt * TT + tb * 128
                rmsnorm_to_xT(kb, cx, "m", h_in[t0:t0 + 128, :], gb, xT, tb, hb[tb])
            rmsnorm_flush(cx)
            for q in range(4):
                for cbl in range(4):
                    cb = q * 4 + cbl
                    w = wb[wi % 2]; wkey = ("wb", wi % 2); wi += 1
                    wv = w[:, :].rearrange("p (k n) -> p k n", k=16)
                    kb.dma("pool", wv, w1v[:, :, cb * 512:(cb + 1) * 512], writes=[wkey])
                    for mi in range(4):
                        ml = cbl * 4 + mi
                        for th in range(NTH):
                            pi = p1 % 4; p1 += 1
                            ps = cx.psum[pi]; pk = ("ps", pi)
                            for kt in range(16):
                                kb.op("pe", lambda e, kt=kt, mi=mi, ps=ps, wv=wv, th=th: e.matmul(
                                    ps[:, :], lhsT=wv[:, kt, mi * 128:(mi + 1) * 128],
                                    rhs=xT[:, kt, th * 512:(th + 1) * 512], start=(kt == 0), stop=(kt == 15)),
                                    reads=[wkey] + [("xT", th * 4 + i) for i in range(4)], writes=[pk])
                            r = rl[ri % 2]; rk = ("rl", ri % 2); ri += 1
                            kb.op("act", lambda e, ps=ps, r=r: e.activation(out=r[:, :], in_=ps[:, :], func=AF.Relu),
                                  reads=[pk], writes=[rk])
                            kb.op("dve", lambda e, r=r, ml=ml, th=th: e.tensor_tensor(
                                out=a1T[:, ml, th * 512:(th + 1) * 512], in0=r[:, :], in1=r[:, :], op=ALU.mult),
                                reads=[rk], writes=[("a1T", ml, th)])
                for cb in range(4):
                    w = wb[wi % 2]; wkey = ("wb", wi % 2); wi += 1
                    wv = w[:, :].rearrange("p (k n) -> p k n", k=16)
                    kb.dma("pool", wv, w2v[:, q * 16:(q + 1) * 16, cb * 512:(cb + 1) * 512], writes=[wkey])
                    for tb in range(NTB):
                        pi = 4 + p2 % 2; p2 += 1
                        ps = cx.psum[pi]; pk = ("ps", pi)
                        for kt in range(16):
                            kb.op("pe", lambda e, kt=kt, tb=tb, ps=ps, wv=wv: e.matmul(
                                ps[:, :], lhsT=a1T[:, kt, tb * 128:(tb + 1) * 128], rhs=wv[:, kt, :],
                                start=(kt == 0), stop=(kt == 15)),
                                reads=[wkey, ("a1T", kt, tb // 4)], writes=[pk])
                        kb.op("dve", lambda e, tb=tb, cb=cb, ps=ps: e.tensor_tensor(
                            out=hb[tb][:, cb * 512:(cb + 1) * 512], in0=ps[:, :],
                            in1=hb[tb][:, cb * 512:(cb + 1) * 512], op=ALU.add),
                            reads=[pk, ("hbuf", "m", tb)], writes=[("hbuf", "m", tb)])
            for tb in range(NTB):
                t0 = tt * TT + tb * 128
                kb.dma("sp", h_out[t0:t0 + 128, :], hb[tb][:, :], reads=[("hbuf", "m", tb)],
                       writes=[("h", layer, "mlp", t0)])
        kb.barrier()


def final_norm_phase(kb, cx, h_in, out, T):
    nc = kb.nc
    with ExitStack() as es:
        sb = lambda name, shape, dt: es.enter_context(nc.sbuf_tensor(_uniq(name), shape, dt))
        hb = [sb(f"f_hb{i}", [128, D], F32) for i in range(2)]
        ob = [sb(f"f_ob{i}", [128, D], F32) for i in range(2)]
        gb = sb("f_gb", [128, D], F32)
        kb.dma("sp", gb[:, :], cx.final_norm_g[0:1, :].broadcast_to([128, D]), writes=["gb"])
        ss = cx.ss
        for i in range(T // 128):
            b = i % 2
            t0 = i * 128
            kb.dma("sp", hb[b][:, :], h_in[t0:t0 + 128, :], writes=[("fhb", b)])
            kb.op("act", lambda e, b=b: e.activation(out=cx.junk[:, :], in_=hb[b][:, :], func=AF.Square,
                                                     accum_out=ss[:, b:b + 1]),
                  reads=[("fhb", b)], writes=["junk", ("ss", b)])
            kb.op("act", lambda e, b=b: e.activation(out=ss[:, b:b + 1], in_=ss[:, b:b + 1], func=AF.Sqrt,
                                                     scale=1.0 / D, bias=cx.epsc[:, 0:1]),
                  reads=[("ss", b)], writes=[("ss", b)])
            kb.op("dve", lambda e, b=b: e.reciprocal(ss[:, b:b + 1], ss[:, b:b + 1]),
                  reads=[("ss", b)], writes=[("ss", b)])
            kb.op("dve", lambda e, b=b: e.scalar_tensor_tensor(out=ob[b][:, :], in0=hb[b][:, :],
                                                               scalar=ss[:, b:b + 1], in1=gb[:, :],
                                                               op0=ALU.mult, op1=ALU.mult),
                  reads=[("fhb", b), ("ss", b), "gb"], writes=[("fob", b)])
            kb.dma("sp", out[t0:t0 + 128, :], ob[b][:, :], reads=[("fob", b)], writes=[("out", i)])
        kb.barrier()


SSD_INNER = 4096
SSD_CONV_DIM = 6144
SSD_PROJ = 10304


def ssd_inproj_phase(kb, cx, layer, li, h_in, T, SEQ):
    nc = kb.nc
    TT = tile_size(T, SEQ)
    NTB = TT // 128
    NTH = TT // 512
    with ExitStack() as es:
        sb = lambda name, shape, dt: es.enter_context(nc.sbuf_tensor(_uniq(name), shape, dt))
        hb = [sb(f"s1_hb{i}", [128, D], F32) for i in range(2)]
        xT = sb("s1_xT", [128, 16, TT], BF16)
        ws = WStream(kb, [sb(f"s1_wb{i}", [128, 16 * 512], BF16) for i in range(2)])
        gb = sb("s1_gb", [128, D], F32)
        cbuf = [sb(f"s1_cb{i}", [128, TT + 4], F32) for i in range(2)]
        acc = [sb(f"s1_acc{i}", [128, TT], F32) for i in range(2)]
        rbf = [sb(f"s1_r{i}", [128, TT], BF16) for i in range(3)]
        carry = sb("s1_carry", [128, 48, 4], F32)
        cw = sb("s1_cw", [128, 48, 4], F32)
        cbias = sb("s1_cbias", [128, 48], F32)
        zst = [sb(f"s1_zst{i}", [128, 512], BF16) for i in range(2)]
        xstg = [sb(f"s1_xstg{i}", [128, NTB, 512], BF16) for i in range(2)]
        dtb = sb("s1_dtb", [128, 64], F32)
        arow = sb("s1_arow", [128, 64], F32)
        dts = [sb(f"s1_dts{i}", [128, 64], F32) for i in range(2)]
        lgs = [sb(f"s1_lgs{i}", [128, 64], F32) for i in range(2)]
        kb.dma("sp", gb[:, :], cx.norm_mix_g[layer:layer + 1, :].broadcast_to([128, D]), writes=["gb"])
        kb.dma("sp", dtb[:, :], cx.ssd_dt_bias[li:li + 1, :].broadcast_to([128, 64]), writes=["dtb"])
        kb.dma("sp", arow[:, :], cx.ssd_a_log[li:li + 1, :].broadcast_to([128, 64]), writes=["arow"])
        kb.op("act", lambda e: e.activation(out=arow[:, :], in_=arow[:, :], func=AF.Exp), reads=["arow"],
              writes=["arow"])
        kb.op("dve", lambda e: e.tensor_scalar(arow[:, :], arow[:, :], -1.0, None, ALU.mult), reads=["arow"],
              writes=["arow"])
        for k in range(4):
            kb.dma("sp", cw[:, :, k], cx.ssd_conv_w[li, k, :].rearrange("(f p) -> p f", p=128),
                   writes=["cw"], allow_slow_non_contiguous=True)
        kb.dma("sp", cbias[:, :], cx.ssd_conv_b[li, :].rearrange("(f p) -> p f", p=128), writes=["cbias"],
               allow_slow_non_contiguous=True)
        wv_all = cx.ssd_w_in[li].rearrange("(kt p) n -> p kt n", p=128)
        zc = [0]
        for tt in range(T // TT):
            tbase = tt * TT
            seq_start = tbase % SEQ == 0
            for tb in range(NTB):
                t0 = tbase + tb * 128
                rmsnorm_to_xT(kb, cx, "s1", h_in[t0:t0 + 128, :], gb, xT, tb, hb[tb % 2], hk=tb % 2)
            rmsnorm_flush(cx)

            def epi_z(cb, tb, ps, pk):
                zi = zc[0] % 2
                zc[0] += 1
                zb = zst[zi]
                kb.op("act", lambda e: e.activation(out=zb[:, :], in_=ps[:, :], func=AF.Silu),
                      reads=[pk], writes=[("zst", zi)])
                t0 = tbase + tb * 128
                kb.dma("sp", cx.s_zs[t0:t0 + 128, cb * 512:(cb + 1) * 512], zb[:, :], reads=[("zst", zi)])

            gemm_tm(kb, cx, ws, wv_all, 0, 8, 512, xT, TT, [0, 1], epi_z)

            pend = []

            def epi_x(ft, th, ps, pk):
                c = cbuf[ft % 2]; ck = ("cbuf", ft % 2)
                a = acc[ft % 2]; ak = ("acc", ft % 2)
                if th == 0:
                    while pend and pend[0][0] <= ft - 2:
                        pend.pop(0)[1]()
                    if seq_start:
                        kb.op("dve", lambda e: e.memset(c[:, 0:4], 0.0), writes=[ck])
                    else:
                        kb.op("dve", lambda e: e.tensor_copy(c[:, 0:4], carry[:, ft, :]),
                              reads=[("carry", ft)], writes=[ck])
                kb.op("act", lambda e: e.copy(out=c[:, 4 + th * 512:4 + (th + 1) * 512], in_=ps[:, :]), reads=[pk],
                      writes=[ck])
                if th < NTH - 1:
                    return
                kb.op("dve", lambda e: e.tensor_copy(carry[:, ft, :], c[:, TT:TT + 4]), reads=[ck],
                      writes=[("carry", ft)])
                kb.op("act", lambda e: e.activation(out=a[:, :], in_=c[:, 1:TT + 1], func=AF.Identity,
                                                    scale=cw[:, ft, 0:1], bias=cbias[:, ft:ft + 1]),
                      reads=[ck, "cw", "cbias"], writes=[ak])
                for k in range(1, 4):
                    kb.op("dve", lambda e, k=k: e.scalar_tensor_tensor(
                        out=a[:, :], in0=c[:, 1 + k:TT + 1 + k], scalar=cw[:, ft, k:k + 1], in1=a[:, :],
                        op0=ALU.mult, op1=ALU.add), reads=[ck, ak, "cw"], writes=[ak])
                r = rbf[ft % 3]; rk = ("rbf", ft % 3)
                kb.op("act", lambda e: e.activation(out=r[:, :], in_=a[:, :], func=AF.Silu), reads=[ak], writes=[rk])
                if ft >= 32:
                    dst = cx.s_BT if ft < 40 else cx.s_CT
                    f0 = (ft - 32) * 128 if ft < 40 else (ft - 40) * 128
                    kb.dma("sp", dst[f0:f0 + 128, tbase:tbase + TT], r[:, :], reads=[rk])
                if ft < 40:
                    def post():
                        hf = ft % 2
                        pt = cx.ptr[hf]
                        sg = (ft // 4) % 2
                        for tb in range(NTB):
                            kb.op("pe", lambda e, tb=tb: e.transpose(
                                out=pt[:, tb * 128:(tb + 1) * 128], in_=r[:, tb * 128:(tb + 1) * 128],
                                identity=cx.ident_bf[:, :]), reads=[rk, "ident"], writes=[("ptr", hf)])
                        dst_v = xstg[sg][:, :, (ft % 4) * 128:(ft % 4 + 1) * 128]
                        src_v = pt[:, 0:NTB * 128].rearrange("p (t f) -> p t f", t=NTB)
                        if ft % 2 == 0:
                            kb.op("act", lambda e: e.copy(out=dst_v, in_=src_v), reads=[("ptr", hf)],
                                  writes=[("xstg", sg)])
                        else:
                            kb.op("dve", lambda e: e.tensor_copy(dst_v, src_v), reads=[("ptr", hf)],
                                  writes=[("xstg", sg)])
                        if ft % 4 == 3:
                            for tb in range(NTB):
                                t0 = tbase + tb * 128
                                if ft < 32:
                                    d_ = cx.s_xs[t0:t0 + 128, (ft // 4) * 512:(ft // 4 + 1) * 512]
                                else:
                                    d_ = cx.s_Btok[t0:t0 + 128, ((ft - 32) // 4) * 512:((ft - 32) // 4 + 1) * 512]
                                kb.dma("sp", d_, xstg[sg][:, tb, :], reads=[("xstg", sg)])
                    pend.append((ft, post))

            gemm_fm(kb, cx, ws, wv_all, 4096, 12, xT, TT, [2, 3, 4, 5], epi_x)
            while pend:
                pend.pop(0)[1]()

            def epi_dt(cb, tb, ps, pk):
                d_ = dts[tb % 2]; dk = ("dts", tb % 2)
                l_ = lgs[tb % 2]; lk = ("lgs", tb % 2)
                kb.op("dve", lambda e: e.tensor_tensor(out=d_[:, :], in0=ps[:, 0:64], in1=dtb[:, :], op=ALU.add),
                      reads=[pk, "dtb"], writes=[dk])
                kb.op("act", lambda e: e.activation(out=d_[:, :], in_=d_[:, :], func=AF.Exp), reads=[dk], writes=[dk])
                kb.op("act", lambda e: e.activation(out=d_[:, :], in_=d_[:, :], func=AF.Ln, bias=cx.onec[:, 0:1],
                                                    scale=1.0), reads=[dk], writes=[dk])
                kb.op("dve", lambda e: e.tensor_tensor(out=l_[:, :], in0=d_[:, :], in1=arow[:, :], op=ALU.mult),
                      reads=[dk, "arow"], writes=[lk])
                t0 = tbase + tb * 128
                kb.dma("sp", cx.s_dt[t0:t0 + 128, :], d_[:, :], reads=[dk])
                kb.dma("sp", cx.s_loga[t0:t0 + 128, :], l_[:, :], reads=[lk])

            gemm_tm(kb, cx, ws, wv_all, 10240, 1, 64, xT, TT, [0, 1], epi_dt)
        kb.barrier()


def ssd_scan_phase(kb, cx, li, T, SEQ):
    nc = kb.nc
    with ExitStack() as es:
        sb = lambda name, shape, dt: es.enter_context(nc.sbuf_tensor(_uniq(name), shape, dt))
        S = sb("s3_S", [128, 4096], F32)
        Sbf = sb("s3_Sbf", [128, 4096], BF16)
        xs = [sb(f"s3_xs{i}", [128, 4096], BF16) for i in range(2)]
        zs = [sb(f"s3_zs{i}", [128, 4096], BF16) for i in range(2)]
        Bt = [sb(f"s3_Bt{i}", [128, 1024], BF16) for i in range(2)]
        BT = [sb(f"s3_BT{i}", [128, 8, 128], BF16) for i in range(2)]
        CT = [sb(f"s3_CT{i}", [128, 8, 128], BF16) for i in range(2)]
        dtt = [sb(f"s3_dt{i}", [128, 64], F32) for i in range(2)]
        lga = [sb(f"s3_lg{i}", [128, 64], F32) for i in range(2)]
        e3 = sb("s3_e3", [128, 192], F32)
        xdt = sb("s3_xdt", [128, 4096], BF16)
        xdt2 = sb("s3_xdt2", [128, 4096], BF16)
        R = [sb(f"s3_R{i}", [128, 1024], F32) for i in range(2)]
        E = [sb(f"s3_E{i}", [128, 1024], BF16) for i in range(2)]
        cbm = [sb(f"s3_cbm{i}", [128, 128], BF16) for i in range(2)]
        M = [sb(f"s3_M{i}", [128, 1024], BF16) for i in range(2)]
        tmp = [sb(f"s3_tmp{i}", [128, 512], F32) for i in range(2)]
        y2 = [sb(f"s3_y{i}", [128, 4096], F32) for i in range(2)]
        yn = sb("s3_yn", [128, 4096], BF16)
        ynT = [sb(f"s3_ynT{i}", [128, 32, 128], BF16) for i in range(2)]
        drow = sb("s3_drow", [128, 64], F32)
        ngb = sb("s3_ngb", [128, 4096], F32)
        tail_pending = [None]
        tle = sb("s3_tle", [128, 128], F32)
        tgt = sb("s3_tgt", [128, 128], F32)
        one = sb("s3_one", [128, 128], F32)
        kb.dma("sp", drow[:, :], cx.ssd_d[li:li + 1, :].broadcast_to([128, 64]), writes=["drow"])
        kb.dma("sp", ngb[:, :], cx.ssd_norm_g[li:li + 1, :].broadcast_to([128, 4096]), writes=["ngb"])
        kb.dma("sp", tle[:, :], cx.c_tri_le[:, :], writes=["tle"])
        kb.dma("sp", tgt[:, :], cx.c_tri_gt[:, :], writes=["tgt"])
        kb.dma("sp", one[:, :], cx.c_ones_f[:, :], writes=["one"])
        BTv = cx.s_BT.rearrange("(g n) t -> n g t", n=128)
        CTv = cx.s_CT.rearrange("(g n) t -> n g t", n=128)
        nch = T // 128
        for ci in range(nch):
            t0 = ci * 128
            b = ci % 2
            if t0 % SEQ == 0:
                kb.op("dve", lambda e: e.memset(S[:, :], 0.0), writes=[("S", g) for g in range(8)])
                kb.op("pool", lambda e: e.memset(Sbf[:, :], 0.0), writes=[("Sbf", g) for g in range(8)])
            kb.dma("sp", xs[b][:, :], cx.s_xs[t0:t0 + 128, :], writes=[("xs", b)])
            kb.dma("sp", zs[b][:, :], cx.s_zs[t0:t0 + 128, :], writes=[("zs", b)])
            kb.dma("sp", Bt[b][:, :], cx.s_Btok[t0:t0 + 128, :], writes=[("Bt", b)])
            kb.dma("sp", BT[b][:, :, :], BTv[:, :, t0:t0 + 128], writes=[("BT", b)])
            kb.dma("sp", CT[b][:, :, :], CTv[:, :, t0:t0 + 128], writes=[("CT", b)])
            kb.dma("sp", dtt[b][:, :], cx.s_dt[t0:t0 + 128, :], writes=[("dt", b)])
            kb.dma("sp", lga[b][:, :], cx.s_loga[t0:t0 + 128, :], writes=[("lg", b)])
            ps0 = cx.psum[0]
            for j, (lt, lk) in enumerate(((tle, "tle"), (tgt, "tgt"), (one, "one"))):
                kb.op("pe", lambda e, j=j, lt=lt: e.matmul(ps0[:, j * 64:(j + 1) * 64], lhsT=lt[:, :],
                                                           rhs=lga[b][:, :], start=True, stop=True),
                      reads=[lk, ("lg", b)], writes=[("ps", 0)])
            kb.op("act", lambda e: e.activation(out=e3[:, :], in_=ps0[:, 0:192], func=AF.Exp),
                  reads=[("ps", 0)], writes=["e3"])
            kb.op("dve", lambda e: e.tensor_tensor(
                out=xdt[:, :].rearrange("p (h q) -> p h q", q=64),
                in0=xs[b][:, :].rearrange("p (h q) -> p h q", q=64),
                in1=dtt[b][:, :].rearrange("p (h o) -> p h o", o=1).broadcast_to([128, 64, 64]), op=ALU.mult),
                reads=[("xs", b), ("dt", b)], writes=["xdt"])
            kb.op("pool", lambda e: e.tensor_tensor(
                out=xdt2[:, :].rearrange("p (h q) -> p h q", q=64),
                in0=xdt[:, :].rearrange("p (h q) -> p h q", q=64),
                in1=e3[:, 64:128].rearrange("p (h o) -> p h o", o=1).broadcast_to([128, 64, 64]), op=ALU.mult),
                reads=["xdt", "e3"], writes=["xdt2"])
            def stage_ar(g):
                gb_ = g % 2
                for j in range(8):
                    kb.op("act", lambda e, g=g, gb_=gb_, j=j: e.activation(
                        out=R[gb_][:, j * 128:(j + 1) * 128], in_=tle[:, :], func=AF.Identity,
                        scale=lga[b][:, g * 8 + j:g * 8 + j + 1]),
                        reads=[("lg", b), "tle"], writes=[("R", gb_)])
            def stage_ape(g):
                gb_ = g % 2
                for hf in range(2):
                    psd = cx.psum[1 + hf]
                    kb.op("pe", lambda e, hf=hf, psd=psd, gb_=gb_: e.matmul(
                        psd[:, :], lhsT=tgt[:, :], rhs=R[gb_][:, hf * 512:(hf + 1) * 512], start=True, stop=True),
                        reads=["tgt", ("R", gb_)], writes=[("ps", 1 + hf)])
                    kb.op("act", lambda e, hf=hf, psd=psd, gb_=gb_: e.activation(
                        out=E[gb_][:, hf * 512:(hf + 1) * 512], in_=psd[:, :], func=AF.Exp),
                        reads=[("ps", 1 + hf)], writes=[("E", gb_)])
                kb.op("pe", lambda e, g=g: e.matmul(ps0[:, 256:384], lhsT=BT[b][:, g, :], rhs=CT[b][:, g, :],
                                                    start=True, stop=True),
                      reads=[("BT", b), ("CT", b)], writes=[("ps", 0)])
            def stage_am(g):
                gb_ = g % 2
                kb.op("dve", lambda e, gb_=gb_: e.tensor_tensor(out=cbm[gb_][:, :], in0=ps0[:, 256:384],
                                                               in1=tle[:, :], op=ALU.mult),
                      reads=[("ps", 0), "tle"], writes=[("cbm", gb_)])
                kb.op("dve", lambda e, gb_=gb_: e.tensor_tensor(
                    out=M[gb_][:, :].rearrange("p (j l) -> p j l", j=8),
                    in0=E[gb_][:, :].rearrange("p (j l) -> p j l", j=8),
                    in1=cbm[gb_][:, :].rearrange("p (o l) -> p o l", o=1).broadcast_to([128, 8, 128]),
                    op=ALU.mult), reads=[("E", gb_), ("cbm", gb_)], writes=[("M", gb_)])
            def stage_b(g):
                gb_ = g % 2
                psy = cx.psum[3]
                for j in range(8):
                    h = g * 8 + j
                    kb.op("pe", lambda e, j=j, h=h, gb_=gb_: e.matmul(
                        psy[:, j * 64:(j + 1) * 64], lhsT=M[gb_][:, j * 128:(j + 1) * 128],
                        rhs=xdt[:, h * 64:(h + 1) * 64], start=True, stop=True),
                        reads=[("M", gb_), "xdt"], writes=[("ps", 3)])
                pso = cx.psum[4]
                kb.op("pe", lambda e, g=g: e.matmul(pso[:, :], lhsT=CT[b][:, g, :],
                                                    rhs=Sbf[:, g * 512:(g + 1) * 512], start=True, stop=True),
                      reads=[("CT", b), ("Sbf", g)], writes=[("ps", 4)])
                tm = tmp[gb_]
                kb.op("dve", lambda e, g=g, tm=tm: e.tensor_tensor(
                    out=tm[:, :].rearrange("p (j q) -> p j q", j=8),
                    in0=pso[:, :].rearrange("p (j q) -> p j q", j=8),
                    in1=e3[:, g * 8:(g + 1) * 8].rearrange("p (j o) -> p j o", o=1).broadcast_to([128, 8, 64]),
                    op=ALU.mult), reads=[("ps", 4), "e3"], writes=[("tmp", gb_)])
                kb.op("dve", lambda e, g=g, tm=tm: e.tensor_tensor(
                    out=y2[b][:, g * 512:(g + 1) * 512], in0=psy[:, :], in1=tm[:, :], op=ALU.add),
                    reads=[("ps", 3), ("tmp", gb_)], writes=[("y", b, g)])
                pss = cx.psum[5]
                kb.op("pe", lambda e, g=g: e.matmul(pss[:, :], lhsT=Bt[b][:, g * 128:(g + 1) * 128],
                                                    rhs=xdt2[:, g * 512:(g + 1) * 512], start=True, stop=True),
                      reads=[("Bt", b), "xdt2"], writes=[("ps", 5)])
                kb.op("pool", lambda e, g=g: e.tensor_tensor(
                    out=S[:, g * 512:(g + 1) * 512].rearrange("p (j q) -> p j q", j=8),
                    in0=S[:, g * 512:(g + 1) * 512].rearrange("p (j q) -> p j q", j=8),
                    in1=e3[:, 128 + g * 8:128 + (g + 1) * 8].rearrange("p (j o) -> p j o", o=1).broadcast_to(
                        [128, 8, 64]), op=ALU.mult), reads=[("S", g), "e3"], writes=[("S", g)])
                kb.op("dve", lambda e, g=g: e.tensor_tensor(
                    out=S[:, g * 512:(g + 1) * 512], in0=pss[:, :], in1=S[:, g * 512:(g + 1) * 512], op=ALU.add),
                    reads=[("ps", 5), ("S", g)], writes=[("S", g)])
                kb.op("pool", lambda e, g=g: e.tensor_copy(Sbf[:, g * 512:(g + 1) * 512],
                                                           S[:, g * 512:(g + 1) * 512]),
                      reads=[("S", g)], writes=[("Sbf", g)])
            stage_ar(0)
            stage_ape(0)
            stage_am(0)
            if tail_pending[0] is not None:
                tail_pending[0]()
                tail_pending[0] = None
            for g in range(8):
                if g + 1 < 8:
                    stage_ar(g + 1)
                stage_b(g)
                if g + 1 < 8:
                    stage_ape(g + 1)
                    stage_am(g + 1)

            def tail(b=b, t0=t0):
                y = y2[b]
                ykeys = [("y", b, g) for g in range(8)]
                kb.op("pool", lambda e: e.tensor_tensor(
                    out=yn[:, :].rearrange("p (h q) -> p h q", q=64),
                    in0=xs[b][:, :].rearrange("p (h q) -> p h q", q=64),
                    in1=drow[:, :].rearrange("p (h o) -> p h o", o=1).broadcast_to([128, 64, 64]), op=ALU.mult),
                    reads=[("xs", b), "drow"], writes=["yn"])
                kb.op("dve", lambda e: e.tensor_tensor(out=y[:, :], in0=y[:, :], in1=yn[:, :], op=ALU.add),
                      reads=ykeys + ["yn"], writes=ykeys)
                kb.op("dve", lambda e: e.tensor_tensor(out=y[:, :], in0=y[:, :], in1=zs[b][:, :], op=ALU.mult),
                      reads=ykeys + [("zs", b)], writes=ykeys)
                ss = cx.ss
                kb.op("act", lambda e: e.activation(out=cx.junk4[:, :], in_=y[:, :], func=AF.Square,
                                                    accum_out=ss[:, 0:1]), reads=ykeys, writes=["junk4", ("ss", 0)])
                kb.op("act", lambda e: e.activation(out=ss[:, 0:1], in_=ss[:, 0:1], func=AF.Sqrt,
                                                    scale=1.0 / 4096, bias=cx.epsc[:, 0:1]),
                      reads=[("ss", 0)], writes=[("ss", 0)])
                kb.op("dve", lambda e: e.reciprocal(ss[:, 0:1], ss[:, 0:1]), reads=[("ss", 0)], writes=[("ss", 0)])
                kb.op("dve", lambda e: e.scalar_tensor_tensor(out=yn[:, :], in0=y[:, :], scalar=ss[:, 0:1],
                                                              in1=ngb[:, :], op0=ALU.mult, op1=ALU.mult),
                      reads=ykeys + [("ss", 0), "ngb"], writes=["yn"])
                yT = ynT[b]
                for q in range(4):
                    pt = cx.ptr[q % 2]
                    for j in range(8):
                        kt = q * 8 + j
                        kb.op("pe", lambda e, kt=kt, j=j, pt=pt: e.transpose(
                            out=pt[:, j * 128:(j + 1) * 128], in_=yn[:, kt * 128:(kt + 1) * 128],
                            identity=cx.ident_bf[:, :]), reads=["yn", "ident"], writes=[("ptr", q % 2)])
                    if q % 2 == 0:
                        kb.op("act", lambda e, q=q, pt=pt: e.copy(
                            out=yT[:, q * 8:(q + 1) * 8, :], in_=pt[:, :].rearrange("p (k t) -> p k t", k=8)),
                            reads=[("ptr", q % 2)], writes=[("ynT", b)])
                    else:
                        kb.op("pool" if False else "dve", lambda e, q=q, pt=pt: e.tensor_copy(
                            yT[:, q * 8:(q + 1) * 8, :], pt[:, :].rearrange("p (k t) -> p k t", k=8)),
                            reads=[("ptr", q % 2)], writes=[("ynT", b)])
                kb.dma("sp", cx.s_ynT.rearrange("(k p) t -> p k t", p=128)[:, :, t0:t0 + 128], yT[:, :, :],
                       reads=[("ynT", b)])

            tail_pending[0] = tail
        if tail_pending[0] is not None:
            tail_pending[0]()
            tail_pending[0] = None
        kb.barrier()


def outproj_phase(kb, cx, w_out_ap, KT, src_T, h_in, h_out, T):
    nc = kb.nc
    TT = 1024 if T % 1024 == 0 else 512
    NTB = TT // 128
    NCOL = 8192 // KT
    with ExitStack() as es:
        sb = lambda name, shape, dt: es.enter_context(nc.sbuf_tensor(_uniq(name), shape, dt))
        hb = [sb(f"o_hb{i}", [128, D], F32) for i in range(NTB)]
        aT = sb("o_aT", [128, KT, TT], BF16)
        wb = [sb(f"o_wb{i}", [128, 8192], BF16) for i in range(2)]
        wv_all = w_out_ap.rearrange("(kt p) n -> p kt n", p=128)
        srcv = src_T.rearrange("(k p) t -> p k t", p=128)
        wi = 0
        pc = 0
        for tt in range(T // TT):
            kb.dma("sp", aT[:, :, :], srcv[:, :, tt * TT:(tt + 1) * TT], writes=["aT"])
            for tb in range(NTB):
                t0 = tt * TT + tb * 128
                kb.dma("sp", hb[tb][:, :], h_in[t0:t0 + 128, :], writes=[("hb", tb)])
            for cb in range(D // NCOL):
                w = wb[wi % 2]; wkey = ("wb", wi % 2); wi += 1
                wv = w[:, :].rearrange("p (k n) -> p k n", k=KT)
                kb.dma("pool", wv, wv_all[:, :, cb * NCOL:(cb + 1) * NCOL], writes=[wkey])
                for tb in range(NTB):
                    pi = pc % 4; pc += 1
                    ps = cx.psum[pi]; pk = ("ps", pi)
                    for kt in range(KT):
                        kb.op("pe", lambda e, kt=kt, tb=tb, ps=ps, wv=wv: e.matmul(
                            ps[:, 0:NCOL], lhsT=aT[:, kt, tb * 128:(tb + 1) * 128], rhs=wv[:, kt, :],
                            start=(kt == 0), stop=(kt == KT - 1)), reads=[wkey, "aT"], writes=[pk])
                    kb.op("dve", lambda e, tb=tb, cb=cb, ps=ps: e.tensor_tensor(
                        out=hb[tb][:, cb * NCOL:(cb + 1) * NCOL], in0=ps[:, 0:NCOL],
                        in1=hb[tb][:, cb * NCOL:(cb + 1) * NCOL], op=ALU.add),
                        reads=[pk, ("hb", tb)], writes=[("hb", tb)])
            for tb in range(NTB):
                t0 = tt * TT + tb * 128
                kb.dma("sp", h_out[t0:t0 + 128, :], hb[tb][:, :], reads=[("hb", tb)])
        kb.barrier()


NEG = -1.0e30


def mlstm_layer(kb, cx, layer, li, h_in, h_out, T, SEQ):
    mlstm_inproj_phase(kb, cx, layer, li, h_in, T)
    mlstm_scan_phase(kb, cx, li, T, SEQ)
    outproj_phase(kb, cx, cx.mlstm_w_out[li], 16, cx.s_ynT[0:2048, :], h_in, h_out, T)


def mlstm_inproj_phase(kb, cx, layer, li, h_in, T):
    nc = kb.nc
    TT = 1024 if T % 1024 == 0 else 512
    NTB = TT // 128
    with ExitStack() as es:
        sb = lambda name, shape, dt: es.enter_context(nc.sbuf_tensor(_uniq(name), shape, dt))
        hb = [sb(f"m1_hb{i}", [128, D], F32) for i in range(2)]
        xT = sb("m1_xT", [128, 16, TT], BF16)
        ws = WStream(kb, [sb(f"m1_wb{i}", [128, 16 * 512], BF16) for i in range(2)])
        gb = sb("m1_gb", [128, D], F32)
        st = [sb(f"m1_st{i}", [128, 512], BF16) for i in range(4)]
        gbias = sb("m1_gbias", [128, 8], F32)
        gt = [sb(f"m1_gt{i}", [128, 8], F32) for i in range(2)]
        kb.dma("sp", gb[:, :], cx.norm_mix_g[layer:layer + 1, :].broadcast_to([128, D]), writes=["gb"])
        kb.dma("sp", gbias[:, :], cx.mlstm_gate_b[li:li + 1, :].broadcast_to([128, 8]), writes=["gbias"])
        wv_all = cx.mlstm_w_in[li].rearrange("(kt p) n -> p kt n", p=128)
        sc_ = [0]
        for tt in range(T // TT):
            tbase = tt * TT
            for tb in range(NTB):
                t0 = tbase + tb * 128
                rmsnorm_to_xT(kb, cx, "m1", h_in[t0:t0 + 128, :], gb, xT, tb, hb[tb % 2], hk=tb % 2)
            rmsnorm_flush(cx)

            def epi_qk(m, th, ps, pk):
                si = sc_[0] % 4
                sc_[0] += 1
                s_ = st[si]; sk = ("st", si)
                sc = 1.0 / 16.0 if m < 8 else 1.0
                kb.op("act", lambda e: e.mul(out=s_[:, :], in_=ps[:, :], mul=sc), reads=[pk], writes=[sk])
                kb.dma("sp", cx.s_qkT[m * 128:(m + 1) * 128, tbase + th * 512:tbase + (th + 1) * 512], s_[:, :],
                       reads=[sk])

            gemm_fm(kb, cx, ws, wv_all, 0, 4, xT, TT, [0, 1], epi_qk)

            def epi_kvo(cb, tb, ps, pk):
                si = sc_[0] % 4
                sc_[0] += 1
                s_ = st[si]; sk = ("st", si)
                fn = AF.Sigmoid if cb >= 6 else AF.Identity
                kb.op("act", lambda e: e.activation(out=s_[:, :], in_=ps[:, :], func=fn), reads=[pk], writes=[sk])
                t0 = tbase + tb * 128
                kb.dma("sp", cx.s_kvo[t0:t0 + 128, cb * 512:(cb + 1) * 512], s_[:, :], reads=[sk])

            gemm_tm(kb, cx, ws, wv_all, 1024, 10, 512, xT, TT, [2, 3], epi_kvo)

            def epi_g(cb, tb, ps, pk):
                g_ = gt[tb % 2]; gk = ("gt", tb % 2)
                kb.op("dve", lambda e: e.tensor_tensor(out=g_[:, :], in0=ps[:, 0:8], in1=gbias[:, :], op=ALU.add),
                      reads=[pk, "gbias"], writes=[gk])
                kb.op("act", lambda e: e.activation(out=g_[:, 4:8], in_=g_[:, 4:8], func=AF.Exp, scale=-1.0),
                      reads=[gk], writes=[gk])
                kb.op("act", lambda e: e.activation(out=g_[:, 4:8], in_=g_[:, 4:8], func=AF.Ln,
                                                    bias=cx.onec[:, 0:1], scale=1.0), reads=[gk], writes=[gk])
                kb.op("dve", lambda e: e.tensor_scalar(g_[:, 4:8], g_[:, 4:8], -1.0, None, ALU.mult),
                      reads=[gk], writes=[gk])
                t0 = tbase + tb * 128
                kb.dma("sp", cx.s_gates[t0:t0 + 128, :], g_[:, :], reads=[gk])

            gemm_tm(kb, cx, ws, wv_all, 6144, 1, 8, xT, TT, [4, 5], epi_g)
        kb.barrier()


def mlstm_scan_phase(kb, cx, li, T, SEQ):
    nc = kb.nc
    with ExitStack() as es:
        sb = lambda name, shape, dt: es.enter_context(nc.sbuf_tensor(_uniq(name), shape, dt))
        C = sb("m2_C", [128, 8, 512], F32)
        Cbf = sb("m2_Cbf", [128, 8, 512], BF16)
        nst = sb("m2_n", [128, 8], F32)
        nbf = sb("m2_nbf", [128, 8], BF16)
        mst = sb("m2_mst", [128, 4], F32)
        qT = [sb(f"m2_qT{i}", [128, 8, 128], BF16) for i in range(2)]
        kT = [sb(f"m2_kT{i}", [128, 8, 128], BF16) for i in range(2)]
        kvo = [sb(f"m2_kvo{i}", [128, 5120], BF16) for i in range(2)]
        gts = [sb(f"m2_g{i}", [128, 8], F32) for i in range(2)]
        sm = sb("m2_sm", [128, 8], F32)
        r1 = sb("m2_r1", [128, 512], F32)
        r2 = sb("m2_r2", [128, 512], F32)
        r3 = sb("m2_r3", [128, 512], F32)
        r4 = sb("m2_r4", [128, 512], F32)
        tle = sb("m2_tle", [128, 128], F32)
        one = sb("m2_one", [128, 128], F32)
        idf = sb("m2_idf", [128, 128], F32)
        sel = sb("m2_sel", [128, 128], F32)
        oneb = sb("m2_oneb", [128, 1], BF16)
        v4 = {n: sb("m2_" + n, [128, 4], F32) for n in
              ("mx", "inter", "mt", "negm", "si", "emt", "den", "qn", "rden", "sir", "mnew", "tv", "wk", "cs", "rs")}
        w = sb("m2_w", [128, 512], F32)
        P = sb("m2_P", [128, 512], BF16)
        PT = sb("m2_PT", [128, 512], BF16)
        tmp = [sb(f"m2_tmp{i}", [128, 512], F32) for i in range(2)]
        hh = sb("m2_hh", [128, 2048], F32)
        hg = sb("m2_hg", [128, 2048], F32)
        hgo = sb("m2_hgo", [128, 2048], F32)
        hmix = sb("m2_hmix", [128, 2048], BF16)
        hmT = [sb(f"m2_hmT{i}", [128, 16, 128], BF16) for i in range(2)]
        kw = sb("m2_kw", [128, 1024], BF16)
        kb.dma("sp", hg[:, :], cx.mlstm_head_g[li:li + 1, :].broadcast_to([128, 2048]), writes=["hg"])
        kb.dma("sp", tle[:, :], cx.c_tri_le[:, :], writes=["tle"])
        kb.dma("sp", one[:, :], cx.c_ones_f[:, :], writes=["one"])
        kb.dma("sp", idf[:, :], cx.c_ident_f[:, :], writes=["idf"])
        kb.dma("sp", sel[:, :], cx.c_sel_last[:, :], writes=["sel"])
        kb.dma("sp", r4[:, :].rearrange("p (h s) -> p h s", h=4),
               cx.c_negmask[:, :].rearrange("p (o s) -> p o s", o=1).broadcast_to([128, 4, 128]), writes=["r4"])
        kb.op("dve", lambda e: e.memset(oneb[:, :], 1.0), writes=["oneb"])
        qTv = cx.s_qkT[0:1024, :].rearrange("(k p) t -> p k t", p=128)
        kTv = cx.s_qkT[1024:2048, :].rearrange("(k p) t -> p k t", p=128)
        ps0 = cx.psum[0]
        V = lambda n: v4[n]
        for ci in range(T // 128):
            t0 = ci * 128
            b = ci % 2
            if t0 % SEQ == 0:
                kb.op("dve", lambda e: e.memset(C[:, :, :], 0.0), writes=[("C", h) for h in range(4)])
                kb.op("pool", lambda e: e.memset(Cbf[:, :, :], 0.0), writes=[("Cbf", h) for h in range(4)])
                kb.op("dve", lambda e: e.memset(nst[:, :], 0.0), writes=["n"])
                kb.op("dve", lambda e: e.memset(nbf[:, :], 0.0), writes=["nbf"])
                kb.op("dve", lambda e: e.memset(mst[:, :], 0.0), writes=["mst"])
            kb.dma("sp", qT[b][:, :, :], qTv[:, :, t0:t0 + 128], writes=[("qT", b)])
            kb.dma("sp", kT[b][:, :, :], kTv[:, :, t0:t0 + 128], writes=[("kT", b)])
            kb.dma("sp", kvo[b][:, :], cx.s_kvo[t0:t0 + 128, :], writes=[("kvo", b)])
            kb.dma("sp", gts[b][:, :], cx.s_gates[t0:t0 + 128, :], writes=[("g", b)])
            g_ = gts[b]
            gk = ("g", b)
            lf = g_[:, 4:8]
            ig = g_[:, 0:4]
            kb.op("pe", lambda e: e.matmul(ps0[:, 0:4], lhsT=tle[:, :], rhs=lf, start=True, stop=True),
                  reads=["tle", gk], writes=[("ps", 0)])
            kb.op("pe", lambda e: e.matmul(ps0[:, 4:8], lhsT=one[:, :], rhs=lf, start=True, stop=True),
                  reads=["one", gk], writes=[("ps", 0)])
            kb.op("act", lambda e: e.copy(out=sm[:, :], in_=ps0[:, 0:8]), reads=[("ps", 0)], writes=["sm"])
            lfb = lf.rearrange("p (h o) -> p h o", o=1).broadcast_to([128, 4, 128])
            igb = ig.rearrange("p (h o) -> p h o", o=1).broadcast_to([128, 4, 128])
            tleb = tle[:, :].rearrange("p (o s) -> p o s", o=1).broadcast_to([128, 4, 128])
            idb = idf[:, :].rearrange("p (o s) -> p o s", o=1).broadcast_to([128, 4, 128])
            r13 = lambda r: r[:, :].rearrange("p (h s) -> p h s", h=4)
            kb.op("dve", lambda e: e.tensor_copy(r13(r1), lfb), reads=[gk], writes=["r1"])
            kb.op("dve", lambda e: e.scalar_tensor_tensor(out=r13(r2), in0=tleb, scalar=-1.0, in1=lfb,
                                                          op0=ALU.mult, op1=ALU.mult),
                  reads=[gk, "tle"], writes=["r2"])
            kb.op("pool", lambda e: e.tensor_tensor(out=r13(r3), in0=igb, in1=idb, op=ALU.mult),
                  reads=[gk, "idf"], writes=["r3"])
            psi = cx.psum[1]
            kb.op("pe", lambda e: e.matmul(psi[:, :], lhsT=tle[:, :], rhs=r1[:, :], start=True, stop=False),
                  reads=["tle", "r1"], writes=[("ps", 1)])
            kb.op("pe", lambda e: e.matmul(psi[:, :], lhsT=one[:, :], rhs=r2[:, :], start=False, stop=False),
                  reads=["one", "r2"], writes=[("ps", 1)])
            kb.op("pe", lambda e: e.matmul(psi[:, :], lhsT=one[:, :], rhs=r3[:, :], start=False, stop=False),
                  reads=["one", "r3"], writes=[("ps", 1)])
            kb.op("pe", lambda e: e.matmul(psi[:, :], lhsT=idf[:, :], rhs=r4[:, :], start=False, stop=True),
                  reads=["idf", "r4"], writes=[("ps", 1)])
            kb.op("dve", lambda e: e.tensor_reduce(out=V("mx")[:, :], in_=psi[:, :].rearrange("p (h s) -> p h s", h=4),
                                                   axis=AX.X, op=ALU.max), reads=[("ps", 1)], writes=["mx"])
            kb.op("dve", lambda e: e.tensor_tensor(out=V("inter")[:, :], in0=sm[:, 0:4], in1=mst[:, :], op=ALU.add),
                  reads=["sm", "mst"], writes=["inter"])
            kb.op("dve", lambda e: e.tensor_tensor(out=V("mt")[:, :], in0=V("mx")[:, :], in1=V("inter")[:, :],
                                                   op=ALU.max), reads=["mx", "inter"], writes=["mt"])
            kb.op("dve", lambda e: e.tensor_scalar(V("negm")[:, :], V("mt")[:, :], -1.0, None, ALU.mult),
                  reads=["mt"], writes=["negm"])
            kb.op("dve", lambda e: e.tensor_tensor(out=V("si")[:, :], in0=V("inter")[:, :], in1=V("mt")[:, :],
                                                   op=ALU.subtract), reads=["inter", "mt"], writes=["si"])
            kb.op("act", lambda e: e.activation(out=V("si")[:, :], in_=V("si")[:, :], func=AF.Exp),
                  reads=["si"], writes=["si"])
            kb.op("act", lambda e: e.activation(out=V("emt")[:, :], in_=V("negm")[:, :], func=AF.Exp),
                  reads=["negm"], writes=["emt"])
            for h in range(4):
                kb.op("act", lambda e, h=h: e.activation(out=w[:, h * 128:(h + 1) * 128],
                                                         in_=psi[:, h * 128:(h + 1) * 128], func=AF.Exp,
                                                         bias=V("negm")[:, h:h + 1], scale=1.0),
                      reads=[("ps", 1), "negm"], writes=["w"])
            psq = cx.psum[2]
            for h in range(4):
                for kk in range(2):
                    kb.op("pe", lambda e, h=h, kk=kk: e.matmul(
                        psq[:, h * 128:(h + 1) * 128], lhsT=qT[b][:, h * 2 + kk, :], rhs=kT[b][:, h * 2 + kk, :],
                        start=(kk == 0), stop=(kk == 1)), reads=[("qT", b), ("kT", b)], writes=[("ps", 2)])
            for h in range(4):
                kb.op("dve", lambda e, h=h: e.scalar_tensor_tensor(
                    out=P[:, h * 128:(h + 1) * 128], in0=psq[:, h * 128:(h + 1) * 128], scalar=1.0,
                    in1=w[:, h * 128:(h + 1) * 128], op0=ALU.mult, op1=ALU.mult, accum_out=V("den")[:, h:h + 1]),
                    reads=[("ps", 2), "w"], writes=["P", "den"])
            for h in range(4):
                for kk in range(2):
                    kb.op("pe", lambda e, h=h, kk=kk: e.matmul(
                        ps0[:, 16 + h:17 + h], lhsT=qT[b][:, h * 2 + kk, :], rhs=nbf[:, h * 2 + kk:h * 2 + kk + 1],
                        start=(kk == 0), stop=(kk == 1)), reads=[("qT", b), "nbf"], writes=[("ps", 0)])
            kb.op("dve", lambda e: e.tensor_tensor(out=V("qn")[:, :], in0=ps0[:, 16:20], in1=V("si")[:, :],
                                                   op=ALU.mult), reads=[("ps", 0), "si"], writes=["qn"])
            kb.op("dve", lambda e: e.tensor_tensor(out=V("den")[:, :], in0=V("den")[:, :], in1=V("qn")[:, :],
                                                   op=ALU.add), reads=["den", "qn"], writes=["den"])
            kb.op("dve", lambda e: e.tensor_scalar(V("qn")[:, :], V("den")[:, :], -1.0, None, ALU.mult),
                  reads=["den"], writes=["qn"])
            kb.op("dve", lambda e: e.tensor_tensor(out=V("den")[:, :], in0=V("den")[:, :], in1=V("qn")[:, :],
                                                   op=ALU.max), reads=["den", "qn"], writes=["den"])
            kb.op("dve", lambda e: e.tensor_tensor(out=V("den")[:, :], in0=V("den")[:, :], in1=V("emt")[:, :],
                                                   op=ALU.max), reads=["den", "emt"], writes=["den"])
            kb.op("dve", lambda e: e.reciprocal(V("rden")[:, :], V("den")[:, :]), reads=["den"], writes=["rden"])
            kb.op("dve", lambda e: e.tensor_tensor(out=V("sir")[:, :], in0=V("si")[:, :], in1=V("rden")[:, :],
                                                   op=ALU.mult), reads=["si", "rden"], writes=["sir"])
            pt = cx.ptr[0]
            for h in range(4):
                kb.op("pe", lambda e, h=h: e.transpose(out=pt[:, h * 128:(h + 1) * 128],
                                                       in_=P[:, h * 128:(h + 1) * 128], identity=cx.ident_bf[:, :]),
                      reads=["P", "ident"], writes=[("ptr", 0)])
            kb.op("act", lambda e: e.copy(out=PT[:, :], in_=pt[:, 0:512]), reads=[("ptr", 0)], writes=["PT"])
            for h in range(4):
                psa = cx.psum[3]
                psb = cx.psum[4]
                kb.op("pe", lambda e, h=h: e.matmul(psa[:, :], lhsT=PT[:, h * 128:(h + 1) * 128],
                                                    rhs=kvo[b][:, 1024 + h * 512:1024 + (h + 1) * 512],
                                                    start=True, stop=True),
                      reads=["PT", ("kvo", b)], writes=[("ps", 3)])
                for kk in range(2):
                    kb.op("pe", lambda e, h=h, kk=kk: e.matmul(psb[:, :], lhsT=qT[b][:, h * 2 + kk, :],
                                                               rhs=Cbf[:, h * 2 + kk, :], start=(kk == 0),
                                                               stop=(kk == 1)),
                          reads=[("qT", b), ("Cbf", h)], writes=[("ps", 4)])
                tm = tmp[h % 2]
                kb.op("act", lambda e, h=h, tm=tm: e.activation(out=tm[:, :], in_=psb[:, :], func=AF.Identity,
                                                                scale=V("sir")[:, h:h + 1]),
                      reads=[("ps", 4), "sir"], writes=[("tmp", h % 2)])
                kb.op("dve", lambda e, h=h, tm=tm: e.scalar_tensor_tensor(
                    out=hh[:, h * 512:(h + 1) * 512], in0=psa[:, :], scalar=V("rden")[:, h:h + 1], in1=tm[:, :],
                    op0=ALU.mult, op1=ALU.add), reads=[("ps", 3), "rden", ("tmp", h % 2)], writes=[("hh", h)])
            hkeys = [("hh", h) for h in range(4)]
            for h in range(4):
                kb.op("act", lambda e, h=h: e.activation(out=cx.junk4[:, 0:512], in_=hh[:, h * 512:(h + 1) * 512],
                                                         func=AF.Square, accum_out=V("rs")[:, h:h + 1]),
                      reads=[("hh", h)], writes=["junk4", "rs"])
            kb.op("act", lambda e: e.activation(out=V("rs")[:, :], in_=V("rs")[:, :], func=AF.Sqrt,
                                                scale=1.0 / 512, bias=cx.epsc[:, 0:1]), reads=["rs"], writes=["rs"])
            kb.op("dve", lambda e: e.reciprocal(V("rs")[:, :], V("rs")[:, :]), reads=["rs"], writes=["rs"])
            kb.op("pool", lambda e: e.tensor_tensor(out=hgo[:, :], in0=hg[:, :], in1=kvo[b][:, 3072:5120],
                                                    op=ALU.mult), reads=["hg", ("kvo", b)], writes=["hgo"])
            for h in range(4):
                kb.op("dve", lambda e, h=h: e.scalar_tensor_tensor(
                    out=hmix[:, h * 512:(h + 1) * 512], in0=hh[:, h * 512:(h + 1) * 512],
                    scalar=V("rs")[:, h:h + 1], in1=hgo[:, h * 512:(h + 1) * 512], op0=ALU.mult, op1=ALU.mult),
                    reads=[("hh", h), "rs", "hgo"], writes=["hmix"])
            hT = hmT[b]
            for q in range(2):
                ptq = cx.ptr[1] if q == 0 else cx.ptr[0]
                pk = ("ptr", 1) if q == 0 else ("ptr", 0)
                for j in range(8):
                    kt = q * 8 + j
                    kb.op("pe", lambda e, kt=kt, j=j, ptq=ptq: e.transpose(
                        out=ptq[:, j * 128:(j + 1) * 128], in_=hmix[:, kt * 128:(kt + 1) * 128],
                        identity=cx.ident_bf[:, :]), reads=["hmix", "ident"], writes=[pk])
                kb.op("act", lambda e, q=q, ptq=ptq, hT=hT: e.copy(
                    out=hT[:, q * 8:(q + 1) * 8, :], in_=ptq[:, :].rearrange("p (k t) -> p k t", k=8)),
                    reads=[pk], writes=[("hmT", b)])
            kb.dma("sp", cx.s_ynT[0:2048, :].rearrange("(k p) t -> p k t", p=128)[:, :, t0:t0 + 128], hT[:, :, :],
                   reads=[("hmT", b)])
            kb.op("pe", lambda e: e.matmul(ps0[:, 24:28], lhsT=sel[:, :], rhs=V("mt")[:, :], start=True, stop=True),
                  reads=["sel", "mt"], writes=[("ps", 0)])
            kb.op("act", lambda e: e.copy(out=V("mnew")[:, :], in_=ps0[:, 24:28]), reads=[("ps", 0)],
                  writes=["mnew"])
            kb.op("dve", lambda e: e.tensor_tensor(out=V("tv")[:, :], in0=sm[:, 4:8], in1=sm[:, 0:4],
                                                   op=ALU.subtract), reads=["sm"], writes=["tv"])
            kb.op("dve", lambda e: e.tensor_tensor(out=V("tv")[:, :], in0=V("tv")[:, :], in1=ig, op=ALU.add),
                  reads=["tv", gk], writes=["tv"])
            kb.op("dve", lambda e: e.tensor_tensor(out=V("tv")[:, :], in0=V("tv")[:, :], in1=V("mnew")[:, :],
                                                   op=ALU.subtract), reads=["tv", "mnew"], writes=["tv"])
            kb.op("act", lambda e: e.activation(out=V("wk")[:, :], in_=V("tv")[:, :], func=AF.Exp),
                  reads=["tv"], writes=["wk"])
            kb.op("dve", lambda e: e.tensor_tensor(out=V("cs")[:, :], in0=sm[:, 4:8], in1=mst[:, :], op=ALU.add),
                  reads=["sm", "mst"], writes=["cs"])
            kb.op("dve", lambda e: e.tensor_tensor(out=V("cs")[:, :], in0=V("cs")[:, :], in1=V("mnew")[:, :],
                                                   op=ALU.subtract), reads=["cs", "mnew"], writes=["cs"])
            kb.op("act", lambda e: e.activation(out=V("cs")[:, :], in_=V("cs")[:, :], func=AF.Exp),
                  reads=["cs"], writes=["cs"])
            kb.op("dve", lambda e: e.tensor_tensor(
                out=kw[:, :].rearrange("p (h d) -> p h d", h=4),
                in0=kvo[b][:, 0:1024].rearrange("p (h d) -> p h d", h=4),
                in1=V("wk")[:, :].rearrange("p (h o) -> p h o", o=1).broadcast_to([128, 4, 256]), op=ALU.mult),
                reads=[("kvo", b), "wk"], writes=["kw"])
            psc = cx.psum[5]
            for h in range(4):
                for kk in range(2):
                    j = h * 2 + kk
                    kb.op("pe", lambda e, h=h, j=j: e.matmul(
                        psc[:, :], lhsT=kw[:, j * 128:(j + 1) * 128],
                        rhs=kvo[b][:, 1024 + h * 512:1024 + (h + 1) * 512], start=True, stop=True),
                        reads=["kw", ("kvo", b)], writes=[("ps", 5)])
                    kb.op("dve", lambda e, h=h, j=j: e.scalar_tensor_tensor(
                        out=C[:, j, :], in0=C[:, j, :], scalar=V("cs")[:, h:h + 1], in1=psc[:, :],
                        op0=ALU.mult, op1=ALU.add), reads=[("C", h), "cs", ("ps", 5)], writes=[("C", h)])
                    kb.op("act", lambda e, j=j: e.copy(out=Cbf[:, j, :], in_=C[:, j, :]), reads=[("C", h)],
                          writes=[("Cbf", h)])
                    kb.op("pe", lambda e, j=j: e.matmul(ps0[:, 32 + j:33 + j], lhsT=kw[:, j * 128:(j + 1) * 128],
                                                        rhs=oneb[:, 0:1], start=True, stop=True),
                          reads=["kw", "oneb"], writes=[("ps", 0)])
            for h in range(4):
                kb.op("dve", lambda e, h=h: e.scalar_tensor_tensor(
                    out=nst[:, h * 2:h * 2 + 2], in0=nst[:, h * 2:h * 2 + 2], scalar=V("cs")[:, h:h + 1],
                    in1=ps0[:, 32 + h * 2:34 + h * 2], op0=ALU.mult, op1=ALU.add),
                    reads=["n", "cs", ("ps", 0)], writes=["n"])
            kb.op("dve", lambda e: e.tensor_copy(nbf[:, :], nst[:, :]), reads=["n"], writes=["nbf"])
            kb.op("dve", lambda e: e.tensor_copy(mst[:, :], V("mnew")[:, :]), reads=["mnew"], writes=["mst"])
        kb.barrier()


TWO_PI_LO = 6.283185


def s5_layer(kb, cx, layer, li, h_in, h_out, T, SEQ):
    s5_inproj_phase(kb, cx, layer, li, h_in, T)
    s5_scan_phase(kb, cx, li, T, SEQ)
    s5_out_phase(kb, cx, li, h_in, h_out, T)


def s5_inproj_phase(kb, cx, layer, li, h_in, T):
    nc = kb.nc
    TT = 1024 if T % 1024 == 0 else 512
    NTB = TT // 128
    with ExitStack() as es:
        sb = lambda name, shape, dt: es.enter_context(nc.sbuf_tensor(_uniq(name), shape, dt))
        hb = [sb(f"p1_hb{i}", [128, D], F32) for i in range(2)]
        xT = sb("p1_xT", [128, 16, TT], BF16)
        ws = WStream(kb, [sb(f"p1_wb{i}", [128, 16 * 512], BF16) for i in range(2)])
        gb = sb("p1_gb", [128, D], F32)
        st = [sb(f"p1_st{i}", [128, 512], BF16) for i in range(4)]
        kb.dma("sp", gb[:, :], cx.norm_mix_g[layer:layer + 1, :].broadcast_to([128, D]), writes=["gb"])
        wv_all = cx.s5_w_in[li].rearrange("(kt p) n -> p kt n", p=128)
        sc_ = [0]
        for tt in range(T // TT):
            tbase = tt * TT
            for tb in range(NTB):
                t0 = tbase + tb * 128
                rmsnorm_to_xT(kb, cx, "p1", h_in[t0:t0 + 128, :], gb, xT, tb, hb[tb % 2], hk=tb % 2)
            rmsnorm_flush(cx)

            def epi_u(m, th, ps, pk):
                si = sc_[0] % 4
                sc_[0] += 1
                s_ = st[si]; sk = ("st", si)
                kb.op("act", lambda e: e.copy(out=s_[:, :], in_=ps[:, :]), reads=[pk], writes=[sk])
                kb.dma("sp", cx.s_qkT[m * 128:(m + 1) * 128, tbase + th * 512:tbase + (th + 1) * 512], s_[:, :],
                       reads=[sk])

            gemm_fm(kb, cx, ws, wv_all, 0, 4, xT, TT, [0, 1, 2, 3], epi_u)
        kb.barrier()


def s5_scan_phase(kb, cx, li, T, SEQ):
    nc = kb.nc
    NT = T // 512
    with ExitStack() as es:
        sb = lambda name, shape, dt: es.enter_context(nc.sbuf_tensor(_uniq(name), shape, dt))
        f64 = lambda n: sb("p2_" + n, [128, 64], F32)
        ar, ai, th, lm, mag, cs_, sn_, m1, den, zr, zi, tA, tB, xx, kf = [f64(n) for n in (
            "ar", "ai", "th", "lm", "mag", "cs", "sn", "m1", "den", "zr", "zi", "tA", "tB", "xx", "kf")]
        ki = sb("p2_ki", [128, 64], I32)
        dtc = sb("p2_dtc", [128, 1], F32)
        dup = sb("p2_dup", [128, 128], F32)
        idf = sb("p2_idf", [128, 128], F32)
        swm = sb("p2_swm", [128, 128], F32)
        gmask = sb("p2_gmask", [128, 8], F32)
        cosT = sb("p2_cosT", [128, 128], F32)
        sinT = sb("p2_sinT", [128, 128], F32)
        magT = sb("p2_magT", [128, 128], F32)
        zrT = sb("p2_zrT", [64, 128], F32)
        ziT = sb("p2_ziT", [64, 128], F32)
        D1a = sb("p2_D1a", [128, 16, 128], F32)
        D2a = sb("p2_D2a", [128, 16, 128], F32)
        LBk1 = [sb(f"p2_LBk1{i}", [128, 8, 128], BF16) for i in range(2)]
        LBk2 = [sb(f"p2_LBk2{i}", [128, 8, 128], BF16) for i in range(2)]
        LC1 = sb("p2_LC1", [128, 128, 16], BF16)
        LC2 = sb("p2_LC2", [128, 128, 16], BF16)
        brn = sb("p2_brn", [64, 128], F32)
        bin_ = sb("p2_bin", [64, 128], F32)
        bbr = sb("p2_bbr", [64, 128], F32)
        bbi = sb("p2_bbi", [64, 128], F32)
        bt1 = sb("p2_bt1", [64, 128], F32)
        bt2 = sb("p2_bt2", [64, 128], F32)
        in1 = sb("p2_in1", [128, 128], F32)
        in2 = sb("p2_in2", [128, 128], F32)
        cin = sb("p2_cin", [128, 64], F32)
        kb.dma("sp", idf[:, :], cx.c_ident_f[:, :], writes=["idf"])
        kb.dma("sp", swm[:, :], cx.c_swap[:, :], writes=["swm"])
        kb.dma("sp", gmask[:, :], cx.c_gmask[:, :], writes=["gmask"])
        kb.dma("sp", ar[:, :], cx.s5_a_re[li], writes=["ar"])
        kb.dma("sp", ai[:, :], cx.s5_a_im[li], writes=["ai"])
        kb.dma("sp", dtc[:, :], cx.s5_log_dt[li].rearrange("(g o) -> g o", o=1), writes=["dtc"])
        dv = lambda fn, r, w: kb.op("dve", fn, reads=r, writes=w)
        ac = lambda fn, r, w: kb.op("act", fn, reads=r, writes=w)
        ac(lambda e: e.activation(out=dtc[:, :], in_=dtc[:, :], func=AF.Exp), ["dtc"], ["dtc"])
        dv(lambda e: e.tensor_scalar(th[:, :], ai[:, :], dtc[:, 0:1], None, ALU.mult), ["ai", "dtc"], ["th"])
        dv(lambda e: e.tensor_scalar(lm[:, :], ar[:, :], dtc[:, 0:1], None, ALU.mult), ["ar", "dtc"], ["lm"])
        ac(lambda e: e.activation(out=mag[:, :], in_=lm[:, :], func=AF.Exp), ["lm"], ["mag"])

        def sincos(dst, dkey, shift):
            c0 = 0.5 + shift / (2.0 * np.pi)
            dv(lambda e: e.tensor_scalar(xx[:, :], th[:, :], float(1.0 / (2.0 * np.pi)), float(c0), ALU.mult, ALU.add),
               ["th"], ["xx"])
            dv(lambda e: e.tensor_copy(ki[:, :], xx[:, :]), ["xx"], ["ki"])
            dv(lambda e: e.tensor_copy(kf[:, :], ki[:, :]), ["ki"], ["kf"])
            dv(lambda e: e.tensor_tensor(out=xx[:, :], in0=xx[:, :], in1=kf[:, :], op=ALU.subtract), ["xx", "kf"],
               ["xx"])
            dv(lambda e: e.tensor_scalar(kf[:, :], xx[:, :], 0.0, None, ALU.is_lt), ["xx"], ["kf"])
            dv(lambda e: e.tensor_tensor(out=xx[:, :], in0=xx[:, :], in1=kf[:, :], op=ALU.add), ["xx", "kf"], ["xx"])
            dv(lambda e: e.tensor_scalar(xx[:, :], xx[:, :], -0.5, TWO_PI_LO, ALU.add, ALU.mult), ["xx"], ["xx"])
            ac(lambda e: e.activation(out=dst[:, :], in_=xx[:, :], func=AF.Sin), ["xx"], [dkey])

        sincos(sn_, "sn", 0.0)
        sincos(cs_, "cs", np.pi / 2.0)
        TT_ = lambda o, a, b, op, r, w: dv(lambda e: e.tensor_tensor(out=o, in0=a, in1=b, op=op), r, w)
        TT_(tA[:, :], mag[:, :], cs_[:, :], ALU.mult, ["mag", "cs"], ["tA"])
        TT_(tB[:, :], mag[:, :], sn_[:, :], ALU.mult, ["mag", "sn"], ["tB"])
        dv(lambda e: e.tensor_scalar(m1[:, :], tA[:, :], -1.0, None, ALU.add), ["tA"], ["m1"])
        TT_(den[:, :], ar[:, :], ar[:, :], ALU.mult, ["ar"], ["den"])
        TT_(xx[:, :], ai[:, :], ai[:, :], ALU.mult, ["ai"], ["xx"])
        TT_(den[:, :], den[:, :], xx[:, :], ALU.add, ["den", "xx"], ["den"])
        dv(lambda e: e.reciprocal(den[:, :], den[:, :]), ["den"], ["den"])
        TT_(zr[:, :], m1[:, :], ar[:, :], ALU.mult, ["m1", "ar"], ["zr"])
        TT_(xx[:, :], tB[:, :], ai[:, :], ALU.mult, ["tB", "ai"], ["xx"])
        TT_(zr[:, :], zr[:, :], xx[:, :], ALU.add, ["zr", "xx"], ["zr"])
        TT_(zr[:, :], zr[:, :], den[:, :], ALU.mult, ["zr", "den"], ["zr"])
        TT_(zi[:, :], tB[:, :], ar[:, :], ALU.mult, ["tB", "ar"], ["zi"])
        TT_(xx[:, :], m1[:, :], ai[:, :], ALU.mult, ["m1", "ai"], ["xx"])
        TT_(zi[:, :], zi[:, :], xx[:, :], ALU.subtract, ["zi", "xx"], ["zi"])
        TT_(zi[:, :], zi[:, :], den[:, :], ALU.mult, ["zi", "den"], ["zi"])
        pst = cx.psum[5]
        for src, skey, dst, dkey in ((cs_, "cs", cosT, "cosT"), (sn_, "sn", sinT, "sinT"), (mag, "mag", magT, "magT")):
            dv(lambda e, src=src: e.tensor_copy(dup[:, 0:64], src[:, :]), [skey], ["dup"])
            dv(lambda e, src=src: e.tensor_copy(dup[:, 64:128], src[:, :]), [skey], ["dup"])
            kb.op("pe", lambda e: e.transpose(out=pst[:, 0:128], in_=dup[:, :], identity=idf[:, :]),
                  reads=["dup", "idf"], writes=[("ps", 5)])
            ac(lambda e, dst=dst: e.copy(out=dst[:, :], in_=pst[:, 0:128]), [("ps", 5)], [dkey])
        for src, skey, dst, dkey in ((zr, "zr", zrT, "zrT"), (zi, "zi", ziT, "ziT")):
            kb.op("pe", lambda e, src=src: e.transpose(out=pst[0:64, 0:128], in_=src[:, :], identity=idf[:, :]),
                  reads=[skey, "idf"], writes=[("ps", 5)])
            ac(lambda e, dst=dst: e.copy(out=dst[:, :], in_=pst[0:64, 0:128]), [("ps", 5)], [dkey])
        b3 = lambda t: t[:, :].rearrange("p (g c) -> p g c", g=8)
        for k in range(16):
            kb.dma("sp", b3(brn), cx.s5_b_re[li, 8 * k:8 * k + 8].rearrange("g p c -> p g c"), writes=["brn"])
            kb.dma("sp", b3(bin_), cx.s5_b_im[li, 8 * k:8 * k + 8].rearrange("g p c -> p g c"), writes=["bin"])
            zrb = zrT[:, 8 * k:8 * k + 8].rearrange("p (g o) -> p g o", o=1).broadcast_to([64, 8, 16])
            zib = ziT[:, 8 * k:8 * k + 8].rearrange("p (g o) -> p g o", o=1).broadcast_to([64, 8, 16])
            TT_(b3(bt1), b3(brn), zrb, ALU.mult, ["brn", "zrT"], ["bt1"])
            TT_(b3(bt2), b3(bin_), zib, ALU.mult, ["bin", "ziT"], ["bt2"])
            TT_(bbr[:, :], bt1[:, :], bt2[:, :], ALU.subtract, ["bt1", "bt2"], ["bbr"])
            TT_(b3(bt1), b3(bin_), zrb, ALU.mult, ["bin", "zrT"], ["bt1"])
            TT_(b3(bt2), b3(brn), zib, ALU.mult, ["brn", "ziT"], ["bt2"])
            TT_(bbi[:, :], bt1[:, :], bt2[:, :], ALU.add, ["bt1", "bt2"], ["bbi"])
            kb.op("pe", lambda e: e.transpose(out=pst[:, 0:64], in_=bbr[:, :], identity=idf[0:64, 0:64]),
                  reads=["bbr", "idf"], writes=[("ps", 5)])
            kb.op("pe", lambda e: e.transpose(out=pst[:, 64:128], in_=bbi[:, :], identity=idf[0:64, 0:64]),
                  reads=["bbi", "idf"], writes=[("ps", 5)])
            ac(lambda e, k=k: e.copy(out=D1a[:, k, :], in_=pst[:, 0:128]), [("ps", 5)], [("D1", k)])
            ac(lambda e, k=k: e.copy(out=D2a[:, k, 0:64], in_=pst[:, 64:128]), [("ps", 5)], [("D2", k)])
            ac(lambda e, k=k: e.mul(out=D2a[:, k, 64:128], in_=pst[:, 0:64], mul=-1.0), [("ps", 5)], [("D2", k)])
            kb.dma("sp", in1[:, 0:64], cx.s5_c_re[li, 8 * k:8 * k + 8].rearrange("g c p -> (g c) p"), writes=["in1"])
            kb.dma("sp", cin[:, :], cx.s5_c_im[li, 8 * k:8 * k + 8].rearrange("g c p -> (g c) p"), writes=["cin"])
            dv(lambda e: e.tensor_scalar(in1[:, 64:128], cin[:, :], -1.0, None, ALU.mult), ["cin"], ["in1"])
            dv(lambda e: e.tensor_scalar(in2[:, 0:64], cin[:, :], -1.0, None, ALU.mult), ["cin"], ["in2"])
            dv(lambda e: e.tensor_scalar(in2[:, 64:128], in1[:, 0:64], -1.0, None, ALU.mult), ["in1"], ["in2"])
            for inx, ik, LC in ((in1, "in1", LC1), (in2, "in2", LC2)):
                kb.op("pe", lambda e, inx=inx: e.transpose(out=pst[:, 256:384], in_=inx[:, :], identity=idf[:, :]),
                      reads=[ik, "idf"], writes=[("ps", 5)])
                ac(lambda e, LC=LC, k=k: e.copy(out=LC[:, 8 * k:8 * k + 8, :],
                                                in_=pst[:, 256:384].rearrange("p (g c) -> p g c", g=8)),
                   [("ps", 5)], ["LC"])
        uT = [sb(f"p2_uT{i}", [128, T], BF16) for i in range(2)]
        TcA = [sb(f"p2_TcA{i}", [128, 8, 512], F32) for i in range(2)]
        TsA = [sb(f"p2_TsA{i}", [128, 8, 512], F32) for i in range(2)]
        ttmp = [sb(f"p2_ttmp{i}", [128, 8, 256], F32) for i in range(2)]
        t1 = [sb(f"p2_t1{i}", [128, 512], F32) for i in range(2)]
        t2 = [sb(f"p2_t2{i}", [128, 512], F32) for i in range(2)]
        vv = [sb(f"p2_v{i}", [128, 512], F32) for i in range(2)]
        rr = [sb(f"p2_r{i}", [128, 512], F32) for i in range(2)]
        X1 = [sb(f"p2_X1{i}", [128, 512], BF16) for i in range(2)]
        X2 = [sb(f"p2_X2{i}", [128, 512], BF16) for i in range(2)]
        stt = sb("p2_st", [128, 2], F32)
        stmp = sb("p2_stmp", [128, 2], F32)
        yst = [sb(f"p2_yst{i}", [16, 512], F32) for i in range(2)]
        def table_steps(k):
            tcv, tsv = TcA[k % 2], TsA[k % 2]
            tck = ("tab", k % 2)
            tA, tB = ttmp[0], ttmp[1]
            steps = []

            def expand():
                gmb = gmask[:, :].rearrange("p (g o) -> p g o", o=1).broadcast_to([128, 8, 128])
                for Da, dk, LB in ((D1a, "D1", LBk1[k % 2]), (D2a, "D2", LBk2[k % 2])):
                    dv(lambda e, Da=Da, LB=LB: e.tensor_tensor(
                        out=LB[:, :, :], in0=Da[:, k:k + 1, :].broadcast_to([128, 8, 128]), in1=gmb, op=ALU.mult),
                       [(dk, k), "gmask"], [("LB", k % 2)])
            steps.append(expand)

            def init():
                ac(lambda e: e.copy(out=tcv[:, :, 0:1], in_=cosT[:, 8 * k:8 * k + 8].rearrange("p (g o) -> p g o", o=1)),
                   ["cosT"], [tck])
                ac(lambda e: e.copy(out=tsv[:, :, 0:1], in_=sinT[:, 8 * k:8 * k + 8].rearrange("p (g o) -> p g o", o=1)),
                   ["sinT"], [tck])
            steps.append(init)
            n = 1
            while n < 512:
                def step(n=n):
                    cn = tcv[:, :, n - 1:n].broadcast_to([128, 8, n])
                    sn = tsv[:, :, n - 1:n].broadcast_to([128, 8, n])
                    A_ = tcv[:, :, 0:n]
                    B_ = tsv[:, :, 0:n]
                    dv(lambda e: e.tensor_tensor(out=tA[:, :, 0:n], in0=A_, in1=cn, op=ALU.mult), [tck], ["ttA"])
                    dv(lambda e: e.tensor_tensor(out=tB[:, :, 0:n], in0=B_, in1=sn, op=ALU.mult), [tck], ["ttB"])
                    dv(lambda e: e.tensor_tensor(out=tcv[:, :, n:2 * n], in0=tA[:, :, 0:n], in1=tB[:, :, 0:n],
                                                 op=ALU.subtract), ["ttA", "ttB", tck], ["tabtmp"])
                    dv(lambda e: e.tensor_tensor(out=tA[:, :, 0:n], in0=B_, in1=cn, op=ALU.mult), [tck, "tabtmp"],
                       ["ttA"])
                    dv(lambda e: e.tensor_tensor(out=tB[:, :, 0:n], in0=A_, in1=sn, op=ALU.mult), [tck, "tabtmp"],
                       ["ttB"])
                    dv(lambda e: e.tensor_tensor(out=tsv[:, :, n:2 * n], in0=tA[:, :, 0:n], in1=tB[:, :, 0:n],
                                                 op=ALU.add), ["ttA", "ttB", "tabtmp"], [tck])
                steps.append(step)
                n *= 2
            return steps

        def make_iter(g, tt, ub, uk, idx):
            k_ = g // 8
            tc, ts = TcA[k_ % 2][:, g % 8, :], TsA[k_ % 2][:, g % 8, :]
            tck = ("tab", k_ % 2)
            sp_ = g % 2
            stk = ("st", sp_)
            b = idx % 2
            pa, pb = cx.psum[b * 2], cx.psum[b * 2 + 1]
            pka, pkb = ("ps", b * 2), ("ps", b * 2 + 1)

            def st_a():
                kb.op("pe", lambda e: e.matmul(pa[:, :], lhsT=LBk1[k_ % 2][:, g % 8, :],
                                               rhs=ub[:, tt * 512:(tt + 1) * 512],
                                               start=True, stop=True), reads=[("LB", k_ % 2), uk], writes=[pka])
                kb.op("pe", lambda e: e.matmul(pb[:, :], lhsT=LBk2[k_ % 2][:, g % 8, :],
                                               rhs=ub[:, tt * 512:(tt + 1) * 512],
                                               start=True, stop=True), reads=[("LB", k_ % 2), uk], writes=[pkb])

            def st_b1():
                dv(lambda e: e.tensor_tensor(out=t1[b][:, :], in0=pa[:, :], in1=tc[:, :], op=ALU.mult),
                   [pka, tck], [("t1", b)])
                dv(lambda e: e.tensor_tensor(out=t2[b][:, :], in0=pb[:, :], in1=ts[:, :], op=ALU.mult),
                   [pkb, tck], [("t2", b)])
                dv(lambda e: e.tensor_tensor(out=vv[b][:, :], in0=t1[b][:, :], in1=t2[b][:, :], op=ALU.add),
                   [("t1", b), ("t2", b)], [("v", b)])

            def st_b2():
                if (tt * 512) % SEQ == 0:
                    dv(lambda e: e.memset(stt[:, sp_:sp_ + 1], 0.0), [], [stk])
                dv(lambda e: e.tensor_tensor_scan(
                    out=rr[b][:, :], data0=magT[:, g:g + 1].broadcast_to([128, 512]), data1=vv[b][:, :],
                    initial=stt[:, sp_:sp_ + 1], op0=ALU.mult, op1=ALU.add), [("v", b), "magT", stk], [("r", b)])
                kb.op("pool", lambda e: e.tensor_tensor(out=X1[b][:, :], in0=rr[b][:, :], in1=tc[:, :], op=ALU.mult),
                      reads=[("r", b), tck], writes=[("X1", b)])
                kb.op("pool", lambda e: e.tensor_tensor(out=X2[b][:, :], in0=rr[b][:, :], in1=ts[:, :], op=ALU.mult),
                      reads=[("r", b), tck], writes=[("X2", b)])
                py = cx.psum[4]
                kb.op("pe", lambda e: e.matmul(py[0:16, :], lhsT=LC1[:, g, :], rhs=X1[b][:, :], start=True,
                                               stop=False), reads=["LC", ("X1", b)], writes=[("ps", 4)])
                kb.op("pe", lambda e: e.matmul(py[0:16, :], lhsT=LC2[:, g, :], rhs=X2[b][:, :], start=False,
                                               stop=True), reads=["LC", ("X2", b)], writes=[("ps", 4)])
                ys = yst[b]
                ac(lambda e: e.copy(out=ys[:, :], in_=py[0:16, :]), [("ps", 4)], [("yst", b)])
                kb.dma("sp", cx.s_y[g * 16:(g + 1) * 16, tt * 512:(tt + 1) * 512], ys[:, :], reads=[("yst", b)])
            def st_b3():
                if tt + 1 < NT and ((tt + 1) * 512) % SEQ != 0:
                    kb.op("pe", lambda e: e.matmul(pst[:, 400 + sp_:401 + sp_], lhsT=swm[:, :],
                                                   rhs=rr[b][:, 511:512], start=True, stop=True),
                          reads=["swm", ("r", b)], writes=[("ps", 5)])
                    dv(lambda e: e.tensor_tensor(out=stmp[:, sp_:sp_ + 1], in0=pst[:, 400 + sp_:401 + sp_],
                                                 in1=ts[:, 511:512], op=ALU.mult), [("ps", 5), tck],
                       [("stmp", sp_)])
                    dv(lambda e: e.scalar_tensor_tensor(
                        out=stt[:, sp_:sp_ + 1], in0=rr[b][:, 511:512], scalar=tc[:, 511:512],
                        in1=stmp[:, sp_:sp_ + 1], op0=ALU.mult, op1=ALU.add),
                        [("r", b), tck, ("stmp", sp_)], [stk])

            return st_a, st_b1, st_b2, st_b3

        iters = []
        for k in range(16):
            ub = uT[k % 2]; uk = ("uT", k % 2)
            nxt = []
            if k + 1 < 16:
                nxt.append(lambda k=k: kb.dma("sp", uT[(k + 1) % 2][:, :],
                                              cx.s_qkT[(k + 1) * 128:(k + 2) * 128, :], writes=[("uT", (k + 1) % 2)]))
                nxt += table_steps(k + 1)
            cnt_k = 0
            for gp in range(4):
                gs = (8 * k + 2 * gp, 8 * k + 2 * gp + 1)
                for tt in range(NT):
                    for g in gs:
                        post = []
                        if cnt_k >= 2 and nxt:
                            post.append(nxt.pop(0))
                        cnt_k += 1
                        iters.append((post, make_iter(g, tt, ub, uk, len(iters))))
            assert not nxt, "not enough iterations per k-tile to hide the next tile's table construction"
        kb.dma("sp", uT[0][:, :], cx.s_qkT[0:128, :], writes=[("uT", 0)])
        for st_ in table_steps(0):
            st_()
        n_it = len(iters)
        for step in range(n_it + 3):
            if 0 <= step - 2 < n_it:
                for p in iters[step - 2][0]:
                    p()
            if step < n_it:
                iters[step][1][0]()
            if 0 <= step - 3 < n_it:
                iters[step - 3][1][3]()
            if 0 <= step - 1 < n_it:
                iters[step - 1][1][1]()
            if 0 <= step - 2 < n_it:
                iters[step - 2][1][2]()
        kb.barrier()


def s5_out_phase(kb, cx, li, h_in, h_out, T):
    nc = kb.nc
    with ExitStack() as es:
        sb = lambda name, shape, dt: es.enter_context(nc.sbuf_tensor(_uniq(name), shape, dt))
        hb = [sb(f"p3_hb{i}", [128, D], F32) for i in range(4)]
        yT = sb("p3_yT", [128, 16, 512], F32)
        uT = sb("p3_uT", [128, 16, 512], BF16)
        gT = sb("p3_gT", [128, 16, 512], BF16)
        wb = [sb(f"p3_wb{i}", [128, 16 * 512], BF16) for i in range(2)]
        dcol = sb("p3_dcol", [128, 16], F32)
        sg = [sb(f"p3_sg{i}", [128, 256], F32) for i in range(2)]
        kb.dma("sp", dcol[:, :], cx.s5_d[li].rearrange("(k p) -> p k", p=128), writes=["dcol"],
               allow_slow_non_contiguous=True)
        wv_all = cx.s5_w_out[li].rearrange("(kt p) n -> p kt n", p=128)
        yv = cx.s_y.rearrange("(k p) t -> p k t", p=128)
        uv = cx.s_qkT.rearrange("(k p) t -> p k t", p=128)
        wi = 0
        for tt in range(T // 512):
            kb.dma("sp", yT[:, :, :], yv[:, :, tt * 512:(tt + 1) * 512], writes=["yT"])
            kb.dma("sp", uT[:, :, :], uv[:, :, tt * 512:(tt + 1) * 512], writes=["uT"])
            for tb in range(4):
                t0 = tt * 512 + tb * 128
                kb.dma("sp", hb[tb][:, :], h_in[t0:t0 + 128, :], writes=[("hb", tb)])
            for k in range(16):
                kb.op("dve", lambda e, k=k: e.scalar_tensor_tensor(
                    out=yT[:, k, :], in0=uT[:, k, :], scalar=dcol[:, k:k + 1], in1=yT[:, k, :],
                    op0=ALU.mult, op1=ALU.add), reads=["uT", "yT", "dcol"], writes=["yT"])
                kb.op("act", lambda e, k=k: e.activation(out=gT[:, k, :], in_=yT[:, k, :], func=AF.Gelu_apprx_tanh),
                      reads=["yT"], writes=[("gT", k)])
            gkeys = [("gT", k) for k in range(16)]
            for cb in range(8):
                w = wb[wi % 2]; wkey = ("wb", wi % 2); wi += 1
                wv = w[:, :].rearrange("p (k n) -> p k n", k=16)
                kb.dma("pool", wv[:, :, 0:256], wv_all[:, :, cb * 256:(cb + 1) * 256], writes=[wkey])
                kb.dma("pool", wv[:, :, 256:512], wv_all[:, :, 2048 + cb * 256:2048 + (cb + 1) * 256], writes=[wkey])
                for tb in range(4):
                    pi = (cb * 4 + tb) % 4
                    ps = cx.psum[pi]; pk = ("ps", pi)
                    for half in range(2):
                        for kt in range(16):
                            kb.op("pe", lambda e, kt=kt, tb=tb, ps=ps, wv=wv, half=half: e.matmul(
                                ps[:, half * 256:(half + 1) * 256], lhsT=gT[:, kt, tb * 128:(tb + 1) * 128],
                                rhs=wv[:, kt, half * 256:(half + 1) * 256], start=(kt == 0), stop=(kt == 15)),
                                reads=[wkey] + gkeys, writes=[pk])
                    s_ = sg[pi % 2]; sk = ("sg", pi % 2)
                    kb.op("act", lambda e, ps=ps, s_=s_: e.activation(out=s_[:, :], in_=ps[:, 256:512],
                                                                      func=AF.Sigmoid), reads=[pk], writes=[sk])
                    kb.op("dve", lambda e, ps=ps, s_=s_: e.tensor_tensor(out=s_[:, :], in0=ps[:, 0:256], in1=s_[:, :],
                                                                        op=ALU.mult), reads=[pk, sk], writes=[sk])
                    kb.op("dve", lambda e, tb=tb, cb=cb, s_=s_: e.tensor_tensor(
                        out=hb[tb][:, cb * 256:(cb + 1) * 256], in0=hb[tb][:, cb * 256:(cb + 1) * 256],
                        in1=s_[:, :], op=ALU.add), reads=[sk, ("hb", tb)], writes=[("hb", tb)])
            for tb in range(4):
                t0 = tt * 512 + tb * 128
                kb.dma("sp", h_out[t0:t0 + 128, :], hb[tb][:, :], reads=[("hb", tb)])
        kb.barrier()


N_CORES = 8
SEQ_FULL = 2048
SEQ_PER_CORE = 2


def kernel(**inputs):
    x = np.asarray(inputs["x"], dtype=np.float32)
    params = {k: np.asarray(v, dtype=np.float32) for k, v in inputs.items() if k != "x"}
    T = SEQ_PER_CORE * SEQ_FULL
    nc = build_program(T, SEQ_FULL)
    in_maps = [make_in_map(x[SEQ_PER_CORE * c:SEQ_PER_CORE * (c + 1)], params) for c in range(N_CORES)]
    res = run_bass_kernel_spmd(nc, in_maps, core_ids=list(range(N_CORES)))
    outs = [np.asarray(r["out"]).reshape(SEQ_PER_CORE, SEQ_FULL, D) for r in res.results]
    return np.concatenate(outs, axis=0).astype(np.float32)


def build_program(T, SEQ, layers=(0, 1, 2, 3), do_mlp=True, do_final=True):
    nc = bass.Bass("TRN2", target_bir_lowering=False)
    cx = Ctx()
    dt = lambda name, shape, dtype=F32, kind="ExternalInput": nc.dram_tensor(name, shape, dtype, kind=kind).ap()
    cx.x = dt("x", [T, D])
    for name, shape in PARAM_SHAPES:
        setattr(cx, name, dt(name, list(shape)))
    for name, arr in _consts_np().items():
        setattr(cx, "c_" + name, dt("c_" + name, list(arr.shape), BF16 if arr.dtype != np.float32 else F32))
    cx.out = dt("out", [T, D], F32, "ExternalOutput")
    cx.hA = dt("hA", [T, D], F32, "Internal")
    cx.hB = dt("hB", [T, D], F32, "Internal")
    cx.s_zs = dt("s_zs", [T, 4096], BF16, "Internal")
    cx.s_xs = dt("s_xs", [T, 4096], BF16, "Internal")
    cx.s_Btok = dt("s_Btok", [T, 1024], BF16, "Internal")
    cx.s_BT = dt("s_BT", [1024, T], BF16, "Internal")
    cx.s_CT = dt("s_CT", [1024, T], BF16, "Internal")
    cx.s_dt = dt("s_dt", [T, 64], F32, "Internal")
    cx.s_loga = dt("s_loga", [T, 64], F32, "Internal")
    cx.s_ynT = dt("s_ynT", [4096, T], BF16, "Internal")
    cx.s_qkT = dt("s_qkT", [2048, T], BF16, "Internal")
    cx.s_kvo = dt("s_kvo", [T, 5120], BF16, "Internal")
    cx.s_gates = dt("s_gates", [T, 8], F32, "Internal")
    cx.s_y = dt("s_y", [2048, T], F32, "Internal")
    with ExitStack() as es:
        block = es.enter_context(nc.Block())
        kb = KB(nc, es, block)
        sb = lambda name, shape, dtp: es.enter_context(nc.sbuf_tensor(name, shape, dtp))
        cx.ident_bf = sb("ident_bf", [128, 128], BF16)
        cx.junk = sb("junk", [128, D], BF16)
        cx.junk4 = sb("junk4", [128, 4096], BF16)
        cx.hn2 = [sb("hn_a", [128, D], BF16), sb("hn_b", [128, D], BF16)]
        cx.norm_pending = None
        cx.ss = sb("ss", [128, 8], F32)
        cx.epsc = sb("epsc", [128, 1], F32)
        cx.onec = sb("onec", [128, 1], F32)
        cx.psum = [es.enter_context(nc.psum_tensor(f"ps{i}", [128, 512], F32)) for i in range(6)]
        cx.ptr = [es.enter_context(nc.psum_tensor(f"ptr{i}", [128, 1024], BF16)) for i in range(2)]
        kb.dma("sp", cx.ident_bf[:, :], cx.c_ident_bf[:, :], writes=["ident"])
        kb.op("dve", lambda e: e.memset(cx.epsc[:, :], EPS), writes=["epsc"])
        kb.op("dve", lambda e: e.memset(cx.onec[:, :], 1.0), writes=["onec"])
        kb.barrier()
        h = cx.x
        for layer in layers:
            kind, li = layer % 3, layer // 3
            if kind == 0:
                ssd_inproj_phase(kb, cx, layer, li, h, T, SEQ)
                ssd_scan_phase(kb, cx, li, T, SEQ)
                outproj_phase(kb, cx, cx.ssd_w_out[li], 32, cx.s_ynT, h, cx.hA, T)
                h = cx.hA
            elif kind == 1:
                mlstm_layer(kb, cx, layer, li, h, cx.hA, T, SEQ)
                h = cx.hA
            else:
                s5_layer(kb, cx, layer, li, h, cx.hA, T, SEQ)
                h = cx.hA
            if do_mlp:
                mlp_phase(kb, cx, layer, h, cx.hB, T)
                h = cx.hB
        if do_final:
            final_norm_phase(kb, cx, h, cx.out, T)
        kb.barrier()
    return nc


PARAM_SHAPES = [
    ("norm_mix_g", (4, 2048)), ("norm_mlp_g", (4, 2048)),
    ("ssd_w_in", (2, 2048, 10304)), ("ssd_conv_w", (2, 4, 6144)), ("ssd_conv_b", (2, 6144)),
    ("ssd_dt_bias", (2, 64)), ("ssd_a_log", (2, 64)), ("ssd_d", (2, 64)), ("ssd_norm_g", (2, 4096)),
    ("ssd_w_out", (2, 4096, 2048)),
    ("mlstm_w_in", (1, 2048, 6152)), ("mlstm_gate_b", (1, 8)), ("mlstm_head_g", (1, 2048)),
    ("mlstm_w_out", (1, 2048, 2048)),
    ("s5_w_in", (1, 2048, 2048)), ("s5_b_re", (1, 128, 64, 16)), ("s5_b_im", (1, 128, 64, 16)),
    ("s5_c_re", (1, 128, 16, 64)), ("s5_c_im", (1, 128, 16, 64)), ("s5_d", (1, 2048)),
    ("s5_log_dt", (1, 128)), ("s5_a_re", (1, 128, 64)), ("s5_a_im", (1, 128, 64)),
    ("s5_w_out", (1, 2048, 4096)),
    ("mlp_w1", (4, 2048, 8192)), ("mlp_w2", (4, 8192, 2048)), ("final_norm_g", (1, 2048)),
]


def make_in_map(x_shard, params):
    m = {"x": np.ascontiguousarray(x_shard.reshape(-1, D))}
    for name, shape in PARAM_SHAPES:
        m[name] = np.ascontiguousarray(params[name]).reshape(shape)
    for name, arr in _consts_np().items():
        m["c_" + name] = arr
    return m
```

```python
import numpy as np
import ml_dtypes
from contextlib import ExitStack
import concourse.bass as bass
import concourse.mybir as mybir
from concourse.bass_utils import run_bass_kernel_spmd

F32 = mybir.dt.float32
BF16 = mybir.dt.bfloat16
I32 = mybir.dt.int32
AF = mybir.ActivationFunctionType
ALU = mybir.AluOpType
AX = mybir.AxisListType

D = 2048
DFF = 8192
EPS = 1e-5
NDS = 40
ENGS = ("pe", "act", "dve", "pool", "sp")


class KB:
    def __init__(self, nc, es, block):
        self.nc = nc
        self.es = es
        self.block = block
        self.cnt = {e: 0 for e in ENGS}
        self.esem = {e: es.enter_context(nc.semaphore("pg_" + e)) for e in ENGS}
        self.dsem = [es.enter_context(nc.semaphore(f"dq{i}")) for i in range(NDS)]
        self.dval = [0] * NDS
        self.dnext = 0
        self.known = {e: {} for e in ENGS}
        self.res = {}
        eobj = {"pe": nc.tensor, "act": nc.scalar, "dve": nc.vector, "pool": nc.gpsimd, "sp": nc.sync}
        self.sect = {k: (lambda body, e=e: body(e)) for k, e in eobj.items()}

    def _sem(self, key):
        return self.esem[key[1]] if key[0] == "e" else self.dsem[key[1]]

    def _deps(self, eng, reads, writes, is_dma):
        deps = {}

        def need(ev, raw):
            if ev is None:
                return
            k, v = ev
            if (not is_dma) and k == ("e", eng) and not raw:
                return
            if v > deps.get(k, 0):
                deps[k] = v

        for r in reads:
            st = self.res.get(r)
            if st is not None:
                need(st[0], True)
        for w in writes:
            st = self.res.get(w)
            if st is not None:
                need(st[0], False)
                for k, v in st[1].items():
                    need((k, v), False)
        return deps

    def _commit(self, ev, reads, writes):
        for r in reads:
            st = self.res.get(r)
            if st is None:
                st = [None, {}]
                self.res[r] = st
            if ev[1] > st[1].get(ev[0], 0):
                st[1][ev[0]] = ev[1]
        for w in writes:
            self.res[w] = [ev, {}]

    def _emit(self, eng, deps, fn, incsem, incval):
        kn = self.known[eng]
        waits = []
        for k, v in deps.items():
            if kn.get(k, 0) >= v:
                continue
            kn[k] = v
            waits.append((self._sem(k), v))

        def body(e):
            for s, v in waits:
                e.wait_ge(s, v)
            fn(e).then_inc(incsem, incval)

        self.sect[eng](body)

    def op(self, eng, fn, reads=(), writes=()):
        deps = self._deps(eng, reads, writes, False)
        self.cnt[eng] += 1
        ev = (("e", eng), self.cnt[eng])
        self._emit(eng, deps, fn, self.esem[eng], 1)
        self._commit(ev, reads, writes)

    def dma(self, eng, out, in_, reads=(), writes=(), **kw):
        deps = self._deps(eng, reads, writes, True)
        i = self.dnext
        self.dnext = (i + 1) % NDS
        if self.dval[i] > 0:
            k = ("d", i)
            deps[k] = max(deps.get(k, 0), self.dval[i])
        self.dval[i] += 16
        ev = (("d", i), self.dval[i])
        self._emit(eng, deps, lambda e: e.dma_start(out=out, in_=in_, **kw), self.dsem[i], 16)
        self._commit(ev, reads, writes)

    def barrier(self):
        allev = {}
        for e in ENGS:
            if self.cnt[e] > 0:
                allev[("e", e)] = self.cnt[e]
        for i in range(NDS):
            if self.dval[i] > 0:
                allev[("d", i)] = self.dval[i]
        for eng in ENGS:
            kn = self.known[eng]
            waits = []
            for k, v in allev.items():
                if k == ("e", eng) or kn.get(k, 0) >= v:
                    continue
                kn[k] = v
                waits.append((self._sem(k), v))
            if waits:
                def body(e, waits=waits):
                    for s, v in waits:
                        e.wait_ge(s, v)
                self.sect[eng](body)
        self.res = {}


def _consts_np():
    c = {}
    c["ident_bf"] = np.eye(128, dtype=np.float32).astype(ml_dtypes.bfloat16)
    c["ident_f"] = np.eye(128, dtype=np.float32)
    i = np.arange(128)
    c["tri_le"] = (i[:, None] <= i[None, :]).astype(np.float32)
    c["tri_gt"] = (i[:, None] > i[None, :]).astype(np.float32)
    c["ones_f"] = np.ones((128, 128), np.float32)
    c["sel_last"] = np.zeros((128, 128), np.float32)
    c["sel_last"][127, :] = 1.0
    c["gmask"] = (np.arange(128)[:, None] // 16 == np.arange(8)[None, :]).astype(np.float32)
    sw = np.zeros((128, 128), np.float32)
    for p in range(64):
        sw[64 + p, p] = -1.0
        sw[p, 64 + p] = 1.0
    c["swap"] = sw
    c["negmask"] = np.where(i[None, :] > i[:, None], np.float32(-1.0e30), np.float32(0.0)).astype(np.float32)
    return c


class Ctx:
    pass


_UNIQ = [0]


def _uniq(name):
    _UNIQ[0] += 1
    return f"{name}_{_UNIQ[0]}"


def rmsnorm_to_xT(kb, cx, tag, hsrc_ap, gb, xT, tb, hbuf, hk=None):
    nc = kb.nc
    hkey = ("hbuf", tag, tb if hk is None else hk)
    kb.dma("sp", hbuf[:, :], hsrc_ap, writes=[hkey])
    ss = cx.ss
    hn = cx.hn2[tb % 2]
    hnk = ("hn", tb % 2)
    kb.op("act", lambda e: e.activation(out=cx.junk[:, :], in_=hbuf[:, :], func=AF.Square,
                                        accum_out=ss[:, tb:tb + 1]),
          reads=[hkey], writes=["junk", ("ss", tb)])
    kb.op("act", lambda e: e.activation(out=ss[:, tb:tb + 1], in_=ss[:, tb:tb + 1], func=AF.Sqrt,
                                        scale=1.0 / D, bias=cx.epsc[:, 0:1]),
          reads=[("ss", tb)], writes=[("ss", tb)])
    kb.op("dve", lambda e: e.reciprocal(ss[:, tb:tb + 1], ss[:, tb:tb + 1]),
          reads=[("ss", tb)], writes=[("ss", tb)])
    kb.op("dve", lambda e: e.scalar_tensor_tensor(out=hn[:, :], in0=hbuf[:, :], scalar=ss[:, tb:tb + 1],
                                                  in1=gb[:, :], op0=ALU.mult, op1=ALU.mult),
          reads=[hkey, ("ss", tb), "gb"], writes=[hnk])

    def stage2():
        for half in range(2):
            pt = cx.ptr[half]
            for j in range(8):
                kt = half * 8 + j
                kb.op("pe", lambda e, kt=kt, j=j, pt=pt: e.transpose(out=pt[:, j * 128:(j + 1) * 128],
                                                                     in_=hn[:, kt * 128:(kt + 1) * 128],
                                                                     identity=cx.ident_bf[:, :]),
                      reads=[hnk, "ident"], writes=[("ptr", half)])
            if half == 0:
                kb.op("act", lambda e, pt=pt: e.copy(
                    out=xT[:, 0:8, tb * 128:(tb + 1) * 128],
                    in_=pt[:, :].rearrange("p (k t) -> p k t", k=8)),
                    reads=[("ptr", half)], writes=[("xT", tb)])
            else:
                kb.op("dve", lambda e, pt=pt: e.tensor_copy(
                    xT[:, 8:16, tb * 128:(tb + 1) * 128],
                    pt[:, :].rearrange("p (k t) -> p k t", k=8)),
                    reads=[("ptr", half)], writes=[("xT", tb)])

    prev = cx.norm_pending
    cx.norm_pending = stage2
    if prev is not None:
        prev()


def rmsnorm_flush(cx):
    if cx.norm_pending is not None:
        cx.norm_pending()
        cx.norm_pending = None


class WStream:
    def __init__(self, kb, bufs):
        self.kb = kb
        self.bufs = bufs
        self.i = 0

    def load(self, src_ap, kt, ncols):
        i = self.i % len(self.bufs)
        self.i += 1
        wv = self.bufs[i][:, 0:kt * ncols].rearrange("p (k n) -> p k n", k=kt)
        key = ("wb", i)
        self.kb.dma("pool", wv, src_ap, writes=[key])
        return wv, key


def tile_size(T, SEQ):
    return 1024 if (T % 1024 == 0 and SEQ % 1024 == 0) else 512


def gemm_fm(kb, cx, ws, wv_all, col0, nblk, xT, TT, pbanks, epi):
    NTH = TT // 512
    cnt = 0
    for cb in range(nblk):
        wv, wkey = ws.load(wv_all[:, :, col0 + cb * 512:col0 + (cb + 1) * 512], 16, 512)
        for mi in range(4):
            for th in range(NTH):
                pi = pbanks[cnt % len(pbanks)]
                cnt += 1
                ps = cx.psum[pi]
                pk = ("ps", pi)
                for kt in range(16):
                    kb.op("pe", lambda e, kt=kt, mi=mi, ps=ps, wv=wv, th=th: e.matmul(
                        ps[:, :], lhsT=wv[:, kt, mi * 128:(mi + 1) * 128], rhs=xT[:, kt, th * 512:(th + 1) * 512],
                        start=(kt == 0), stop=(kt == 15)),
                        reads=[wkey] + [("xT", th * 4 + i) for i in range(4)], writes=[pk])
                epi(cb * 4 + mi, th, ps, pk)


def gemm_tm(kb, cx, ws, wv_all, col0, nblk, ncols, xT, TT, pbanks, epi):
    NTB = TT // 128
    cnt = 0
    for cb in range(nblk):
        wv, wkey = ws.load(wv_all[:, :, col0 + cb * ncols:col0 + (cb + 1) * ncols], 16, ncols)
        for tb in range(NTB):
            pi = pbanks[cnt % len(pbanks)]
            cnt += 1
            ps = cx.psum[pi]
            pk = ("ps", pi)
            for kt in range(16):
                kb.op("pe", lambda e, kt=kt, tb=tb, ps=ps, wv=wv: e.matmul(
                    ps[:, 0:ncols], lhsT=xT[:, kt, tb * 128:(tb + 1) * 128], rhs=wv[:, kt, :],
                    start=(kt == 0), stop=(kt == 15)), reads=[wkey, ("xT", tb)], writes=[pk])
            epi(cb, tb, ps, pk)


def mlp_phase(kb, cx, layer, h_in, h_out, T):
    nc = kb.nc
    TT = 1024 if T % 1024 == 0 else 512
    NTB = TT // 128
    NTH = TT // 512
    with ExitStack() as es:
        sb = lambda name, shape, dt: es.enter_context(nc.sbuf_tensor(_uniq(name), shape, dt))
        hb = [sb(f"m_hb{i}", [128, D], F32) for i in range(NTB)]
        xT = sb("m_xT", [128, 16, TT], BF16)
        a1T = sb("m_a1T", [128, 16, TT], BF16)
        wb = [sb(f"m_wb{i}", [128, 16 * 512], BF16) for i in range(2)]
        gb = sb("m_gb", [128, D], F32)
        rl = [sb(f"m_rl{i}", [128, 512], F32) for i in range(2)]
        kb.dma("sp", gb[:, :], cx.norm_mlp_g[layer:layer + 1, :].broadcast_to([128, D]), writes=["gb"])
        w1v = cx.mlp_w1[layer].rearrange("(kt p) n -> p kt n", p=128)
        w2v = cx.mlp_w2[layer].rearrange("(kt p) n -> p kt n", p=128)
        wi = 0
        ri = 0
        p1 = 0
        p2 = 0
        for tt in range(T // TT):
            for tb in range(NTB):
                t0 = tt * TT + tb * 128
                rmsnorm_to_xT(kb, cx, "m", h_in[t0:t0 + 128, :], gb, xT, tb, hb[tb])
            rmsnorm_flush(cx)
            for q in range(4):
                for cbl in range(4):
                    cb = q * 4 + cbl
                    w = wb[wi % 2]; wkey = ("wb", wi % 2); wi += 1
                    wv = w[:, :].rearrange("p (k n) -> p k n", k=16)
                    kb.dma("pool", wv, w1v[:, :, cb * 512:(cb + 1) * 512], writes=[wkey])
                    for mi in range(4):
                        ml = cbl * 4 + mi
                        for th in range(NTH):
                            pi = p1 % 4; p1 += 1
                            ps = cx.psum[pi]; pk = ("ps", pi)
                            for kt in range(16):
                                kb.op("pe", lambda e, kt=kt, mi=mi, ps=ps, wv=wv, th=th: e.matmul(
                                    ps[:, :], lhsT=wv[:, kt, mi * 128:(mi + 1) * 128],
                                    rhs=xT[:, kt, th * 512:(th + 1) * 512], start=(kt == 0), stop=(kt == 15)),
                                    reads=[wkey] + [("xT", th * 4 + i) for i in range(4)], writes=[pk])
                            r = rl[ri % 2]; rk = ("rl", ri % 2); ri += 1
                            kb.op("act", lambda e, ps=ps, r=r: e.activation(out=r[:, :], in_=ps[:, :], func=AF.Relu),
                                  reads=[pk], writes=[rk])
                            kb.op("dve", lambda e, r=r, ml=ml, th=th: e.tensor_tensor(
                                out=a1T[:, ml, th * 512:(th + 1) * 512], in0=r[:, :], in1=r[:, :], op=ALU.mult),
                                reads=[rk], writes=[("a1T", ml, th)])
                for cb in range(4):
                    w = wb[wi % 2]; wkey = ("wb", wi % 2); wi += 1
                    wv = w[:, :].rearrange("p (k n) -> p k n", k=16)
                    kb.dma("pool", wv, w2v[:, q * 16:(q + 1) * 16, cb * 512:(cb + 1) * 512], writes=[wkey])
                    for tb in range(NTB):
                        pi = 4 + p2 % 2; p2 += 1
                        ps = cx.psum[pi]; pk = ("ps", pi)
                        for kt in range(16):
                            kb.op("pe", lambda e, kt=kt, tb=tb, ps=ps, wv=wv: e.matmul(
                                ps[:, :], lhsT=a1T[:, kt, tb * 128:(tb + 1) * 128], rhs=wv[:, kt, :],
                                start=(kt == 0), stop=(kt == 15)),
                                reads=[wkey, ("a1T", kt, tb // 4)], writes=[pk])
                        kb.op("dve", lambda e, tb=tb, cb=cb, ps=ps: e.tensor_tensor(
                            out=hb[tb][:, cb * 512:(cb + 1) * 512], in0=ps[:, :],
                            in1=hb[tb][:, cb * 512:(cb + 1) * 512], op=ALU.add),
                            reads=[pk, ("hbuf", "m", tb)], writes=[("hbuf", "m", tb)])
            for tb in range(NTB):
                t0 = tt * TT + tb * 128
                kb.dma("sp", h_out[t0:t0 + 128, :], hb[tb][:, :], reads=[("hbuf", "m", tb)],
                       writes=[("h", layer, "mlp", t0)])
        kb.barrier()


def final_norm_phase(kb, cx, h_in, out, T):
    nc = kb.nc
    with ExitStack() as es:
        sb = lambda name, shape, dt: es.enter_context(nc.sbuf_tensor(_uniq(name), shape, dt))
        hb = [sb(f"f_hb{i}", [128, D], F32) for i in range(2)]
        ob = [sb(f"f_ob{i}", [128, D], F32) for i in range(2)]
        gb = sb("f_gb", [128, D], F32)
        kb.dma("sp", gb[:, :], cx.final_norm_g[0:1, :].broadcast_to([128, D]), writes=["gb"])
        ss = cx.ss
        for i in range(T // 128):
            b = i % 2
            t0 = i * 128
            kb.dma("sp", hb[b][:, :], h_in[t0:t0 + 128, :], writes=[("fhb", b)])
            kb.op("act", lambda e, b=b: e.activation(out=cx.junk[:, :], in_=hb[b][:, :], func=AF.Square,
                                                     accum_out=ss[:, b:b + 1]),
                  reads=[("fhb", b)], writes=["junk", ("ss", b)])
            kb.op("act", lambda e, b=b: e.activation(out=ss[:, b:b + 1], in_=ss[:, b:b + 1], func=AF.Sqrt,
                                                     scale=1.0 / D, bias=cx.epsc[:, 0:1]),
                  reads=[("ss", b)], writes=[("ss", b)])
            kb.op("dve", lambda e, b=b: e.reciprocal(ss[:, b:b + 1], ss[:, b:b + 1]),
                  reads=[("ss", b)], writes=[("ss", b)])
            kb.op("dve", lambda e, b=b: e.scalar_tensor_tensor(out=ob[b][:, :], in0=hb[b][:, :],
                                                               scalar=ss[:, b:b + 1], in1=gb[:, :],
                                                               op0=ALU.mult, op1=ALU.mult),
                  reads=[("fhb", b), ("ss", b), "gb"], writes=[("fob", b)])
            kb.dma("sp", out[t0:t0 + 128, :], ob[b][:, :], reads=[("fob", b)], writes=[("out", i)])
        kb.barrier()


SSD_INNER = 4096
SSD_CONV_DIM = 6144
SSD_PROJ = 10304


def ssd_inproj_phase(kb, cx, layer, li, h_in, T, SEQ):
    nc = kb.nc
    TT = tile_size(T, SEQ)
    NTB = TT // 128
    NTH = TT // 512
    with ExitStack() as es:
        sb = lambda name, shape, dt: es.enter_context(nc.sbuf_tensor(_uniq(name), shape, dt))
        hb = [sb(f"s1_hb{i}", [128, D], F32) for i in range(2)]
        xT = sb("s1_xT", [128, 16, TT], BF16)
        ws = WStream(kb, [sb(f"s1_wb{i}", [128, 16 * 512], BF16) for i in range(2)])
        gb = sb("s1_gb", [128, D], F32)
        cbuf = [sb(f"s1_cb{i}", [128, TT + 4], F32) for i in range(2)]
        acc = [sb(f"s1_acc{i}", [128, TT], F32) for i in range(2)]
        rbf = [sb(f"s1_r{i}", [128, TT], BF16) for i in range(3)]
        carry = sb("s1_carry", [128, 48, 4], F32)
        cw = sb("s1_cw", [128, 48, 4], F32)
        cbias = sb("s1_cbias", [128, 48], F32)
        zst = [sb(f"s1_zst{i}", [128, 512], BF16) for i in range(2)]
        xstg = [sb(f"s1_xstg{i}", [128, NTB, 512], BF16) for i in range(2)]
        dtb = sb("s1_dtb", [128, 64], F32)
        arow = sb("s1_arow", [128, 64], F32)
        dts = [sb(f"s1_dts{i}", [128, 64], F32) for i in range(2)]
        lgs = [sb(f"s1_lgs{i}", [128, 64], F32) for i in range(2)]
        kb.dma("sp", gb[:, :], cx.norm_mix_g[layer:layer + 1, :].broadcast_to([128, D]), writes=["gb"])
        kb.dma("sp", dtb[:, :], cx.ssd_dt_bias[li:li + 1, :].broadcast_to([128, 64]), writes=["dtb"])
        kb.dma("sp", arow[:, :], cx.ssd_a_log[li:li + 1, :].broadcast_to([128, 64]), writes=["arow"])
        kb.op("act", lambda e: e.activation(out=arow[:, :], in_=arow[:, :], func=AF.Exp), reads=["arow"],
              writes=["arow"])
        kb.op("dve", lambda e: e.tensor_scalar(arow[:, :], arow[:, :], -1.0, None, ALU.mult), reads=["arow"],
              writes=["arow"])
        for k in range(4):
            kb.dma("sp", cw[:, :, k], cx.ssd_conv_w[li, k, :].rearrange("(f p) -> p f", p=128),
                   writes=["cw"], allow_slow_non_contiguous=True)
        kb.dma("sp", cbias[:, :], cx.ssd_conv_b[li, :].rearrange("(f p) -> p f", p=128), writes=["cbias"],
               allow_slow_non_contiguous=True)
        wv_all = cx.ssd_w_in[li].rearrange("(kt p) n -> p kt n", p=128)
        zc = [0]
        for tt in range(T // TT):
            tbase = tt * TT
            seq_start = tbase % SEQ == 0
            for tb in range(NTB):
                t0 = tbase + tb * 128
                rmsnorm_to_xT(kb, cx, "s1", h_in[t0:t0 + 128, :], gb, xT, tb, hb[tb % 2], hk=tb % 2)
            rmsnorm_flush(cx)

            def epi_z(cb, tb, ps, pk):
                zi = zc[0] % 2
                zc[0] += 1
                zb = zst[zi]
                kb.op("act", lambda e: e.activation(out=zb[:, :], in_=ps[:, :], func=AF.Silu),
                      reads=[pk], writes=[("zst", zi)])
                t0 = tbase + tb * 128
                kb.dma("sp", cx.s_zs[t0:t0 + 128, cb * 512:(cb + 1) * 512], zb[:, :], reads=[("zst", zi)])

            gemm_tm(kb, cx, ws, wv_all, 0, 8, 512, xT, TT, [0, 1], epi_z)

            pend = []

            def epi_x(ft, th, ps, pk):
                c = cbuf[ft % 2]; ck = ("cbuf", ft % 2)
                a = acc[ft % 2]; ak = ("acc", ft % 2)
                if th == 0:
                    while pend and pend[0][0] <= ft - 2:
                        pend.pop(0)[1]()
                    if seq_start:
                        kb.op("dve", lambda e: e.memset(c[:, 0:4], 0.0), writes=[ck])
                    else:
                        kb.op("dve", lambda e: e.tensor_copy(c[:, 0:4], carry[:, ft, :]),
                              reads=[("carry", ft)], writes=[ck])
                kb.op("act", lambda e: e.copy(out=c[:, 4 + th * 512:4 + (th + 1) * 512], in_=ps[:, :]), reads=[pk],
                      writes=[ck])
                if th < NTH - 1:
                    return
                kb.op("dve", lambda e: e.tensor_copy(carry[:, ft, :], c[:, TT:TT + 4]), reads=[ck],
                      writes=[("carry", ft)])
                kb.op("act", lambda e: e.activation(out=a[:, :], in_=c[:, 1:TT + 1], func=AF.Identity,
                                                    scale=cw[:, ft, 0:1], bias=cbias[:, ft:ft + 1]),
                      reads=[ck, "cw", "cbias"], writes=[ak])
                for k in range(1, 4):
                    kb.op("dve", lambda e, k=k: e.scalar_tensor_tensor(
                        out=a[:, :], in0=c[:, 1 + k:TT + 1 + k], scalar=cw[:, ft, k:k + 1], in1=a[:, :],
                        op0=ALU.mult, op1=ALU.add), reads=[ck, ak, "cw"], writes=[ak])
                r = rbf[ft % 3]; rk = ("rbf", ft % 3)
                kb.op("act", lambda e: e.activation(out=r[:, :], in_=a[:, :], func=AF.Silu), reads=[ak], writes=[rk])
                if ft >= 32:
                    dst = cx.s_BT if ft < 40 else cx.s_CT
                    f0 = (ft - 32) * 128 if ft < 40 else (ft - 40) * 128
                    kb.dma("sp", dst[f0:f0 + 128, tbase:tbase + TT], r[:, :], reads=[rk])
                if ft < 40:
                    def post():
                        hf = ft % 2
                        pt = cx.ptr[hf]
                        sg = (ft // 4) % 2
                        for tb in range(NTB):
                            kb.op("pe", lambda e, tb=tb: e.transpose(
                                out=pt[:, tb * 128:(tb + 1) * 128], in_=r[:, tb * 128:(tb + 1) * 128],
                                identity=cx.ident_bf[:, :]), reads=[rk, "ident"], writes=[("ptr", hf)])
                        dst_v = xstg[sg][:, :, (ft % 4) * 128:(ft % 4 + 1) * 128]
                        src_v = pt[:, 0:NTB * 128].rearrange("p (t f) -> p t f", t=NTB)
                        if ft % 2 == 0:
                            kb.op("act", lambda e: e.copy(out=dst_v, in_=src_v), reads=[("ptr", hf)],
                                  writes=[("xstg", sg)])
                        else:
                            kb.op("dve", lambda e: e.tensor_copy(dst_v, src_v), reads=[("ptr", hf)],
                                  writes=[("xstg", sg)])
                        if ft % 4 == 3:
                            for tb in range(NTB):
                                t0 = tbase + tb * 128
                                if ft < 32:
                                    d_ = cx.s_xs[t0:t0 + 128, (ft // 4) * 512:(ft // 4 + 1) * 512]
                                else:
                                    d_ = cx.s_Btok[t0:t0 + 128, ((ft - 32) // 4) * 512:((ft - 32) // 4 + 1) * 512]
                                kb.dma("sp", d_, xstg[sg][:, tb, :], reads=[("xstg", sg)])
                    pend.append((ft, post))

            gemm_fm(kb, cx, ws, wv_all, 4096, 12, xT, TT, [2, 3, 4, 5], epi_x)
            while pend:
                pend.pop(0)[1]()

            def epi_dt(cb, tb, ps, pk):
                d_ = dts[tb % 2]; dk = ("dts", tb % 2)
                l_ = lgs[tb % 2]; lk = ("lgs", tb % 2)
                kb.op("dve", lambda e: e.tensor_tensor(out=d_[:, :], in0=ps[:, 0:64], in1=dtb[:, :], op=ALU.add),
                      reads=[pk, "dtb"], writes=[dk])
                kb.op("act", lambda e: e.activation(out=d_[:, :], in_=d_[:, :], func=AF.Exp), reads=[dk], writes=[dk])
                kb.op("act", lambda e: e.activation(out=d_[:, :], in_=d_[:, :], func=AF.Ln, bias=cx.onec[:, 0:1],
                                                    scale=1.0), reads=[dk], writes=[dk])
                kb.op("dve", lambda e: e.tensor_tensor(out=l_[:, :], in0=d_[:, :], in1=arow[:, :], op=ALU.mult),
                      reads=[dk, "arow"], writes=[lk])
                t0 = tbase + tb * 128
                kb.dma("sp", cx.s_dt[t0:t0 + 128, :], d_[:, :], reads=[dk])
                kb.dma("sp", cx.s_loga[t0:t0 + 128, :], l_[:, :], reads=[lk])

            gemm_tm(kb, cx, ws, wv_all, 10240, 1, 64, xT, TT, [0, 1], epi_dt)
        kb.barrier()


def ssd_scan_phase(kb, cx, li, T, SEQ):
    nc = kb.nc
    with ExitStack() as es:
        sb = lambda name, shape, dt: es.enter_context(nc.sbuf_tensor(_uniq(name), shape, dt))
        S = sb("s3_S", [128, 4096], F32)
        Sbf = sb("s3_Sbf", [128, 4096], BF16)
        xs = [sb(f"s3_xs{i}", [128, 4096], BF16) for i in range(2)]
        zs = [sb(f"s3_zs{i}", [128, 4096], BF16) for i in range(2)]
        Bt = [sb(f"s3_Bt{i}", [128, 1024], BF16) for i in range(2)]
        BT = [sb(f"s3_BT{i}", [128, 8, 128], BF16) for i in range(2)]
        CT = [sb(f"s3_CT{i}", [128, 8, 128], BF16) for i in range(2)]
        dtt = [sb(f"s3_dt{i}", [128, 64], F32) for i in range(2)]
        lga = [sb(f"s3_lg{i}", [128, 64], F32) for i in range(2)]
        e3 = sb("s3_e3", [128, 192], F32)
        xdt = sb("s3_xdt", [128, 4096], BF16)
        xdt2 = sb("s3_xdt2", [128, 4096], BF16)
        R = [sb(f"s3_R{i}", [128, 1024], F32) for i in range(2)]
        E = [sb(f"s3_E{i}", [128, 1024], BF16) for i in range(2)]
        cbm = [sb(f"s3_cbm{i}", [128, 128], BF16) for i in range(2)]
        M = [sb(f"s3_M{i}", [128, 1024], BF16) for i in range(2)]
        tmp = [sb(f"s3_tmp{i}", [128, 512], F32) for i in range(2)]
        y2 = [sb(f"s3_y{i}", [128, 4096], F32) for i in range(2)]
        yn = sb("s3_yn", [128, 4096], BF16)
        ynT = [sb(f"s3_ynT{i}", [128, 32, 128], BF16) for i in range(2)]
        drow = sb("s3_drow", [128, 64], F32)
        ngb = sb("s3_ngb", [128, 4096], F32)
        tail_pending = [None]
        tle = sb("s3_tle", [128, 128], F32)
        tgt = sb("s3_tgt", [128, 128], F32)
        one = sb("s3_one", [128, 128], F32)
        kb.dma("sp", drow[:, :], cx.ssd_d[li:li + 1, :].broadcast_to([128, 64]), writes=["drow"])
        kb.dma("sp", ngb[:, :], cx.ssd_norm_g[li:li + 1, :].broadcast_to([128, 4096]), writes=["ngb"])
        kb.dma("sp", tle[:, :], cx.c_tri_le[:, :], writes=["tle"])
        kb.dma("sp", tgt[:, :], cx.c_tri_gt[:, :], writes=["tgt"])
        kb.dma("sp", one[:, :], cx.c_ones_f[:, :], writes=["one"])
        BTv = cx.s_BT.rearrange("(g n) t -> n g t", n=128)
        CTv = cx.s_CT.rearrange("(g n) t -> n g t", n=128)
        nch = T // 128
        for ci in range(nch):
            t0 = ci * 128
            b = ci % 2
            if t0 % SEQ == 0:
                kb.op("dve", lambda e: e.memset(S[:, :], 0.0), writes=[("S", g) for g in range(8)])
                kb.op("pool", lambda e: e.memset(Sbf[:, :], 0.0), writes=[("Sbf", g) for g in range(8)])
            kb.dma("sp", xs[b][:, :], cx.s_xs[t0:t0 + 128, :], writes=[("xs", b)])
            kb.dma("sp", zs[b][:, :], cx.s_zs[t0:t0 + 128, :], writes=[("zs", b)])
            kb.dma("sp", Bt[b][:, :], cx.s_Btok[t0:t0 + 128, :], writes=[("Bt", b)])
            kb.dma("sp", BT[b][:, :, :], BTv[:, :, t0:t0 + 128], writes=[("BT", b)])
            kb.dma("sp", CT[b][:, :, :], CTv[:, :, t0:t0 + 128], writes=[("CT", b)])
            kb.dma("sp", dtt[b][:, :], cx.s_dt[t0:t0 + 128, :], writes=[("dt", b)])
            kb.dma("sp", lga[b][:, :], cx.s_loga[t0:t0 + 128, :], writes=[("lg", b)])
            ps0 = cx.psum[0]
            for j, (lt, lk) in enumerate(((tle, "tle"), (tgt, "tgt"), (one, "one"))):
                kb.op("pe", lambda e, j=j, lt=lt: e.matmul(ps0[:, j * 64:(j + 1) * 64], lhsT=lt[:, :],
                                                           rhs=lga[b][:, :], start=True, stop=True),
                      reads=[lk, ("lg", b)], writes=[("ps", 0)])
            kb.op("act", lambda e: e.activation(out=e3[:, :], in_=ps0[:, 0:192], func=AF.Exp),
                  reads=[("ps", 0)], writes=["e3"])
            kb.op("dve", lambda e: e.tensor_tensor(
                out=xdt[:, :].rearrange("p (h q) -> p h q", q=64),
                in0=xs[b][:, :].rearrange("p (h q) -> p h q", q=64),
                in1=dtt[b][:, :].rearrange("p (h o) -> p h o", o=1).broadcast_to([128, 64, 64]), op=ALU.mult),
                reads=[("xs", b), ("dt", b)], writes=["xdt"])
            kb.op("pool", lambda e: e.tensor_tensor(
                out=xdt2[:, :].rearrange("p (h q) -> p h q", q=64),
                in0=xdt[:, :].rearrange("p (h q) -> p h q", q=64),
                in1=e3[:, 64:128].rearrange("p (h o) -> p h o", o=1).broadcast_to([128, 64, 64]), op=ALU.mult),
                reads=["xdt", "e3"], writes=["xdt2"])
            def stage_ar(g):
                gb_ = g % 2
                for j in range(8):
                    kb.op("act", lambda e, g=g, gb_=gb_, j=j: e.activation(
                        out=R[gb_][:, j * 128:(j + 1) * 128], in_=tle[:, :], func=AF.Identity,
                        scale=lga[b][:, g * 8 + j:g * 8 + j + 1]),
                        reads=[("lg", b), "tle"], writes=[("R", gb_)])
            def stage_ape(g):
                gb_ = g % 2
                for hf in range(2):
                    psd = cx.psum[1 + hf]
                    kb.op("pe", lambda e, hf=hf, psd=psd, gb_=gb_: e.matmul(
                        psd[:, :], lhsT=tgt[:, :], rhs=R[gb_][:, hf * 512:(hf + 1) * 512], start=True, stop=True),
                        reads=["tgt", ("R", gb_)], writes=[("ps", 1 + hf)])
                    kb.op("act", lambda e, hf=hf, psd=psd, gb_=gb_: e.activation(
                        out=E[gb_][:, hf * 512:(hf + 1) * 512], in_=psd[:, :], func=AF.Exp),
                        reads=[("ps", 1 + hf)], writes=[("E", gb_)])
                kb.op("pe", lambda e, g=g: e.matmul(ps0[:, 256:384], lhsT=BT[b][:, g, :], rhs=CT[b][:, g, :],
                                                    start=True, stop=True),
                      reads=[("BT", b), ("CT", b)], writes=[("ps", 0)])
            def stage_am(g):
                gb_ = g % 2
                kb.op("dve", lambda e, gb_=gb_: e.tensor_tensor(out=cbm[gb_][:, :], in0=ps0[:, 256:384],
                                                               in1=tle[:, :], op=ALU.mult),
                      reads=[("ps", 0), "tle"], writes=[("cbm", gb_)])
                kb.op("dve", lambda e, gb_=gb_: e.tensor_tensor(
                    out=M[gb_][:, :].rearrange("p (j l) -> p j l", j=8),
                    in0=E[gb_][:, :].rearrange("p (j l) -> p j l", j=8),
                    in1=cbm[gb_][:, :].rearrange("p (o l) -> p o l", o=1).broadcast_to([128, 8, 128]),
                    op=ALU.mult), reads=[("E", gb_), ("cbm", gb_)], writes=[("M", gb_)])
            def stage_b(g):
                gb_ = g % 2
                psy = cx.psum[3]
                for j in range(8):
                    h = g * 8 + j
                    kb.op("pe", lambda e, j=j, h=h, gb_=gb_: e.matmul(
                        psy[:, j * 64:(j + 1) * 64], lhsT=M[gb_][:, j * 128:(j + 1) * 128],
                        rhs=xdt[:, h * 64:(h + 1) * 64], start=True, stop=True),
                        reads=[("M", gb_), "xdt"], writes=[("ps", 3)])
                pso = cx.psum[4]
                kb.op("pe", lambda e, g=g: e.matmul(pso[:, :], lhsT=CT[b][:, g, :],
                                                    rhs=Sbf[:, g * 512:(g + 1) * 512], start=True, stop=True),
                      reads=[("CT", b), ("Sbf", g)], writes=[("ps", 4)])
                tm = tmp[gb_]
                kb.op("dve", lambda e, g=g, tm=tm: e.tensor_tensor(
                    out=tm[:, :].rearrange("p (j q) -> p j q", j=8),
                    in0=pso[:, :].rearrange("p (j q) -> p j q", j=8),
                    in1=e3[:, g * 8:(g + 1) * 8].rearrange("p (j o) -> p j o", o=1).broadcast_to([128, 8, 64]),
                    op=ALU.mult), reads=[("ps", 4), "e3"], writes=[("tmp", gb_)])
                kb.op("dve", lambda e, g=g, tm=tm: e.tensor_tensor(
                    out=y2[b][:, g * 512:(g + 1) * 512], in0=psy[:, :], in1=tm[:, :], op=ALU.add),
                    reads=[("ps", 3), ("tmp", gb_)], writes=[("y", b, g)])
                pss = cx.psum[5]
                kb.op("pe", lambda e, g=g: e.matmul(pss[:, :], lhsT=Bt[b][:, g * 128:(g + 1) * 128],
                                                    rhs=xdt2[:, g * 512:(g + 1) * 512], start=True, stop=True),
                      reads=[("Bt", b), "xdt2"], writes=[("ps", 5)])
                kb.op("pool", lambda e, g=g: e.tensor_tensor(
                    out=S[:, g * 512:(g + 1) * 512].rearrange("p (j q) -> p j q", j=8),
                    in0=S[:, g * 512:(g + 1) * 512].rearrange("p (j q) -> p j q", j=8),
                    in1=e3[:, 128 + g * 8:128 + (g + 1) * 8].rearrange("p (j o) -> p j o", o=1).broadcast_to(
                        [128, 8, 64]), op=ALU.mult), reads=[("S", g), "e3"], writes=[("S", g)])
                kb.op("dve", lambda e, g=g: e.tensor_tensor(
                    out=S[:, g * 512:(g + 1) * 512], in0=pss[:, :], in1=S[:, g * 512:(g + 1) * 512], op=ALU.add),
                    reads=[("ps", 5), ("S", g)], writes=[("S", g)])
                kb.op("pool", lambda e, g=g: e.tensor_copy(Sbf[:, g * 512:(g + 1) * 512],
                                                           S[:, g * 512:(g + 1) * 512]),
                      reads=[("S", g)], writes=[("Sbf", g)])
            stage_ar(0)
            stage_ape(0)
            stage_am(0)
            if tail_pending[0] is not None:
                tail_pending[0]()
                tail_pending[0] = None
            for g in range(8):
                if g + 1 < 8:
                    stage_ar(g + 1)
                stage_b(g)
                if g + 1 < 8:
                    stage_ape(g + 1)
                    stage_am(g + 1)

            def tail(b=b, t0=t0):
                y = y2[b]
                ykeys = [("y", b, g) for g in range(8)]
                kb.op("pool", lambda e: e.tensor_tensor(
                    out=yn[:, :].rearrange("p (h q) -> p h q", q=64),
                    in0=xs[b][:, :].rearrange("p (h q) -> p h q", q=64),
                    in1=drow[:, :].rearrange("p (h o) -> p h o", o=1).broadcast_to([128, 64, 64]), op=ALU.mult),
                    reads=[("xs", b), "drow"], writes=["yn"])
                kb.op("dve", lambda e: e.tensor_tensor(out=y[:, :], in0=y[:, :], in1=yn[:, :], op=ALU.add),
                      reads=ykeys + ["yn"], writes=ykeys)
                kb.op("dve", lambda e: e.tensor_tensor(out=y[:, :], in0=y[:, :], in1=zs[b][:, :], op=ALU.mult),
                      reads=ykeys + [("zs", b)], writes=ykeys)
                ss = cx.ss
                kb.op("act", lambda e: e.activation(out=cx.junk4[:, :], in_=y[:, :], func=AF.Square,
                                                    accum_out=ss[:, 0:1]), reads=ykeys, writes=["junk4", ("ss", 0)])
                kb.op("act", lambda e: e.activation(out=ss[:, 0:1], in_=ss[:, 0:1], func=AF.Sqrt,
                                                    scale=1.0 / 4096, bias=cx.epsc[:, 0:1]),
                      reads=[("ss", 0)], writes=[("ss", 0)])
                kb.op("dve", lambda e: e.reciprocal(ss[:, 0:1], ss[:, 0:1]), reads=[("ss", 0)], writes=[("ss", 0)])
                kb.op("dve", lambda e: e.scalar_tensor_tensor(out=yn[:, :], in0=y[:, :], scalar=ss[:, 0:1],
                                                              in1=ngb[:, :], op0=ALU.mult, op1=ALU.mult),
                      reads=ykeys + [("ss", 0), "ngb"], writes=["yn"])
                yT = ynT[b]
                for q in range(4):
                    pt = cx.ptr[q % 2]
                    for j in range(8):
                        kt = q * 8 + j
                        kb.op("pe", lambda e, kt=kt, j=j, pt=pt: e.transpose(
                            out=pt[:, j * 128:(j + 1) * 128], in_=yn[:, kt * 128:(kt + 1) * 128],
                            identity=cx.ident_bf[:, :]), reads=["yn", "ident"], writes=[("ptr", q % 2)])
                    if q % 2 == 0:
                        kb.op("act", lambda e, q=q, pt=pt: e.copy(
                            out=yT[:, q * 8:(q + 1) * 8, :], in_=pt[:, :].rearrange("p (k t) -> p k t", k=8)),
                            reads=[("ptr", q % 2)], writes=[("ynT", b)])
                    else:
                        kb.op("pool" if False else "dve", lambda e, q=q, pt=pt: e.tensor_copy(
                            yT[:, q * 8:(q + 1) * 8, :], pt[:, :].rearrange("p (k t) -> p k t", k=8)),
                            reads=[("ptr", q % 2)], writes=[("ynT", b)])
                kb.dma("sp", cx.s_ynT.rearrange("(k p) t -> p k t", p=128)[:, :, t0:t0 + 128], yT[:, :, :],
                       reads=[("ynT", b)])

            tail_pending[0] = tail
        if tail_pending[0] is not None:
            tail_pending[0]()
            tail_pending[0] = None
        kb.barrier()


def outproj_phase(kb, cx, w_out_ap, KT, src_T, h_in, h_out, T):
    nc = kb.nc
    TT = 1024 if T % 1024 == 0 else 512
    NTB = TT // 128
    NCOL = 8192 // KT
    with ExitStack() as es:
        sb = lambda name, shape, dt: es.enter_context(nc.sbuf_tensor(_uniq(name), shape, dt))
        hb = [sb(f"o_hb{i}", [128, D], F32) for i in range(NTB)]
        aT = sb("o_aT", [128, KT, TT], BF16)
        wb = [sb(f"o_wb{i}", [128, 8192], BF16) for i in range(2)]
        wv_all = w_out_ap.rearrange("(kt p) n -> p kt n", p=128)
        srcv = src_T.rearrange("(k p) t -> p k t", p=128)
        wi = 0
        pc = 0
        for tt in range(T // TT):
            kb.dma("sp", aT[:, :, :], srcv[:, :, tt * TT:(tt + 1) * TT], writes=["aT"])
            for tb in range(NTB):
                t0 = tt * TT + tb * 128
                kb.dma("sp", hb[tb][:, :], h_in[t0:t0 + 128, :], writes=[("hb", tb)])
            for cb in range(D // NCOL):
                w = wb[wi % 2]; wkey = ("wb", wi % 2); wi += 1
                wv = w[:, :].rearrange("p (k n) -> p k n", k=KT)
                kb.dma("pool", wv, wv_all[:, :, cb * NCOL:(cb + 1) * NCOL], writes=[wkey])
                for tb in range(NTB):
                    pi = pc % 4; pc += 1
                    ps = cx.psum[pi]; pk = ("ps", pi)
                    for kt in range(KT):
                        kb.op("pe", lambda e, kt=kt, tb=tb, ps=ps, wv=wv: e.matmul(
                            ps[:, 0:NCOL], lhsT=aT[:, kt, tb * 128:(tb + 1) * 128], rhs=wv[:, kt, :],
                            start=(kt == 0), stop=(kt == KT - 1)), reads=[wkey, "aT"], writes=[pk])
                    kb.op("dve", lambda e, tb=tb, cb=cb, ps=ps: e.tensor_tensor(
                        out=hb[tb][:, cb * NCOL:(cb + 1) * NCOL], in0=ps[:, 0:NCOL],
                        in1=hb[tb][:, cb * NCOL:(cb + 1) * NCOL], op=ALU.add),
                        reads=[pk, ("hb", tb)], writes=[("hb", tb)])
            for tb in range(NTB):
                t0 = tt * TT + tb * 128
                kb.dma("sp", h_out[t0:t0 + 128, :], hb[tb][:, :], reads=[("hb", tb)])
        kb.barrier()


NEG = -1.0e30


def mlstm_layer(kb, cx, layer, li, h_in, h_out, T, SEQ):
    mlstm_inproj_phase(kb, cx, layer, li, h_in, T)
    mlstm_scan_phase(kb, cx, li, T, SEQ)
    outproj_phase(kb, cx, cx.mlstm_w_out[li], 16, cx.s_ynT[0:2048, :], h_in, h_out, T)


def mlstm_inproj_phase(kb, cx, layer, li, h_in, T):
    nc = kb.nc
    TT = 1024 if T % 1024 == 0 else 512
    NTB = TT // 128
    with ExitStack() as es:
        sb = lambda name, shape, dt: es.enter_context(nc.sbuf_tensor(_uniq(name), shape, dt))
        hb = [sb(f"m1_hb{i}", [128, D], F32) for i in range(2)]
        xT = sb("m1_xT", [128, 16, TT], BF16)
        ws = WStream(kb, [sb(f"m1_wb{i}", [128, 16 * 512], BF16) for i in range(2)])
        gb = sb("m1_gb", [128, D], F32)
        st = [sb(f"m1_st{i}", [128, 512], BF16) for i in range(4)]
        gbias = sb("m1_gbias", [128, 8], F32)
        gt = [sb(f"m1_gt{i}", [128, 8], F32) for i in range(2)]
        kb.dma("sp", gb[:, :], cx.norm_mix_g[layer:layer + 1, :].broadcast_to([128, D]), writes=["gb"])
        kb.dma("sp", gbias[:, :], cx.mlstm_gate_b[li:li + 1, :].broadcast_to([128, 8]), writes=["gbias"])
        wv_all = cx.mlstm_w_in[li].rearrange("(kt p) n -> p kt n", p=128)
        sc_ = [0]
        for tt in range(T // TT):
            tbase = tt * TT
            for tb in range(NTB):
                t0 = tbase + tb * 128
                rmsnorm_to_xT(kb, cx, "m1", h_in[t0:t0 + 128, :], gb, xT, tb, hb[tb % 2], hk=tb % 2)
            rmsnorm_flush(cx)

            def epi_qk(m, th, ps, pk):
                si = sc_[0] % 4
                sc_[0] += 1
                s_ = st[si]; sk = ("st", si)
                sc = 1.0 / 16.0 if m < 8 else 1.0
                kb.op("act", lambda e: e.mul(out=s_[:, :], in_=ps[:, :], mul=sc), reads=[pk], writes=[sk])
                kb.dma("sp", cx.s_qkT[m * 128:(m + 1) * 128, tbase + th * 512:tbase + (th + 1) * 512], s_[:, :],
                       reads=[sk])

            gemm_fm(kb, cx, ws, wv_all, 0, 4, xT, TT, [0, 1], epi_qk)

            def epi_kvo(cb, tb, ps, pk):
                si = sc_[0] % 4
                sc_[0] += 1
                s_ = st[si]; sk = ("st", si)
                fn = AF.Sigmoid if cb >= 6 else AF.Identity
                kb.op("act", lambda e: e.activation(out=s_[:, :], in_=ps[:, :], func=fn), reads=[pk], writes=[sk])
                t0 = tbase + tb * 128
                kb.dma("sp", cx.s_kvo[t0:t0 + 128, cb * 512:(cb + 1) * 512], s_[:, :], reads=[sk])

            gemm_tm(kb, cx, ws, wv_all, 1024, 10, 512, xT, TT, [2, 3], epi_kvo)

            def epi_g(cb, tb, ps, pk):
                g_ = gt[tb % 2]; gk = ("gt", tb % 2)
                kb.op("dve", lambda e: e.tensor_tensor(out=g_[:, :], in0=ps[:, 0:8], in1=gbias[:, :], op=ALU.add),
                      reads=[pk, "gbias"], writes=[gk])
                kb.op("act", lambda e: e.activation(out=g_[:, 4:8], in_=g_[:, 4:8], func=AF.Exp, scale=-1.0),
                      reads=[gk], writes=[gk])
                kb.op("act", lambda e: e.activation(out=g_[:, 4:8], in_=g_[:, 4:8], func=AF.Ln,
                                                    bias=cx.onec[:, 0:1], scale=1.0), reads=[gk], writes=[gk])
                kb.op("dve", lambda e: e.tensor_scalar(g_[:, 4:8], g_[:, 4:8], -1.0, None, ALU.mult),
                      reads=[gk], writes=[gk])
                t0 = tbase + tb * 128
                kb.dma("sp", cx.s_gates[t0:t0 + 128, :], g_[:, :], reads=[gk])

            gemm_tm(kb, cx, ws, wv_all, 6144, 1, 8, xT, TT, [4, 5], epi_g)
        kb.barrier()


def mlstm_scan_phase(kb, cx, li, T, SEQ):
    nc = kb.nc
    with ExitStack() as es:
        sb = lambda name, shape, dt: es.enter_context(nc.sbuf_tensor(_uniq(name), shape, dt))
        C = sb("m2_C", [128, 8, 512], F32)
        Cbf = sb("m2_Cbf", [128, 8, 512], BF16)
        nst = sb("m2_n", [128, 8], F32)
        nbf = sb("m2_nbf", [128, 8], BF16)
        mst = sb("m2_mst", [128, 4], F32)
        qT = [sb(f"m2_qT{i}", [128, 8, 128], BF16) for i in range(2)]
        kT = [sb(f"m2_kT{i}", [128, 8, 128], BF16) for i in range(2)]
        kvo = [sb(f"m2_kvo{i}", [128, 5120], BF16) for i in range(2)]
        gts = [sb(f"m2_g{i}", [128, 8], F32) for i in range(2)]
        sm = sb("m2_sm", [128, 8], F32)
        r1 = sb("m2_r1", [128, 512], F32)
        r2 = sb("m2_r2", [128, 512], F32)
        r3 = sb("m2_r3", [128, 512], F32)
        r4 = sb("m2_r4", [128, 512], F32)
        tle = sb("m2_tle", [128, 128], F32)
        one = sb("m2_one", [128, 128], F32)
        idf = sb("m2_idf", [128, 128], F32)
        sel = sb("m2_sel", [128, 128], F32)
        oneb = sb("m2_oneb", [128, 1], BF16)
        v4 = {n: sb("m2_" + n, [128, 4], F32) for n in
              ("mx", "inter", "mt", "negm", "si", "emt", "den", "qn", "rden", "sir", "mnew", "tv", "wk", "cs", "rs")}
        w = sb("m2_w", [128, 512], F32)
        P = sb("m2_P", [128, 512], BF16)
        PT = sb("m2_PT", [128, 512], BF16)
        tmp = [sb(f"m2_tmp{i}", [128, 512], F32) for i in range(2)]
        hh = sb("m2_hh", [128, 2048], F32)
        hg = sb("m2_hg", [128, 2048], F32)
        hgo = sb("m2_hgo", [128, 2048], F32)
        hmix = sb("m2_hmix", [128, 2048], BF16)
        hmT = [sb(f"m2_hmT{i}", [128, 16, 128], BF16) for i in range(2)]
        kw = sb("m2_kw", [128, 1024], BF16)
        kb.dma("sp", hg[:, :], cx.mlstm_head_g[li:li + 1, :].broadcast_to([128, 2048]), writes=["hg"])
        kb.dma("sp", tle[:, :], cx.c_tri_le[:, :], writes=["tle"])
        kb.dma("sp", one[:, :], cx.c_ones_f[:, :], writes=["one"])
        kb.dma("sp", idf[:, :], cx.c_ident_f[:, :], writes=["idf"])
        kb.dma("sp", sel[:, :], cx.c_sel_last[:, :], writes=["sel"])
        kb.dma("sp", r4[:, :].rearrange("p (h s) -> p h s", h=4),
               cx.c_negmask[:, :].rearrange("p (o s) -> p o s", o=1).broadcast_to([128, 4, 128]), writes=["r4"])
        kb.op("dve", lambda e: e.memset(oneb[:, :], 1.0), writes=["oneb"])
        qTv = cx.s_qkT[0:1024, :].rearrange("(k p) t -> p k t", p=128)
        kTv = cx.s_qkT[1024:2048, :].rearrange("(k p) t -> p k t", p=128)
        ps0 = cx.psum[0]
        V = lambda n: v4[n]
        for ci in range(T // 128):
            t0 = ci * 128
            b = ci % 2
            if t0 % SEQ == 0:
                kb.op("dve", lambda e: e.memset(C[:, :, :], 0.0), writes=[("C", h) for h in range(4)])
                kb.op("pool", lambda e: e.memset(Cbf[:, :, :], 0.0), writes=[("Cbf", h) for h in range(4)])
                kb.op("dve", lambda e: e.memset(nst[:, :], 0.0), writes=["n"])
                kb.op("dve", lambda e: e.memset(nbf[:, :], 0.0), writes=["nbf"])
                kb.op("dve", lambda e: e.memset(mst[:, :], 0.0), writes=["mst"])
            kb.dma("sp", qT[b][:, :, :], qTv[:, :, t0:t0 + 128], writes=[("qT", b)])
            kb.dma("sp", kT[b][:, :, :], kTv[:, :, t0:t0 + 128], writes=[("kT", b)])
            kb.dma("sp", kvo[b][:, :], cx.s_kvo[t0:t0 + 128, :], writes=[("kvo", b)])
            kb.dma("sp", gts[b][:, :], cx.s_gates[t0:t0 + 128, :], writes=[("g", b)])
            g_ = gts[b]
            gk = ("g", b)
            lf = g_[:, 4:8]
            ig = g_[:, 0:4]
            kb.op("pe", lambda e: e.matmul(ps0[:, 0:4], lhsT=tle[:, :], rhs=lf, start=True, stop=True),
                  reads=["tle", gk], writes=[("ps", 0)])
            kb.op("pe", lambda e: e.matmul(ps0[:, 4:8], lhsT=one[:, :], rhs=lf, start=True, stop=True),
                  reads=["one", gk], writes=[("ps", 0)])
            kb.op("act", lambda e: e.copy(out=sm[:, :], in_=ps0[:, 0:8]), reads=[("ps", 0)], writes=["sm"])
            lfb = lf.rearrange("p (h o) -> p h o", o=1).broadcast_to([128, 4, 128])
            igb = ig.rearrange("p (h o) -> p h o", o=1).broadcast_to([128, 4, 128])
            tleb = tle[:, :].rearrange("p (o s) -> p o s", o=1).broadcast_to([128, 4, 128])
            idb = idf[:, :].rearrange("p (o s) -> p o s", o=1).broadcast_to([128, 4, 128])
            r13 = lambda r: r[:, :].rearrange("p (h s) -> p h s", h=4)
            kb.op("dve", lambda e: e.tensor_copy(r13(r1), lfb), reads=[gk], writes=["r1"])
            kb.op("dve", lambda e: e.scalar_tensor_tensor(out=r13(r2), in0=tleb, scalar=-1.0, in1=lfb,
                                                          op0=ALU.mult, op1=ALU.mult),
                  reads=[gk, "tle"], writes=["r2"])
            kb.op("pool", lambda e: e.tensor_tensor(out=r13(r3), in0=igb, in1=idb, op=ALU.mult),
                  reads=[gk, "idf"], writes=["r3"])
            psi = cx.psum[1]
            kb.op("pe", lambda e: e.matmul(psi[:, :], lhsT=tle[:, :], rhs=r1[:, :], start=True, stop=False),
                  reads=["tle", "r1"], writes=[("ps", 1)])
            kb.op("pe", lambda e: e.matmul(psi[:, :], lhsT=one[:, :], rhs=r2[:, :], start=False, stop=False),
                  reads=["one", "r2"], writes=[("ps", 1)])
            kb.op("pe", lambda e: e.matmul(psi[:, :], lhsT=one[:, :], rhs=r3[:, :], start=False, stop=False),
                  reads=["one", "r3"], writes=[("ps", 1)])
            kb.op("pe", lambda e: e.matmul(psi[:, :], lhsT=idf[:, :], rhs=r4[:, :], start=False, stop=True),
                  reads=["idf", "r4"], writes=[("ps", 1)])
            kb.op("dve", lambda e: e.tensor_reduce(out=V("mx")[:, :], in_=psi[:, :].rearrange("p (h s) -> p h s", h=4),
                                                   axis=AX.X, op=ALU.max), reads=[("ps", 1)], writes=["mx"])
            kb.op("dve", lambda e: e.tensor_tensor(out=V("inter")[:, :], in0=sm[:, 0:4], in1=mst[:, :], op=ALU.add),
                  reads=["sm", "mst"], writes=["inter"])
            kb.op("dve", lambda e: e.tensor_tensor(out=V("mt")[:, :], in0=V("mx")[:, :], in1=V("inter")[:, :],
                                                   op=ALU.max), reads=["mx", "inter"], writes=["mt"])
            kb.op("dve", lambda e: e.tensor_scalar(V("negm")[:, :], V("mt")[:, :], -1.0, None, ALU.mult),
                  reads=["mt"], writes=["negm"])
            kb.op("dve", lambda e: e.tensor_tensor(out=V("si")[:, :], in0=V("inter")[:, :], in1=V("mt")[:, :],
                                                   op=ALU.subtract), reads=["inter", "mt"], writes=["si"])
            kb.op("act", lambda e: e.activation(out=V("si")[:, :], in_=V("si")[:, :], func=AF.Exp),
                  reads=["si"], writes=["si"])
            kb.op("act", lambda e: e.activation(out=V("emt")[:, :], in_=V("negm")[:, :], func=AF.Exp),
                  reads=["negm"], writes=["emt"])
            for h in range(4):
                kb.op("act", lambda e, h=h: e.activation(out=w[:, h * 128:(h + 1) * 128],
                                                         in_=psi[:, h * 128:(h + 1) * 128], func=AF.Exp,
                                                         bias=V("negm")[:, h:h + 1], scale=1.0),
                      reads=[("ps", 1), "negm"], writes=["w"])
            psq = cx.psum[2]
            for h in range(4):
                for kk in range(2):
                    kb.op("pe", lambda e, h=h, kk=kk: e.matmul(
                        psq[:, h * 128:(h + 1) * 128], lhsT=qT[b][:, h * 2 + kk, :], rhs=kT[b][:, h * 2 + kk, :],
                        start=(kk == 0), stop=(kk == 1)), reads=[("qT", b), ("kT", b)], writes=[("ps", 2)])
            for h in range(4):
                kb.op("dve", lambda e, h=h: e.scalar_tensor_tensor(
                    out=P[:, h * 128:(h + 1) * 128], in0=psq[:, h * 128:(h + 1) * 128], scalar=1.0,
                    in1=w[:, h * 128:(h + 1) * 128], op0=ALU.mult, op1=ALU.mult, accum_out=V("den")[:, h:h + 1]),
                    reads=[("ps", 2), "w"], writes=["P", "den"])
            for h in range(4):
                for kk in range(2):
                    kb.op("pe", lambda e, h=h, kk=kk: e.matmul(
                        ps0[:, 16 + h:17 + h], lhsT=qT[b][:, h * 2 + kk, :], rhs=nbf[:, h * 2 + kk:h * 2 + kk + 1],
                        start=(kk == 0), stop=(kk == 1)), reads=[("qT", b), "nbf"], writes=[("ps", 0)])
            kb.op("dve", lambda e: e.tensor_tensor(out=V("qn")[:, :], in0=ps0[:, 16:20], in1=V("si")[:, :],
                                                   op=ALU.mult), reads=[("ps", 0), "si"], writes=["qn"])
            kb.op("dve", lambda e: e.tensor_tensor(out=V("den")[:, :], in0=V("den")[:, :], in1=V("qn")[:, :],
                                                   op=ALU.add), reads=["den", "qn"], writes=["den"])
            kb.op("dve", lambda e: e.tensor_scalar(V("qn")[:, :], V("den")[:, :], -1.0, None, ALU.mult),
                  reads=["den"], writes=["qn"])
            kb.op("dve", lambda e: e.tensor_tensor(out=V("den")[:, :], in0=V("den")[:, :], in1=V("qn")[:, :],
                                                   op=ALU.max), reads=["den", "qn"], writes=["den"])
            kb.op("dve", lambda e: e.tensor_tensor(out=V("den")[:, :], in0=V("den")[:, :], in1=V("emt")[:, :],
                                                   op=ALU.max), reads=["den", "emt"], writes=["den"])
            kb.op("dve", lambda e: e.reciprocal(V("rden")[:, :], V("den")[:, :]), reads=["den"], writes=["rden"])
            kb.op("dve", lambda e: e.tensor_tensor(out=V("sir")[:, :], in0=V("si")[:, :], in1=V("rden")[:, :],
                                                   op=ALU.mult), reads=["si", "rden"], writes=["sir"])
            pt = cx.ptr[0]
            for h in range(4):
                kb.op("pe", lambda e, h=h: e.transpose(out=pt[:, h * 128:(h + 1) * 128],
                                                       in_=P[:, h * 128:(h + 1) * 128], identity=cx.ident_bf[:, :]),
                      reads=["P", "ident"], writes=[("ptr", 0)])
            kb.op("act", lambda e: e.copy(out=PT[:, :], in_=pt[:, 0:512]), reads=[("ptr", 0)], writes=["PT"])
            for h in range(4):
                psa = cx.psum[3]
                psb = cx.psum[4]
                kb.op("pe", lambda e, h=h: e.matmul(psa[:, :], lhsT=PT[:, h * 128:(h + 1) * 128],
                                                    rhs=kvo[b][:, 1024 + h * 512:1024 + (h + 1) * 512],
                                                    start=True, stop=True),
                      reads=["PT", ("kvo", b)], writes=[("ps", 3)])
                for kk in range(2):
                    kb.op("pe", lambda e, h=h, kk=kk: e.matmul(psb[:, :], lhsT=qT[b][:, h * 2 + kk, :],
                                                               rhs=Cbf[:, h * 2 + kk, :], start=(kk == 0),
                                                               stop=(kk == 1)),
                          reads=[("qT", b), ("Cbf", h)], writes=[("ps", 4)])
                tm = tmp[h % 2]
                kb.op("act", lambda e, h=h, tm=tm: e.activation(out=tm[:, :], in_=psb[:, :], func=AF.Identity,
                                                                scale=V("sir")[:, h:h + 1]),
                      reads=[("ps", 4), "sir"], writes=[("tmp", h % 2)])
                kb.op("dve", lambda e, h=h, tm=tm: e.scalar_tensor_tensor(
                    out=hh[:, h * 512:(h + 1) * 512], in0=psa[:, :], scalar=V("rden")[:, h:h + 1], in1=tm[:, :],
                    op0=ALU.mult, op1=ALU.add), reads=[("ps", 3), "rden", ("tmp", h % 2)], writes=[("hh", h)])
            hkeys = [("hh", h) for h in range(4)]
            for h in range(4):
                kb.op("act", lambda e, h=h: e.activation(out=cx.junk4[:, 0:512], in_=hh[:, h * 512:(h + 1) * 512],
                                                         func=AF.Square, accum_out=V("rs")[:, h:h + 1]),
                      reads=[("hh", h)], writes=["junk4", "rs"])
            kb.op("act", lambda e: e.activation(out=V("rs")[:, :], in_=V("rs")[:, :], func=AF.Sqrt,
                                                scale=1.0 / 512, bias=cx.epsc[:, 0:1]), reads=["rs"], writes=["rs"])
            kb.op("dve", lambda e: e.reciprocal(V("rs")[:, :], V("rs")[:, :]), reads=["rs"], writes=["rs"])
            kb.op("pool", lambda e: e.tensor_tensor(out=hgo[:, :], in0=hg[:, :], in1=kvo[b][:, 3072:5120],
                                                    op=ALU.mult), reads=["hg", ("kvo", b)], writes=["hgo"])
            for h in range(4):
                kb.op("dve", lambda e, h=h: e.scalar_tensor_tensor(
                    out=hmix[:, h * 512:(h + 1) * 512], in0=hh[:, h * 512:(h + 1) * 512],
                    scalar=V("rs")[:, h:h + 1], in1=hgo[:, h * 512:(h + 1) * 512], op0=ALU.mult, op1=ALU.mult),
                    reads=[("hh", h), "rs", "hgo"], writes=["hmix"])
            hT = hmT[b]
            for q in range(2):
                ptq = cx.ptr[1] if q == 0 else cx.ptr[0]
                pk = ("ptr", 1) if q == 0 else ("ptr", 0)
                for j in range(8):
                    kt = q * 8 + j
                    kb.op("pe", lambda e, kt=kt, j=j, ptq=ptq: e.transpose(
                        out=ptq[:, j * 128:(j + 1) * 128], in_=hmix[:, kt * 128:(kt + 1) * 128],
                        identity=cx.ident_bf[:, :]), reads=["hmix", "ident"], writes=[pk])
                kb.op("act", lambda e, q=q, ptq=ptq, hT=hT: e.copy(
                    out=hT[:, q * 8:(q + 1) * 8, :], in_=ptq[:, :].rearrange("p (k t) -> p k t", k=8)),
                    reads=[pk], writes=[("hmT", b)])
            kb.dma("sp", cx.s_ynT[0:2048, :].rearrange("(k p) t -> p k t", p=128)[:, :, t0:t0 + 128], hT[:, :, :],
                   reads=[("hmT", b)])
            kb.op("pe", lambda e: e.matmul(ps0[:, 24:28], lhsT=sel[:, :], rhs=V("mt")[:, :], start=True, stop=True),
                  reads=["sel", "mt"], writes=[("ps", 0)])
            kb.op("act", lambda e: e.copy(out=V("mnew")[:, :], in_=ps0[:, 24:28]), reads=[("ps", 0)],
                  writes=["mnew"])
            kb.op("dve", lambda e: e.tensor_tensor(out=V("tv")[:, :], in0=sm[:, 4:8], in1=sm[:, 0:4],
                                                   op=ALU.subtract), reads=["sm"], writes=["tv"])
            kb.op("dve", lambda e: e.tensor_tensor(out=V("tv")[:, :], in0=V("tv")[:, :], in1=ig, op=ALU.add),
                  reads=["tv", gk], writes=["tv"])
            kb.op("dve", lambda e: e.tensor_tensor(out=V("tv")[:, :], in0=V("tv")[:, :], in1=V("mnew")[:, :],
                                                   op=ALU.subtract), reads=["tv", "mnew"], writes=["tv"])
            kb.op("act", lambda e: e.activation(out=V("wk")[:, :], in_=V("tv")[:, :], func=AF.Exp),
                  reads=["tv"], writes=["wk"])
            kb.op("dve", lambda e: e.tensor_tensor(out=V("cs")[:, :], in0=sm[:, 4:8], in1=mst[:, :], op=ALU.add),
                  reads=["sm", "mst"], writes=["cs"])
            kb.op("dve", lambda e: e.tensor_tensor(out=V("cs")[:, :], in0=V("cs")[:, :], in1=V("mnew")[:, :],
                                                   op=ALU.subtract), reads=["cs", "mnew"], writes=["cs"])
            kb.op("act", lambda e: e.activation(out=V("cs")[:, :], in_=V("cs")[:, :], func=AF.Exp),
                  reads=["cs"], writes=["cs"])
            kb.op("dve", lambda e: e.tensor_tensor(
                out=kw[:, :].rearrange("p (h d) -> p h d", h=4),
                in0=kvo[b][:, 0:1024].rearrange("p (h d) -> p h d", h=4),
                in1=V("wk")[:, :].rearrange("p (h o) -> p h o", o=1).broadcast_to([128, 4, 256]), op=ALU.mult),
                reads=[("kvo", b), "wk"], writes=["kw"])
            psc = cx.psum[5]
            for h in range(4):
                for kk in range(2):
                    j = h * 2 + kk
                    kb.op("pe", lambda e, h=h, j=j: e.matmul(
                        psc[:, :], lhsT=kw[:, j * 128:(j + 1) * 128],
                        rhs=kvo[b][:, 1024 + h * 512:1024 + (h + 1) * 512], start=True, stop=True),
                        reads=["kw", ("kvo", b)], writes=[("ps", 5)])
                    kb.op("dve", lambda e, h=h, j=j: e.scalar_tensor_tensor(
                        out=C[:, j, :], in0=C[:, j, :], scalar=V("cs")[:, h:h + 1], in1=psc[:, :],
                        op0=ALU.mult, op1=ALU.add), reads=[("C", h), "cs", ("ps", 5)], writes=[("C", h)])
                    kb.op("act", lambda e, j=j: e.copy(out=Cbf[:, j, :], in_=C[:, j, :]), reads=[("C", h)],
                          writes=[("Cbf", h)])
                    kb.op("pe", lambda e, j=j: e.matmul(ps0[:, 32 + j:33 + j], lhsT=kw[:, j * 128:(j + 1) * 128],
                                                        rhs=oneb[:, 0:1], start=True, stop=True),
                          reads=["kw", "oneb"], writes=[("ps", 0)])
            for h in range(4):
                kb.op("dve", lambda e, h=h: e.scalar_tensor_tensor(
                    out=nst[:, h * 2:h * 2 + 2], in0=nst[:, h * 2:h * 2 + 2], scalar=V("cs")[:, h:h + 1],
                    in1=ps0[:, 32 + h * 2:34 + h * 2], op0=ALU.mult, op1=ALU.add),
                    reads=["n", "cs", ("ps", 0)], writes=["n"])
            kb.op("dve", lambda e: e.tensor_copy(nbf[:, :], nst[:, :]), reads=["n"], writes=["nbf"])
            kb.op("dve", lambda e: e.tensor_copy(mst[:, :], V("mnew")[:, :]), reads=["mnew"], writes=["mst"])
        kb.barrier()


TWO_PI_LO = 6.283185


def s5_layer(kb, cx, layer, li, h_in, h_out, T, SEQ):
    s5_inproj_phase(kb, cx, layer, li, h_in, T)
    s5_scan_phase(kb, cx, li, T, SEQ)
    s5_out_phase(kb, cx, li, h_in, h_out, T)


def s5_inproj_phase(kb, cx, layer, li, h_in, T):
    nc = kb.nc
    TT = 1024 if T % 1024 == 0 else 512
    NTB = TT // 128
    with ExitStack() as es:
        sb = lambda name, shape, dt: es.enter_context(nc.sbuf_tensor(_uniq(name), shape, dt))
        hb = [sb(f"p1_hb{i}", [128, D], F32) for i in range(2)]
        xT = sb("p1_xT", [128, 16, TT], BF16)
        ws = WStream(kb, [sb(f"p1_wb{i}", [128, 16 * 512], BF16) for i in range(2)])
        gb = sb("p1_gb", [128, D], F32)
        st = [sb(f"p1_st{i}", [128, 512], BF16) for i in range(4)]
        kb.dma("sp", gb[:, :], cx.norm_mix_g[layer:layer + 1, :].broadcast_to([128, D]), writes=["gb"])
        wv_all = cx.s5_w_in[li].rearrange("(kt p) n -> p kt n", p=128)
        sc_ = [0]
        for tt in range(T // TT):
            tbase = tt * TT
            for tb in range(NTB):
                t0 = tbase + tb * 128
                rmsnorm_to_xT(kb, cx, "p1", h_in[t0:t0 + 128, :], gb, xT, tb, hb[tb % 2], hk=tb % 2)
            rmsnorm_flush(cx)

            def epi_u(m, th, ps, pk):
                si = sc_[0] % 4
                sc_[0] += 1
                s_ = st[si]; sk = ("st", si)
                kb.op("act", lambda e: e.copy(out=s_[:, :], in_=ps[:, :]), reads=[pk], writes=[sk])
                kb.dma("sp", cx.s_qkT[m * 128:(m + 1) * 128, tbase + th * 512:tbase + (th + 1) * 512], s_[:, :],
                       reads=[sk])

            gemm_fm(kb, cx, ws, wv_all, 0, 4, xT, TT, [0, 1, 2, 3], epi_u)
        kb.barrier()


def s5_scan_phase(kb, cx, li, T, SEQ):
    nc = kb.nc
    NT = T // 512
    with ExitStack() as es:
        sb = lambda name, shape, dt: es.enter_context(nc.sbuf_tensor(_uniq(name), shape, dt))
        f64 = lambda n: sb("p2_" + n, [128, 64], F32)
        ar, ai, th, lm, mag, cs_, sn_, m1, den, zr, zi, tA, tB, xx, kf = [f64(n) for n in (
            "ar", "ai", "th", "lm", "mag", "cs", "sn", "m1", "den", "zr", "zi", "tA", "tB", "xx", "kf")]
        ki = sb("p2_ki", [128, 64], I32)
        dtc = sb("p2_dtc", [128, 1], F32)
        dup = sb("p2_dup", [128, 128], F32)
        idf = sb("p2_idf", [128, 128], F32)
        swm = sb("p2_swm", [128, 128], F32)
        gmask = sb("p2_gmask", [128, 8], F32)
        cosT = sb("p2_cosT", [128, 128], F32)
        sinT = sb("p2_sinT", [128, 128], F32)
        magT = sb("p2_magT", [128, 128], F32)
        zrT = sb("p2_zrT", [64, 128], F32)
        ziT = sb("p2_ziT", [64, 128], F32)
        D1a = sb("p2_D1a", [128, 16, 128], F32)
        D2a = sb("p2_D2a", [128, 16, 128], F32)
        LBk1 = [sb(f"p2_LBk1{i}", [128, 8, 128], BF16) for i in range(2)]
        LBk2 = [sb(f"p2_LBk2{i}", [128, 8, 128], BF16) for i in range(2)]
        LC1 = sb("p2_LC1", [128, 128, 16], BF16)
        LC2 = sb("p2_LC2", [128, 128, 16], BF16)
        brn = sb("p2_brn", [64, 128], F32)
        bin_ = sb("p2_bin", [64, 128], F32)
        bbr = sb("p2_bbr", [64, 128], F32)
        bbi = sb("p2_bbi", [64, 128], F32)
        bt1 = sb("p2_bt1", [64, 128], F32)
        bt2 = sb("p2_bt2", [64, 128], F32)
        in1 = sb("p2_in1", [128, 128], F32)
        in2 = sb("p2_in2", [128, 128], F32)
        cin = sb("p2_cin", [128, 64], F32)
        kb.dma("sp", idf[:, :], cx.c_ident_f[:, :], writes=["idf"])
        kb.dma("sp", swm[:, :], cx.c_swap[:, :], writes=["swm"])
        kb.dma("sp", gmask[:, :], cx.c_gmask[:, :], writes=["gmask"])
        kb.dma("sp", ar[:, :], cx.s5_a_re[li], writes=["ar"])
        kb.dma("sp", ai[:, :], cx.s5_a_im[li], writes=["ai"])
        kb.dma("sp", dtc[:, :], cx.s5_log_dt[li].rearrange("(g o) -> g o", o=1), writes=["dtc"])
        dv = lambda fn, r, w: kb.op("dve", fn, reads=r, writes=w)
        ac = lambda fn, r, w: kb.op("act", fn, reads=r, writes=w)
        ac(lambda e: e.activation(out=dtc[:, :], in_=dtc[:, :], func=AF.Exp), ["dtc"], ["dtc"])
        dv(lambda e: e.tensor_scalar(th[:, :], ai[:, :], dtc[:, 0:1], None, ALU.mult), ["ai", "dtc"], ["th"])
        dv(lambda e: e.tensor_scalar(lm[:, :], ar[:, :], dtc[:, 0:1], None, ALU.mult), ["ar", "dtc"], ["lm"])
        ac(lambda e: e.activation(out=mag[:, :], in_=lm[:, :], func=AF.Exp), ["lm"], ["mag"])

        def sincos(dst, dkey, shift):
            c0 = 0.5 + shift / (2.0 * np.pi)
            dv(lambda e: e.tensor_scalar(xx[:, :], th[:, :], float(1.0 / (2.0 * np.pi)), float(c0), ALU.mult, ALU.add),
               ["th"], ["xx"])
            dv(lambda e: e.tensor_copy(ki[:, :], xx[:, :]), ["xx"], ["ki"])
            dv(lambda e: e.tensor_copy(kf[:, :], ki[:, :]), ["ki"], ["kf"])
            dv(lambda e: e.tensor_tensor(out=xx[:, :], in0=xx[:, :], in1=kf[:, :], op=ALU.subtract), ["xx", "kf"],
               ["xx"])
            dv(lambda e: e.tensor_scalar(kf[:, :], xx[:, :], 0.0, None, ALU.is_lt), ["xx"], ["kf"])
            dv(lambda e: e.tensor_tensor(out=xx[:, :], in0=xx[:, :], in1=kf[:, :], op=ALU.add), ["xx", "kf"], ["xx"])
            dv(lambda e: e.tensor_scalar(xx[:, :], xx[:, :], -0.5, TWO_PI_LO, ALU.add, ALU.mult), ["xx"], ["xx"])
            ac(lambda e: e.activation(out=dst[:, :], in_=xx[:, :], func=AF.Sin), ["xx"], [dkey])

        sincos(sn_, "sn", 0.0)
        sincos(cs_, "cs", np.pi / 2.0)
        TT_ = lambda o, a, b, op, r, w: dv(lambda e: e.tensor_tensor(out=o, in0=a, in1=b, op=op), r, w)
        TT_(tA[:, :], mag[:, :], cs_[:, :], ALU.mult, ["mag", "cs"], ["tA"])
        TT_(tB[:, :], mag[:, :], sn_[:, :], ALU.mult, ["mag", "sn"], ["tB"])
        dv(lambda e: e.tensor_scalar(m1[:, :], tA[:, :], -1.0, None, ALU.add), ["tA"], ["m1"])
        TT_(den[:, :], ar[:, :], ar[:, :], ALU.mult, ["ar"], ["den"])
        TT_(xx[:, :], ai[:, :], ai[:, :], ALU.mult, ["ai"], ["xx"])
        TT_(den[:, :], den[:, :], xx[:, :], ALU.add, ["den", "xx"], ["den"])
        dv(lambda e: e.reciprocal(den[:, :], den[:, :]), ["den"], ["den"])
        TT_(zr[:, :], m1[:, :], ar[:, :], ALU.mult, ["m1", "ar"], ["zr"])
        TT_(xx[:, :], tB[:, :], ai[:, :], ALU.mult, ["tB", "ai"], ["xx"])
        TT_(zr[:, :], zr[:, :], xx[:, :], ALU.add, ["zr", "xx"], ["zr"])
        TT_(zr[:, :], zr[:, :], den[:, :], ALU.mult, ["zr", "den"], ["zr"])
        TT_(zi[:, :], tB[:, :], ar[:, :], ALU.mult, ["tB", "ar"], ["zi"])
        TT_(xx[:, :], m1[:, :], ai[:, :], ALU.mult, ["m1", "ai"], ["xx"])
        TT_(zi[:, :], zi[:, :], xx[:, :], ALU.subtract, ["zi", "xx"], ["zi"])
        TT_(zi[:, :], zi[:, :], den[:, :], ALU.mult, ["zi", "den"], ["zi"])
        pst = cx.psum[5]
        for src, skey, dst, dkey in ((cs_, "cs", cosT, "cosT"), (sn_, "sn", sinT, "sinT"), (mag, "mag", magT, "magT")):
            dv(lambda e, src=src: e.tensor_copy(dup[:, 0:64], src[:, :]), [skey], ["dup"])
            dv(lambda e, src=src: e.tensor_copy(dup[:, 64:128], src[:, :]), [skey], ["dup"])
            kb.op("pe", lambda e: e.transpose(out=pst[:, 0:128], in_=dup[:, :], identity=idf[:, :]),
                  reads=["dup", "idf"], writes=[("ps", 5)])
            ac(lambda e, dst=dst: e.copy(out=dst[:, :], in_=pst[:, 0:128]), [("ps", 5)], [dkey])
        for src, skey, dst, dkey in ((zr, "zr", zrT, "zrT"), (zi, "zi", ziT, "ziT")):
            kb.op("pe", lambda e, src=src: e.transpose(out=pst[0:64, 0:128], in_=src[:, :], identity=idf[:, :]),
                  reads=[skey, "idf"], writes=[("ps", 5)])
            ac(lambda e, dst=dst: e.copy(out=dst[:, :], in_=pst[0:64, 0:128]), [("ps", 5)], [dkey])
        b3 = lambda t: t[:, :].rearrange("p (g c) -> p g c", g=8)
        for k in range(16):
            kb.dma("sp", b3(brn), cx.s5_b_re[li, 8 * k:8 * k + 8].rearrange("g p c -> p g c"), writes=["brn"])
            kb.dma("sp", b3(bin_), cx.s5_b_im[li, 8 * k:8 * k + 8].rearrange("g p c -> p g c"), writes=["bin"])
            zrb = zrT[:, 8 * k:8 * k + 8].rearrange("p (g o) -> p g o", o=1).broadcast_to([64, 8, 16])
            zib = ziT[:, 8 * k:8 * k + 8].rearrange("p (g o) -> p g o", o=1).broadcast_to([64, 8, 16])
            TT_(b3(bt1), b3(brn), zrb, ALU.mult, ["brn", "zrT"], ["bt1"])
            TT_(b3(bt2), b3(bin_), zib, ALU.mult, ["bin", "ziT"], ["bt2"])
            TT_(bbr[:, :], bt1[:, :], bt2[:, :], ALU.subtract, ["bt1", "bt2"], ["bbr"])
            TT_(b3(bt1), b3(bin_), zrb, ALU.mult, ["bin", "zrT"], ["bt1"])
            TT_(b3(bt2), b3(brn), zib, ALU.mult, ["brn", "ziT"], ["bt2"])
            TT_(bbi[:, :], bt1[:, :], bt2[:, :], ALU.add, ["bt1", "bt2"], ["bbi"])
            kb.op("pe", lambda e: e.transpose(out=pst[:, 0:64], in_=bbr[:, :], identity=idf[0:64, 0:64]),
                  reads=["bbr", "idf"], writes=[("ps", 5)])
            kb.op("pe", lambda e: e.transpose(out=pst[:, 64:128], in_=bbi[:, :], identity=idf[0:64, 0:64]),
                  reads=["bbi", "idf"], writes=[("ps", 5)])
            ac(lambda e, k=k: e.copy(out=D1a[:, k, :], in_=pst[:, 0:128]), [("ps", 5)], [("D1", k)])
            ac(lambda e, k=k: e.copy(out=D2a[:, k, 0:64], in_=pst[:, 64:128]), [("ps", 5)], [("D2", k)])
            ac(lambda e, k=k: e.mul(out=D2a[:, k, 64:128], in_=pst[:, 0:64], mul=-1.0), [("ps", 5)], [("D2", k)])
            kb.dma("sp", in1[:, 0:64], cx.s5_c_re[li, 8 * k:8 * k + 8].rearrange("g c p -> (g c) p"), writes=["in1"])
            kb.dma("sp", cin[:, :], cx.s5_c_im[li, 8 * k:8 * k + 8].rearrange("g c p -> (g c) p"), writes=["cin"])
            dv(lambda e: e.tensor_scalar(in1[:, 64:128], cin[:, :], -1.0, None, ALU.mult), ["cin"], ["in1"])
            dv(lambda e: e.tensor_scalar(in2[:, 0:64], cin[:, :], -1.0, None, ALU.mult), ["cin"], ["in2"])
            dv(lambda e: e.tensor_scalar(in2[:, 64:128], in1[:, 0:64], -1.0, None, ALU.mult), ["in1"], ["in2"])
            for inx, ik, LC in ((in1, "in1", LC1), (in2, "in2", LC2)):
                kb.op("pe", lambda e, inx=inx: e.transpose(out=pst[:, 256:384], in_=inx[:, :], identity=idf[:, :]),
                      reads=[ik, "idf"], writes=[("ps", 5)])
                ac(lambda e, LC=LC, k=k: e.copy(out=LC[:, 8 * k:8 * k + 8, :],
                                                in_=pst[:, 256:384].rearrange("p (g c) -> p g c", g=8)),
                   [("ps", 5)], ["LC"])
        uT = [sb(f"p2_uT{i}", [128, T], BF16) for i in range(2)]
        TcA = [sb(f"p2_TcA{i}", [128, 8, 512], F32) for i in range(2)]
        TsA = [sb(f"p2_TsA{i}", [128, 8, 512], F32) for i in range(2)]
        ttmp = [sb(f"p2_ttmp{i}", [128, 8, 256], F32) for i in range(2)]
        t1 = [sb(f"p2_t1{i}", [128, 512], F32) for i in range(2)]
        t2 = [sb(f"p2_t2{i}", [128, 512], F32) for i in range(2)]
        vv = [sb(f"p2_v{i}", [128, 512], F32) for i in range(2)]
        rr = [sb(f"p2_r{i}", [128, 512], F32) for i in range(2)]
        X1 = [sb(f"p2_X1{i}", [128, 512], BF16) for i in range(2)]
        X2 = [sb(f"p2_X2{i}", [128, 512], BF16) for i in range(2)]
        stt = sb("p2_st", [128, 2], F32)
        stmp = sb("p2_stmp", [128, 2], F32)
        yst = [sb(f"p2_yst{i}", [16, 512], F32) for i in range(2)]
        def table_steps(k):
            tcv, tsv = TcA[k % 2], TsA[k % 2]
            tck = ("tab", k % 2)
            tA, tB = ttmp[0], ttmp[1]
            steps = []

            def expand():
                gmb = gmask[:, :].rearrange("p (g o) -> p g o", o=1).broadcast_to([128, 8, 128])
                for Da, dk, LB in ((D1a, "D1", LBk1[k % 2]), (D2a, "D2", LBk2[k % 2])):
                    dv(lambda e, Da=Da, LB=LB: e.tensor_tensor(
                        out=LB[:, :, :], in0=Da[:, k:k + 1, :].broadcast_to([128, 8, 128]), in1=gmb, op=ALU.mult),
                       [(dk, k), "gmask"], [("LB", k % 2)])
            steps.append(expand)

            def init():
                ac(lambda e: e.copy(out=tcv[:, :, 0:1], in_=cosT[:, 8 * k:8 * k + 8].rearrange("p (g o) -> p g o", o=1)),
                   ["cosT"], [tck])
                ac(lambda e: e.copy(out=tsv[:, :, 0:1], in_=sinT[:, 8 * k:8 * k + 8].rearrange("p (g o) -> p g o", o=1)),
                   ["sinT"], [tck])
            steps.append(init)
            n = 1
            while n < 512:
                def step(n=n):
                    cn = tcv[:, :, n - 1:n].broadcast_to([128, 8, n])
                    sn = tsv[:, :, n - 1:n].broadcast_to([128, 8, n])
                    A_ = tcv[:, :, 0:n]
                    B_ = tsv[:, :, 0:n]
                    dv(lambda e: e.tensor_tensor(out=tA[:, :, 0:n], in0=A_, in1=cn, op=ALU.mult), [tck], ["ttA"])
                    dv(lambda e: e.tensor_tensor(out=tB[:, :, 0:n], in0=B_, in1=sn, op=ALU.mult), [tck], ["ttB"])
                    dv(lambda e: e.tensor_tensor(out=tcv[:, :, n:2 * n], in0=tA[:, :, 0:n], in1=tB[:, :, 0:n],
                                                 op=ALU.subtract), ["ttA", "ttB", tck], ["tabtmp"])
                    dv(lambda e: e.tensor_tensor(out=tA[:, :, 0:n], in0=B_, in1=cn, op=ALU.mult), [tck, "tabtmp"],
                       ["ttA"])
                    dv(lambda e: e.tensor_tensor(out=tB[:, :, 0:n], in0=A_, in1=sn, op=ALU.mult), [tck, "tabtmp"],
                       ["ttB"])
                    dv(lambda e: e.tensor_tensor(out=tsv[:, :, n:2 * n], in0=tA[:, :, 0:n], in1=tB[:, :, 0:n],
                                                 op=ALU.add), ["ttA", "ttB", "tabtmp"], [tck])
                steps.append(step)
                n *= 2
            return steps

        def make_iter(g, tt, ub, uk, idx):
            k_ = g // 8
            tc, ts = TcA[k_ % 2][:, g % 8, :], TsA[k_ % 2][:, g % 8, :]
            tck = ("tab", k_ % 2)
            sp_ = g % 2
            stk = ("st", sp_)
            b = idx % 2
            pa, pb = cx.psum[b * 2], cx.psum[b * 2 + 1]
            pka, pkb = ("ps", b * 2), ("ps", b * 2 + 1)

            def st_a():
                kb.op("pe", lambda e: e.matmul(pa[:, :], lhsT=LBk1[k_ % 2][:, g % 8, :],
                                               rhs=ub[:, tt * 512:(tt + 1) * 512],
                                               start=True, stop=True), reads=[("LB", k_ % 2), uk], writes=[pka])
                kb.op("pe", lambda e: e.matmul(pb[:, :], lhsT=LBk2[k_ % 2][:, g % 8, :],
                                               rhs=ub[:, tt * 512:(tt + 1) * 512],
                                               start=True, stop=True), reads=[("LB", k_ % 2), uk], writes=[pkb])

            def st_b1():
                dv(lambda e: e.tensor_tensor(out=t1[b][:, :], in0=pa[:, :], in1=tc[:, :], op=ALU.mult),
                   [pka, tck], [("t1", b)])
                dv(lambda e: e.tensor_tensor(out=t2[b][:, :], in0=pb[:, :], in1=ts[:, :], op=ALU.mult),
                   [pkb, tck], [("t2", b)])
                dv(lambda e: e.tensor_tensor(out=vv[b][:, :], in0=t1[b][:, :], in1=t2[b][:, :], op=ALU.add),
                   [("t1", b), ("t2", b)], [("v", b)])

            def st_b2():
                if (tt * 512) % SEQ == 0:
                    dv(lambda e: e.memset(stt[:, sp_:sp_ + 1], 0.0), [], [stk])
                dv(lambda e: e.tensor_tensor_scan(
                    out=rr[b][:, :], data0=magT[:, g:g + 1].broadcast_to([128, 512]), data1=vv[b][:, :],
                    initial=stt[:, sp_:sp_ + 1], op0=ALU.mult, op1=ALU.add), [("v", b), "magT", stk], [("r", b)])
                if tt + 1 < NT and ((tt + 1) * 512) % SEQ != 0:
                    kb.op("pe", lambda e: e.matmul(pst[:, 400 + sp_:401 + sp_], lhsT=swm[:, :],
                                                   rhs=rr[b][:, 511:512], start=True, stop=True),
                          reads=["swm", ("r", b)], writes=[("ps", 5)])
                    dv(lambda e: e.tensor_tensor(out=stmp[:, sp_:sp_ + 1], in0=pst[:, 400 + sp_:401 + sp_],
                                                 in1=ts[:, 511:512], op=ALU.mult), [("ps", 5), tck],
                       [("stmp", sp_)])
                    dv(lambda e: e.scalar_tensor_tensor(
                        out=stt[:, sp_:sp_ + 1], in0=rr[b][:, 511:512], scalar=tc[:, 511:512],
                        in1=stmp[:, sp_:sp_ + 1], op0=ALU.mult, op1=ALU.add),
                        [("r", b), tck, ("stmp", sp_)], [stk])
                kb.op("pool", lambda e: e.tensor_tensor(out=X1[b][:, :], in0=rr[b][:, :], in1=tc[:, :], op=ALU.mult),
                      reads=[("r", b), tck], writes=[("X1", b)])
                kb.op("pool", lambda e: e.tensor_tensor(out=X2[b][:, :], in0=rr[b][:, :], in1=ts[:, :], op=ALU.mult),
                      reads=[("r", b), tck], writes=[("X2", b)])
                py = cx.psum[4]
                kb.op("pe", lambda e: e.matmul(py[0:16, :], lhsT=LC1[:, g, :], rhs=X1[b][:, :], start=True,
                                               stop=False), reads=["LC", ("X1", b)], writes=[("ps", 4)])
                kb.op("pe", lambda e: e.matmul(py[0:16, :], lhsT=LC2[:, g, :], rhs=X2[b][:, :], start=False,
                                               stop=True), reads=["LC", ("X2", b)], writes=[("ps", 4)])
                ys = yst[b]
                ac(lambda e: e.copy(out=ys[:, :], in_=py[0:16, :]), [("ps", 4)], [("yst", b)])
                kb.dma("sp", cx.s_y[g * 16:(g + 1) * 16, tt * 512:(tt + 1) * 512], ys[:, :], reads=[("yst", b)])
            return st_a, st_b1, st_b2

        iters = []
        for k in range(16):
            ub = uT[k % 2]; uk = ("uT", k % 2)
            nxt = []
            if k + 1 < 16:
                nxt.append(lambda k=k: kb.dma("sp", uT[(k + 1) % 2][:, :],
                                              cx.s_qkT[(k + 1) * 128:(k + 2) * 128, :], writes=[("uT", (k + 1) % 2)]))
                nxt += table_steps(k + 1)
            cnt_k = 0
            for gp in range(4):
                gs = (8 * k + 2 * gp, 8 * k + 2 * gp + 1)
                for tt in range(NT):
                    for g in gs:
                        post = []
                        if cnt_k >= 2 and nxt:
                            post.append(nxt.pop(0))
                        cnt_k += 1
                        iters.append((post, make_iter(g, tt, ub, uk, len(iters))))
            assert not nxt, "not enough iterations per k-tile to hide the next tile's table construction"
        kb.dma("sp", uT[0][:, :], cx.s_qkT[0:128, :], writes=[("uT", 0)])
        for st_ in table_steps(0):
            st_()
        n_it = len(iters)
        for step in range(n_it + 2):
            if 0 <= step - 2 < n_it:
                for p in iters[step - 2][0]:
                    p()
            if step < n_it:
                iters[step][1][0]()
            if 0 <= step - 1 < n_it:
                iters[step - 1][1][1]()
            if 0 <= step - 2 < n_it:
                iters[step - 2][1][2]()
        kb.barrier()


def s5_out_phase(kb, cx, li, h_in, h_out, T):
    nc = kb.nc
    with ExitStack() as es:
        sb = lambda name, shape, dt: es.enter_context(nc.sbuf_tensor(_uniq(name), shape, dt))
        hb = [sb(f"p3_hb{i}", [128, D], F32) for i in range(4)]
        yT = sb("p3_yT", [128, 16, 512], F32)
        uT = sb("p3_uT", [128, 16, 512], BF16)
        gT = sb("p3_gT", [128, 16, 512], BF16)
        wb = [sb(f"p3_wb{i}", [128, 16 * 512], BF16) for i in range(2)]
        dcol = sb("p3_dcol", [128, 16], F32)
        sg = [sb(f"p3_sg{i}", [128, 256], F32) for i in range(2)]
        kb.dma("sp", dcol[:, :], cx.s5_d[li].rearrange("(k p) -> p k", p=128), writes=["dcol"],
               allow_slow_non_contiguous=True)
        wv_all = cx.s5_w_out[li].rearrange("(kt p) n -> p kt n", p=128)
        yv = cx.s_y.rearrange("(k p) t -> p k t", p=128)
        uv = cx.s_qkT.rearrange("(k p) t -> p k t", p=128)
        wi = 0
        for tt in range(T // 512):
            kb.dma("sp", yT[:, :, :], yv[:, :, tt * 512:(tt + 1) * 512], writes=["yT"])
            kb.dma("sp", uT[:, :, :], uv[:, :, tt * 512:(tt + 1) * 512], writes=["uT"])
            for tb in range(4):
                t0 = tt * 512 + tb * 128
                kb.dma("sp", hb[tb][:, :], h_in[t0:t0 + 128, :], writes=[("hb", tb)])
            for k in range(16):
                kb.op("dve", lambda e, k=k: e.scalar_tensor_tensor(
                    out=yT[:, k, :], in0=uT[:, k, :], scalar=dcol[:, k:k + 1], in1=yT[:, k, :],
                    op0=ALU.mult, op1=ALU.add), reads=["uT", "yT", "dcol"], writes=["yT"])
                kb.op("act", lambda e, k=k: e.activation(out=gT[:, k, :], in_=yT[:, k, :], func=AF.Gelu_apprx_tanh),
                      reads=["yT"], writes=[("gT", k)])
            gkeys = [("gT", k) for k in range(16)]
            for cb in range(8):
                w = wb[wi % 2]; wkey = ("wb", wi % 2); wi += 1
                wv = w[:, :].rearrange("p (k n) -> p k n", k=16)
                kb.dma("pool", wv[:, :, 0:256], wv_all[:, :, cb * 256:(cb + 1) * 256], writes=[wkey])
                kb.dma("pool", wv[:, :, 256:512], wv_all[:, :, 2048 + cb * 256:2048 + (cb + 1) * 256], writes=[wkey])
                for tb in range(4):
                    pi = (cb * 4 + tb) % 4
                    ps = cx.psum[pi]; pk = ("ps", pi)
                    for half in range(2):
                        for kt in range(16):
                            kb.op("pe", lambda e, kt=kt, tb=tb, ps=ps, wv=wv, half=half: e.matmul(
                                ps[:, half * 256:(half + 1) * 256], lhsT=gT[:, kt, tb * 128:(tb + 1) * 128],
                                rhs=wv[:, kt, half * 256:(half + 1) * 256], start=(kt == 0), stop=(kt == 15)),
                                reads=[wkey] + gkeys, writes=[pk])
                    s_ = sg[pi % 2]; sk = ("sg", pi % 2)
                    kb.op("act", lambda e, ps=ps, s_=s_: e.activation(out=s_[:, :], in_=ps[:, 256:512],
                                                                      func=AF.Sigmoid), reads=[pk], writes=[sk])
                    kb.op("dve", lambda e, ps=ps, s_=s_: e.tensor_tensor(out=s_[:, :], in0=ps[:, 0:256], in1=s_[:, :],
                                                                        op=ALU.mult), reads=[pk, sk], writes=[sk])
                    kb.op("dve", lambda e, tb=tb, cb=cb, s_=s_: e.tensor_tensor(
                        out=hb[tb][:, cb * 256:(cb + 1) * 256], in0=hb[tb][:, cb * 256:(cb + 1) * 256],
                        in1=s_[:, :], op=ALU.add), reads=[sk, ("hb", tb)], writes=[("hb", tb)])
            for tb in range(4):
                t0 = tt * 512 + tb * 128
                kb.dma("sp", h_out[t0:t0 + 128, :], hb[tb][:, :], reads=[("hb", tb)])
        kb.barrier()


N_CORES = 8
SEQ_FULL = 2048
SEQ_PER_CORE = 2


def kernel(**inputs):
    x = np.asarray(inputs["x"], dtype=np.float32)
    params = {k: np.asarray(v, dtype=np.float32) for k, v in inputs.items() if k != "x"}
    T = SEQ_PER_CORE * SEQ_FULL
    nc = build_program(T, SEQ_FULL)
    in_maps = [make_in_map(x[SEQ_PER_CORE * c:SEQ_PER_CORE * (c + 1)], params) for c in range(N_CORES)]
    res = run_bass_kernel_spmd(nc, in_maps, core_ids=list(range(N_CORES)))
    outs = [np.asarray(r["out"]).reshape(SEQ_PER_CORE, SEQ_FULL, D) for r in res.results]
    return np.concatenate(outs, axis=0).astype(np.float32)


def build_program(T, SEQ, layers=(0, 1, 2, 3), do_mlp=True, do_final=True):
    nc = bass.Bass("TRN2", target_bir_lowering=False)
    cx = Ctx()
    dt = lambda name, shape, dtype=F32, kind="ExternalInput": nc.dram_tensor(name, shape, dtype, kind=kind).ap()
    cx.x = dt("x", [T, D])
    for name, shape in PARAM_SHAPES:
        setattr(cx, name, dt(name, list(shape)))
    for name, arr in _consts_np().items():
        setattr(cx, "c_" + name, dt("c_" + name, list(arr.shape), BF16 if arr.dtype != np.float32 else F32))
    cx.out = dt("out", [T, D], F32, "ExternalOutput")
    cx.hA = dt("hA", [T, D], F32, "Internal")
    cx.hB = dt("hB", [T, D], F32, "Internal")
    cx.s_zs = dt("s_zs", [T, 4096], BF16, "Internal")
    cx.s_xs = dt("s_xs", [T, 4096], BF16, "Internal")
    cx.s_Btok = dt("s_Btok", [T, 1024], BF16, "Internal")
    cx.s_BT = dt("s_BT", [1024, T], BF16, "Internal")
    cx.s_CT = dt("s_CT", [1024, T], BF16, "Internal")
    cx.s_dt = dt("s_dt", [T, 64], F32, "Internal")
    cx.s_loga = dt("s_loga", [T, 64], F32, "Internal")
    cx.s_ynT = dt("s_ynT", [4096, T], BF16, "Internal")
    cx.s_qkT = dt("s_qkT", [2048, T], BF16, "Internal")
    cx.s_kvo = dt("s_kvo", [T, 5120], BF16, "Internal")
    cx.s_gates = dt("s_gates", [T, 8], F32, "Internal")
    cx.s_y = dt("s_y", [2048, T], F32, "Internal")
    with ExitStack() as es:
        block = es.enter_context(nc.Block())
        kb = KB(nc, es, block)
        sb = lambda name, shape, dtp: es.enter_context(nc.sbuf_tensor(name, shape, dtp))
        cx.ident_bf = sb("ident_bf", [128, 128], BF16)
        cx.junk = sb("junk", [128, D], BF16)
        cx.junk4 = sb("junk4", [128, 4096], BF16)
        cx.hn2 = [sb("hn_a", [128, D], BF16), sb("hn_b", [128, D], BF16)]
        cx.norm_pending = None
        cx.ss = sb("ss", [128, 8], F32)
        cx.epsc = sb("epsc", [128, 1], F32)
        cx.onec = sb("onec", [128, 1], F32)
        cx.psum = [es.enter_context(nc.psum_tensor(f"ps{i}", [128, 512], F32)) for i in range(6)]
        cx.ptr = [es.enter_context(nc.psum_tensor(f"ptr{i}", [128, 1024], BF16)) for i in range(2)]
        kb.dma("sp", cx.ident_bf[:, :], cx.c_ident_bf[:, :], writes=["ident"])
        kb.op("dve", lambda e: e.memset(cx.epsc[:, :], EPS), writes=["epsc"])
        kb.op("dve", lambda e: e.memset(cx.onec[:, :], 1.0), writes=["onec"])
        kb.barrier()
        h = cx.x
        for layer in layers:
            kind, li = layer % 3, layer // 3
            if kind == 0:
                ssd_inproj_phase(kb, cx, layer, li, h, T, SEQ)
                ssd_scan_phase(kb, cx, li, T, SEQ)
                outproj_phase(kb, cx, cx.ssd_w_out[li], 32, cx.s_ynT, h, cx.hA, T)
                h = cx.hA
            elif kind == 1:
                mlstm_layer(kb, cx, layer, li, h, cx.hA, T, SEQ)
                h = cx.hA
            else:
                s5_layer(kb, cx, layer, li, h, cx.hA, T, SEQ)
                h = cx.hA
            if do_mlp:
                mlp_phase(kb, cx, layer, h, cx.hB, T)
                h = cx.hB
        if do_final:
            final_norm_phase(kb, cx, h, cx.out, T)
        kb.barrier()
    return nc


PARAM_SHAPES = [
    ("norm_mix_g", (4, 2048)), ("norm_mlp_g", (4, 2048)),
    ("ssd_w_in", (2, 2048, 10304)), ("ssd_conv_w", (2, 4, 6144)), ("ssd_conv_b", (2, 6144)),
    ("ssd_dt_bias", (2, 64)), ("ssd_a_log", (2, 64)), ("ssd_d", (2, 64)), ("ssd_norm_g", (2, 4096)),
    ("ssd_w_out", (2, 4096, 2048)),
    ("mlstm_w_in", (1, 2048, 6152)), ("mlstm_gate_b", (1, 8)), ("mlstm_head_g", (1, 2048)),
    ("mlstm_w_out", (1, 2048, 2048)),
    ("s5_w_in", (1, 2048, 2048)), ("s5_b_re", (1, 128, 64, 16)), ("s5_b_im", (1, 128, 64, 16)),
    ("s5_c_re", (1, 128, 16, 64)), ("s5_c_im", (1, 128, 16, 64)), ("s5_d", (1, 2048)),
    ("s5_log_dt", (1, 128)), ("s5_a_re", (1, 128, 64)), ("s5_a_im", (1, 128, 64)),
    ("s5_w_out", (1, 2048, 4096)),
    ("mlp_w1", (4, 2048, 8192)), ("mlp_w2", (4, 8192, 2048)), ("final_norm_g", (1, 2048)),
]


def make_in_map(x_shard, params):
    m = {"x": np.ascontiguousarray(x_shard.reshape(-1, D))}
    for name, shape in PARAM_SHAPES:
        m[name] = np.ascontiguousarray(params[name]).reshape(shape)
    for name, arr in _consts_np().items():
        m["c_" + name] = arr
    return m
```
